# Optimizing a Trainium2 kernel written in Bass

```python
import jax, jax.numpy as jnp
from jax import lax
import numpy as np

D_MODEL = 2048
BATCH = 4
SEQ = 4096
DEPTH = 2

HEAD_DIM = 64
D_SB = D_MODEL // 4
D_FOX = D_MODEL // 2
CONV_CH = D_MODEL // 4
N_SB_HEADS = D_SB // HEAD_DIM
N_FOX_HEADS = D_FOX // HEAD_DIM
CONV_WIDTH = 31
MIX_COLS = 3 * D_SB + 3 * D_FOX + N_FOX_HEADS + 2 * CONV_CH
Q_BLOCK = 128

N_GROUPS = 4
EXPERTS_PER_GROUP = 8
N_EXPERTS = N_GROUPS * EXPERTS_PER_GROUP
TOP_K = 2
D_EXPERT = D_MODEL // 2
MOE_BLOCK = 256

ALPHA = (2 * DEPTH) ** 0.25
INIT_BETA = (8 * DEPTH) ** -0.25
LN_EPS = 1e-5

kernel_name = 'hybrid_sb_fox_conformer_hmoe_deepnorm_adaln'


def _ln(x):
    xf = x.astype(jnp.float32)
    mu = jnp.mean(xf, axis=-1, keepdims=True)
    var = jnp.mean(jnp.square(xf - mu), axis=-1, keepdims=True)
    return ((xf - mu) * lax.rsqrt(var + LN_EPS)).astype(x.dtype)


def _heads(t, n):
    b, s, _ = t.shape
    return t.reshape(b, s, n, HEAD_DIM).transpose(0, 2, 1, 3)


def _merge_blocks(o):
    nb, b, h, blk, dh = o.shape
    return o.transpose(1, 0, 3, 2, 4).reshape(b, nb * blk, h * dh)


def stick_breaking_attention(q, k, v):
    s = q.shape[2]
    scale = HEAD_DIM ** -0.5
    key_pos = jnp.arange(s)

    def block(i):
        q_blk = lax.dynamic_slice_in_dim(q, i * Q_BLOCK, Q_BLOCK, axis=2)
        z = jnp.einsum('bhqd,bhkd->bhqk', q_blk, k).astype(jnp.float32) * scale
        q_pos = i * Q_BLOCK + jnp.arange(Q_BLOCK)
        mask = key_pos[None, :] < q_pos[:, None]
        log_keep = jnp.where(mask, jax.nn.log_sigmoid(-z), 0.0)
        later = lax.cumsum(log_keep, axis=3, reverse=True) - log_keep
        w = jnp.where(mask, jnp.exp(jax.nn.log_sigmoid(z) + later), 0.0)
        return jnp.einsum('bhqk,bhkd->bhqd', w.astype(v.dtype), v)

    return _merge_blocks(lax.map(block, jnp.arange(s // Q_BLOCK)))


def forgetting_attention(q, k, v, log_f):
    s = q.shape[2]
    scale = HEAD_DIM ** -0.5
    key_pos = jnp.arange(s)
    cum = jnp.cumsum(log_f, axis=-1)

    def block(i):
        q_blk = lax.dynamic_slice_in_dim(q, i * Q_BLOCK, Q_BLOCK, axis=2)
        cum_q = lax.dynamic_slice_in_dim(cum, i * Q_BLOCK, Q_BLOCK, axis=2)
        z = jnp.einsum('bhqd,bhkd->bhqk', q_blk, k).astype(jnp.float32) * scale
        logits = z + cum_q[..., :, None] - cum[..., None, :]
        q_pos = i * Q_BLOCK + jnp.arange(Q_BLOCK)
        mask = key_pos[None, :] <= q_pos[:, None]
        p = jax.nn.softmax(jnp.where(mask, logits, -jnp.inf), axis=-1)
        return jnp.einsum('bhqk,bhkd->bhqd', p.astype(v.dtype), v)

    return _merge_blocks(lax.map(block, jnp.arange(s // Q_BLOCK)))


def conformer_conv(a, g, conv_w, conv_b, ln_g, ln_b):
    u = a * jax.nn.sigmoid(g)
    u = lax.conv_general_dilated(
        u, conv_w[:, None, :].astype(u.dtype), window_strides=(1,),
        padding=[(CONV_WIDTH - 1, 0)], dimension_numbers=('NWC', 'WIO', 'NWC'),
        feature_group_count=u.shape[-1]) + conv_b
    u = _ln(u) * ln_g + ln_b
    return jax.nn.silu(u)


def hybrid_mixer(h, w_in, b_forget, conv_w, conv_b, conv_ln_g, conv_ln_b, w_out):
    sizes = [D_SB, D_SB, D_SB, D_FOX, D_FOX, D_FOX, N_FOX_HEADS, CONV_CH, CONV_CH]
    offsets = [int(o) for o in np.cumsum(sizes)[:-1]]
    proj = h @ w_in
    q_sb, k_sb, v_sb, q_fx, k_fx, v_fx, f_logit, glu_a, glu_g = jnp.split(proj, offsets, axis=-1)

    o_sb = stick_breaking_attention(_heads(q_sb, N_SB_HEADS), _heads(k_sb, N_SB_HEADS),
                                    _heads(v_sb, N_SB_HEADS))
    log_f = jax.nn.log_sigmoid((f_logit + b_forget).astype(jnp.float32)).transpose(0, 2, 1)
    o_fx = forgetting_attention(_heads(q_fx, N_FOX_HEADS), _heads(k_fx, N_FOX_HEADS),
                                _heads(v_fx, N_FOX_HEADS), log_f)
    o_cv = conformer_conv(glu_a, glu_g, conv_w, conv_b, conv_ln_g, conv_ln_b)
    return jnp.concatenate([o_sb, o_fx, o_cv], axis=-1) @ w_out


def hierarchical_moe(h, r1_w, r1_b, r2_w, r2_b, w_gate, w_up, w_down):
    b, s, d = h.shape
    ht = h.reshape(b * s, d)
    n_tok = ht.shape[0]
    logits1 = (ht @ r1_w + r1_b).astype(jnp.float32)
    p1 = jax.nn.softmax(logits1, axis=-1)
    grp = jnp.argmax(logits1, axis=-1)
    p_grp = jnp.take_along_axis(p1, grp[:, None], axis=1)
    logits2 = (jnp.einsum('td,gde->tge', ht, r2_w) + r2_b).astype(jnp.float32)
    logits2 = jnp.take_along_axis(logits2, grp[:, None, None], axis=1)[:, 0]
    top_p, top_e = lax.top_k(jax.nn.softmax(logits2, axis=-1), TOP_K)
    weights = (p_grp * top_p / jnp.sum(top_p, axis=-1, keepdims=True)).astype(h.dtype)
    expert = grp[:, None] * EXPERTS_PER_GROUP + top_e

    flat_e = expert.reshape(-1)
    n_assign = flat_e.shape[0]
    order = jnp.argsort(flat_e)
    sorted_e = flat_e[order]
    counts = jnp.bincount(flat_e, length=N_EXPERTS)
    padded = (counts + MOE_BLOCK - 1) // MOE_BLOCK * MOE_BLOCK
    pad_start = jnp.cumsum(padded) - padded
    seg_start = jnp.cumsum(counts) - counts
    dest = pad_start[sorted_e] + jnp.arange(n_assign) - seg_start[sorted_e]
    n_blocks = -(-n_assign // MOE_BLOCK) + N_EXPERTS
    buf_tok = jnp.zeros((n_blocks * MOE_BLOCK,), jnp.int32).at[dest].set((order // TOP_K).astype(jnp.int32))
    block_expert = jnp.zeros((n_blocks,), jnp.int32).at[dest // MOE_BLOCK].set(sorted_e.astype(jnp.int32))
    xb = ht[buf_tok].reshape(n_blocks, MOE_BLOCK, d)

    def expert_block(args):
        xe, e = args
        hid = jax.nn.silu(xe @ w_gate[e]) * (xe @ w_up[e])
        return hid @ w_down[e]

    yb = lax.map(expert_block, (xb, block_expert)).reshape(n_blocks * MOE_BLOCK, d)
    y_assign = jnp.zeros((n_assign, d), h.dtype).at[order].set(yb[dest])
    y = jnp.sum(y_assign.reshape(n_tok, TOP_K, d) * weights[..., None], axis=1)
    return y.reshape(b, s, d)


def setup_inputs(seed: int = 0) -> dict:
    key = jax.random.key(seed)
    ks = jax.random.split(key, 24)
    L, D = DEPTH, D_MODEL
    nrm = lambda k, shape, sc: jax.random.normal(k, shape, jnp.float32) * sc
    return {
        'x': nrm(ks[0], (BATCH, SEQ, D), 1.0),
        'c': nrm(ks[1], (BATCH, D), 1.0),
        'ada_w': nrm(ks[2], (L, D, 6 * D), D ** -0.5),
        'ada_b': nrm(ks[3], (L, 6 * D), 0.01),
        'w_in': nrm(ks[4], (L, D, MIX_COLS), D ** -0.5),
        'b_forget': 1.0 + 4.0 * jax.random.uniform(ks[5], (L, N_FOX_HEADS), jnp.float32),
        'conv_w': nrm(ks[6], (L, CONV_WIDTH, CONV_CH), CONV_WIDTH ** -0.5),
        'conv_b': nrm(ks[7], (L, CONV_CH), 0.01),
        'conv_ln_g': 1.0 + nrm(ks[8], (L, CONV_CH), 0.05),
        'conv_ln_b': nrm(ks[9], (L, CONV_CH), 0.01),
        'w_out': nrm(ks[10], (L, D, D), D ** -0.5 * INIT_BETA),
        'ln1_g': 1.0 + nrm(ks[11], (L, D), 0.05),
        'ln1_b': nrm(ks[12], (L, D), 0.01),
        'r1_w': nrm(ks[13], (L, D, N_GROUPS), D ** -0.5),
        'r1_b': nrm(ks[14], (L, N_GROUPS), 0.01),
        'r2_w': nrm(ks[15], (L, N_GROUPS, D, EXPERTS_PER_GROUP), D ** -0.5),
        'r2_b': nrm(ks[16], (L, N_GROUPS, EXPERTS_PER_GROUP), 0.01),
        'w_gate': nrm(ks[17], (L, N_EXPERTS, D, D_EXPERT), D ** -0.5),
        'w_up': nrm(ks[18], (L, N_EXPERTS, D, D_EXPERT), D ** -0.5),
        'w_down': nrm(ks[19], (L, N_EXPERTS, D_EXPERT, D), D_EXPERT ** -0.5 * INIT_BETA),
        'ln2_g': 1.0 + nrm(ks[20], (L, D), 0.05),
        'ln2_b': nrm(ks[21], (L, D), 0.01),
    }


def reference(x, c, ada_w, ada_b, w_in, b_forget, conv_w, conv_b, conv_ln_g, conv_ln_b,
              w_out, ln1_g, ln1_b, r1_w, r1_b, r2_w, r2_b, w_gate, w_up, w_down,
              ln2_g, ln2_b):
    for l in range(DEPTH):
        mod = jax.nn.silu(c) @ ada_w[l] + ada_b[l]
        sh1, sc1, gt1, sh2, sc2, gt2 = [m[:, None, :] for m in jnp.split(mod, 6, axis=-1)]
        h = _ln(x) * (1.0 + sc1) + sh1
        y = hybrid_mixer(h, w_in[l], b_forget[l], conv_w[l], conv_b[l], conv_ln_g[l],
                         conv_ln_b[l], w_out[l])
        x = _ln(ALPHA * x + gt1 * y) * ln1_g[l] + ln1_b[l]
        h = _ln(x) * (1.0 + sc2) + sh2
        y = hierarchical_moe(h, r1_w[l], r1_b[l], r2_w[l], r2_b[l], w_gate[l], w_up[l], w_down[l])
        x = _ln(ALPHA * x + gt2 * y) * ln2_g[l] + ln2_b[l]
    return x
```

```python
D_MODEL = 2048; BATCH = 4; SEQ = 4096; DEPTH = 2
MIX_COLS = 5648
LN_EPS = 1e-5
ALPHA = (2 * DEPTH) ** 0.25
import numpy as np
import concourse.bass as bass
import concourse.mybir as mybir

F32 = mybir.dt.float32
BF16 = mybir.dt.bfloat16
I32 = mybir.dt.int32
ALU = mybir.AluOpType
AF = mybir.ActivationFunctionType
AX = mybir.AxisListType


class Buf:
    __slots__ = ("t", "name", "w", "r")

    def __init__(self, t, name):
        self.t = t
        self.name = name
        self.w = None
        self.r = []

    def __getitem__(self, idx):
        return self.t[idx]


class MK:
    def __init__(self, nc, n_dma_sems=24):
        self.nc = nc
        self.E = {"pe": nc.tensor, "act": nc.scalar, "dve": nc.vector,
                  "pool": nc.gpsimd, "sp": nc.sync}
        self.sem = {k: nc.alloc_semaphore("c_" + k) for k in self.E}
        self.cnt = {k: 0 for k in self.E}
        self.dsem = [nc.alloc_semaphore("d%d" % i) for i in range(n_dma_sems)]
        self.dcnt = [0] * n_dma_sems
        self.dnext = 0
        self.seen = {k: {} for k in self.E}
        self.nbuf = 0
        self.out_tokens = []

    def sb(self, shape, dtype, name=None):
        self.nbuf += 1
        name = name or "b%d" % self.nbuf
        return Buf(self.nc.alloc_sbuf_tensor(name, list(shape), dtype), name)

    def ps(self, shape, dtype, name=None):
        self.nbuf += 1
        name = name or "p%d" % self.nbuf
        return Buf(self.nc.alloc_psum_tensor(name, list(shape), dtype), name)

    def dram(self, name, shape, dtype, kind="Internal"):
        t = self.nc.dram_tensor(name, list(shape), dtype, kind=kind)
        return Buf(t.ap(), name)

    def _semobj(self, key):
        return self.sem[key] if isinstance(key, str) else self.dsem[key]

    def _wait(self, eng, tok):
        if tok is None:
            return
        key, val, _ = tok
        if self.seen[eng].get(key, 0) >= val:
            return
        self.E[eng].wait_ge(self._semobj(key), val)
        self.seen[eng][key] = val

    def _deps(self, eng, reads, writes, pe_accum=False):
        for b in reads:
            self._wait(eng, b.w)
        for b in writes:
            if not (pe_accum and b.w is not None and b.w[2] == "pe" and eng == "pe"):
                self._wait(eng, b.w)
            for tok in b.r:
                self._wait(eng, tok)

    def _commit(self, tok, reads, writes):
        for b in reads:
            b.r.append(tok)
            if len(b.r) > 6:
                d = {}
                for t in b.r:
                    if t[0] not in d or d[t[0]][1] < t[1]:
                        d[t[0]] = t
                b.r = list(d.values())
        for b in writes:
            b.w = tok
            b.r = []

    def op(self, eng, fn, reads=(), writes=(), pe_accum=False):
        self._deps(eng, reads, writes, pe_accum)
        ins = fn(self.E[eng])
        self.cnt[eng] += 1
        ins.then_inc(self.sem[eng], 1)
        tok = (eng, self.cnt[eng], eng)
        self._commit(tok, reads, writes)
        return tok

    def dma(self, out, in_, reads=(), writes=(), eng="sp", is_output=False, **kw):
        i = self.dnext
        self.dnext = (self.dnext + 1) % len(self.dsem)
        if self.dcnt[i] > 0:
            self._wait(eng, (i, self.dcnt[i], "dma"))
        self._deps(eng, reads, writes)
        ins = self.E[eng].dma_start(out=out, in_=in_, **kw)
        self.dcnt[i] += 16
        ins.then_inc(self.dsem[i], 16)
        tok = (i, self.dcnt[i], "dma")
        self._commit(tok, reads, writes)
        if is_output:
            self.out_tokens.append(tok)
        return tok

    def finish(self, eng="sp"):
        for tok in self.out_tokens:
            key, val, _ = tok
            self.E[eng].wait_ge(self._semobj(key), val)
        for i, c in enumerate(self.dcnt):
            if c > 0:
                self.E[eng].wait_ge(self.dsem[i], c)
def build_M():
    nc = bass.Bass("TRN2", target_bir_lowering=False)
    cT = nc.dram_tensor("cT", [128, 16, 4], F32, kind="ExternalInput").ap()
    aw = nc.dram_tensor("aw", [2, 2048, 1536], F32, kind="ExternalInput").ap()
    ab = nc.dram_tensor("ab", [2, 1536], F32, kind="ExternalInput").ap()
    mo = nc.dram_tensor("mo", [2, 4, 1536], F32, kind="ExternalOutput").ap()
    m = MK(nc)
    ct = m.sb([128, 16, 4], F32)
    sc = m.sb([128, 16, 4], F32)
    wst = [m.sb([128, 16, 512], F32) for _ in range(2)]
    bt = [m.sb([4, 512], F32) for _ in range(2)]
    ot = [m.sb([4, 512], F32) for _ in range(2)]
    pp = [m.ps([4, 512], F32) for _ in range(2)]
    m.dma(ct[:], cT, writes=[ct])
    m.op("act", lambda e: e.activation(sc[:], ct[:], AF.Silu), reads=[ct], writes=[sc])
    i = 0
    for l in range(2):
        for n in range(3):
            w = wst[i % 2]; b = bt[i % 2]; o = ot[i % 2]; p = pp[i % 2]
            m.dma(w[:], aw[l, :, n * 512:(n + 1) * 512].rearrange("(k p) n -> p k n", p=128), writes=[w])
            m.dma(b[:], ab[l, n * 512:(n + 1) * 512].partition_broadcast(4), writes=[b])
            for k in range(16):
                m.op("pe", lambda e: e.matmul(p[:], sc[:, k, :], w[:, k, :], start=(k == 0), stop=(k == 15)),
                     reads=[sc, w], writes=[p], pe_accum=True)
            m.op("dve", lambda e: e.tensor_tensor(o[:], p[:], b[:], ALU.add), reads=[p, b], writes=[o])
            m.dma(mo[l, :, n * 512:(n + 1) * 512], o[:], reads=[o], is_output=True)
            i += 1
    m.finish()
    return nc


def make_ident(m, dtype):
    idf = m.sb([128, 128], F32)
    m.op("pool", lambda e: e.memset(idf[:], 1.0), writes=[idf])
    m.op("pool", lambda e: e.affine_select(out=idf[:], in_=idf[:], pattern=[[1, 128]], compare_op=ALU.is_equal,
                                           fill=0.0, base=0, channel_multiplier=-1), reads=[idf], writes=[idf])
    if dtype == F32:
        return idf
    idb = m.sb([128, 128], dtype)
    m.op("dve", lambda e: e.tensor_copy(idb[:], idf[:]), reads=[idf], writes=[idb])
    return idb


def ln_stats(m, xt, st, mv, rstd):
    for q in range(4):
        m.op("dve", lambda e: e.bn_stats(st[:, q * 6:(q + 1) * 6], xt[:, q * 512:(q + 1) * 512]), reads=[xt], writes=[st])
    m.op("dve", lambda e: e.bn_aggr(mv[:], st[:]), reads=[st], writes=[mv])
    m.op("dve", lambda e: e.tensor_scalar_add(rstd[:], mv[:, 1:2], LN_EPS), reads=[mv], writes=[rstd])
    m.op("act", lambda e: e.activation(rstd[:], rstd[:], AF.Sqrt), reads=[rstd], writes=[rstd])
    m.op("dve", lambda e: e.reciprocal(rstd[:], rstd[:]), reads=[rstd], writes=[rstd])


FM_BLOCKS = [(0, "qk", 0), (256, "qk", 256), (512, "qk", 512), (768, "qk", 768),
             (1536, "qk", 1024), (1792, "qk", 1280), (2048, "qk", 1536), (2304, "qk", 1792),
             (2560, "qk", 2048), (2816, "qk", 2304), (3072, "qk", 2560), (3328, "qk", 2816),
             (4608, "fag", 0), (4864, "fag", 256), (5120, "fag", 512), (5376, "fag", 768)]
TM_BLOCKS = [(1024, 0), (1280, 256), (3584, 512), (3840, 768), (4096, 1024), (4352, 1280)]


def emit_A(m, nc, x, modv, w_in, qkT, fagT, vtm, ident):
    NT = 16
    hT = m.sb([128, 16, 2048], BF16, "hT")
    scb = m.sb([128, 2048], F32, "scb")
    shb = m.sb([128, 2048], F32, "shb")
    xts = [m.sb([128, 2048], F32) for _ in range(2)]
    hb = [m.sb([128, 2048], BF16) for _ in range(2)]
    st = m.sb([128, 24], F32); mv = m.sb([128, 2], F32); rstd = m.sb([128, 1], F32)
    pT = [m.ps([128, 8, 128], BF16) for _ in range(2)]
    m.dma(shb[:], modv[0, :].partition_broadcast(128), writes=[shb])
    m.dma(scb[:], modv[1, :].partition_broadcast(128), writes=[scb])
    m.op("pool", lambda e: e.tensor_scalar_add(scb[:], scb[:], 1.0), reads=[scb], writes=[scb])
    m.dma(xts[0][:], x[0:128, :], writes=[xts[0]])
    for t in range(NT):
        xt = xts[t % 2]; h = hb[t % 2]
        if t + 1 < NT:
            m.dma(xts[(t + 1) % 2][:], x[(t + 1) * 128:(t + 2) * 128, :], writes=[xts[(t + 1) % 2]])
        ln_stats(m, xt, st, mv, rstd)
        m.op("dve", lambda e: e.tensor_scalar(xt[:], xt[:], mv[:, 0:1], rstd[:, 0:1], ALU.subtract, ALU.mult),
             reads=[xt, mv, rstd], writes=[xt])
        m.op("pool", lambda e: e.tensor_tensor(xt[:], xt[:], scb[:], ALU.mult), reads=[xt, scb], writes=[xt])
        m.op("dve", lambda e: e.tensor_tensor(h[:], xt[:], shb[:], ALU.add), reads=[xt, shb], writes=[h])
        for half in range(2):
            p = pT[half]
            for j in range(8):
                k = half * 8 + j
                m.op("pe", lambda e: e.transpose(p[:, j, :], h[:, k * 128:(k + 1) * 128], ident[:]),
                     reads=[h, ident], writes=[p], pe_accum=True)
            m.op("act", lambda e: e.copy(hT[:, half * 8:(half + 1) * 8, t * 128:(t + 1) * 128], p[:]),
                 reads=[p], writes=[hT])
    wst = [m.sb([128, 16, 256], F32) for _ in range(2)]
    wbf = [m.sb([128, 16, 256], BF16) for _ in range(2)]
    pm = [m.ps([128, 512], F32) for _ in range(4)]
    oq = [m.sb([128, 2048], BF16) for _ in range(2)]
    of = [m.sb([128, 2048], F32) for _ in range(2)]
    ov = [m.sb([128, 256], BF16) for _ in range(2)]
    blocks = [("fm", c0, kind, o0) for (c0, kind, o0) in FM_BLOCKS] + [("tm", c0, None, o0) for (c0, o0) in TM_BLOCKS]
    blocks.append(("fm16", 5632, "fag", 1024))
    pi = 0; oi = 0
    def load_w(bi):
        typ, c0, kind, o0 = blocks[bi]
        cw = 16 if typ == "fm16" else 256
        m.dma(wst[bi % 2][:, :, 0:cw], w_in[:, c0:c0 + cw].rearrange("(k p) n -> p k n", p=128), writes=[wst[bi % 2]])
    load_w(0)
    for bi, (typ, c0, kind, o0) in enumerate(blocks):
        cw = 16 if typ == "fm16" else 256
        ws = wst[bi % 2]; wb = wbf[bi % 2]
        if bi + 1 < len(blocks):
            load_w(bi + 1)
        m.op("act", lambda e: e.copy(wb[:, 0:8, 0:cw], ws[:, 0:8, 0:cw]), reads=[ws], writes=[wb])
        m.op("pool", lambda e: e.tensor_copy(wb[:, 8:16, 0:cw], ws[:, 8:16, 0:cw]), reads=[ws], writes=[wb])
        if typ in ("fm", "fm16"):
            for cc in range(0, cw, 128):
                cn = min(128, cw - cc)
                o = (oq if kind == "qk" else of)[oi % 2]; oi += 1
                for tg in range(4):
                    p = pm[pi % 4]; pi += 1
                    for k in range(16):
                        m.op("pe", lambda e: e.matmul(p[0:cn, :], wb[:, k, cc:cc + cn], hT[:, k, tg * 512:(tg + 1) * 512],
                                                      start=(k == 0), stop=(k == 15)),
                             reads=[wb, hT], writes=[p], pe_accum=True)
                    if tg % 2 == 0:
                        m.op("act", lambda e: e.copy(o[0:cn, tg * 512:(tg + 1) * 512], p[0:cn, :]), reads=[p], writes=[o])
                    else:
                        m.op("dve", lambda e: e.tensor_copy(o[0:cn, tg * 512:(tg + 1) * 512], p[0:cn, :]), reads=[p], writes=[o])
                dst = qkT if kind == "qk" else fagT
                m.dma(dst[o0 + cc:o0 + cc + cn, :], o[0:cn, :], reads=[o], is_output=True)
        else:
            for tt in range(16):
                p = pm[pi % 4]; pi += 1
                o = ov[oi % 2]; oi += 1
                for k in range(16):
                    m.op("pe", lambda e: e.matmul(p[:, 0:256], hT[:, k, tt * 128:(tt + 1) * 128], wb[:, k, :],
                                                  start=(k == 0), stop=(k == 15)),
                         reads=[wb, hT], writes=[p], pe_accum=True)
                if tt % 2 == 0:
                    m.op("act", lambda e: e.copy(o[:], p[:, 0:256]), reads=[p], writes=[o])
                else:
                    m.op("dve", lambda e: e.tensor_copy(o[:], p[:, 0:256]), reads=[p], writes=[o])
                m.dma(vtm[tt * 128:(tt + 1) * 128, o0:o0 + 256], o[:], reads=[o], is_output=True)


def build_A():
    nc = bass.Bass("TRN2", target_bir_lowering=False)
    x = nc.dram_tensor("x", [2048, 2048], F32, kind="ExternalInput").ap()
    modv = nc.dram_tensor("modv", [2, 2048], F32, kind="ExternalInput").ap()
    w_in = nc.dram_tensor("w_in", [2048, MIX_COLS], F32, kind="ExternalInput").ap()
    qkT = nc.dram_tensor("qkT", [3072, 2048], BF16, kind="ExternalOutput").ap()
    fagT = nc.dram_tensor("fagT", [1040, 2048], F32, kind="ExternalOutput").ap()
    vtm = nc.dram_tensor("vtm", [2048, 1536], BF16, kind="ExternalOutput").ap()
    m = MK(nc)
    ident = make_ident(m, BF16)
    emit_A(m, nc, x, modv, w_in, qkT, fagT, vtm, ident)
    m.finish()
    return nc
def barrier(m):
    for e in m.E:
        for f in m.E:
            if m.cnt[f] > 0:
                m._wait(e, (f, m.cnt[f], f))
        for i, c in enumerate(m.dcnt):
            if c > 0:
                m._wait(e, (i, c, "dma"))


def emit_conv(m, nc, es, aT, gT, cwT, cb, lng, lnb, o_cv, identf, PS):
    def sb(shape, dt):
        m.nbuf += 1
        return Buf(es.enter_context(nc.sbuf_tensor("cv%d" % m.nbuf, list(shape), dt)), "cv")
    cvT = sb([128, 4, 2048], F32)
    at = [sb([128, 2078], F32) for _ in range(2)]
    gt = [sb([128, 2078], F32) for _ in range(2)]
    cw = sb([128, 4, 31], F32); cbt = sb([128, 4], F32)
    lg = sb([128, 512], F32); lb = sb([128, 512], F32)
    m.dma(cw[:], cwT, writes=[cw])
    m.dma(cbt[:], cb, writes=[cbt])
    m.dma(lg[:], lng.partition_broadcast(128), writes=[lg])
    m.dma(lb[:], lnb.partition_broadcast(128), writes=[lb])
    for cc in range(4):
        a = at[cc % 2]; g = gt[cc % 2]
        m.dma(a[:], aT[cc * 128:(cc + 1) * 128, :], writes=[a])
        m.dma(g[:], gT[cc * 128:(cc + 1) * 128, :], writes=[g])
        m.op("act", lambda e: e.activation(g[:], g[:], AF.Sigmoid), reads=[g], writes=[g])
        m.op("dve", lambda e: e.tensor_tensor(a[:], a[:], g[:], ALU.mult), reads=[a, g], writes=[a])
        m.op("dve", lambda e: e.tensor_scalar(cvT[:, cc, :], a[:, 0:2048], cw[:, cc, 0:1], cbt[:, cc:cc + 1], ALU.mult, ALU.add),
             reads=[a, cw, cbt], writes=[cvT])
        for w in range(1, 31):
            m.op("dve", lambda e: e.scalar_tensor_tensor(cvT[:, cc, :], a[:, w:w + 2048], cw[:, cc, w:w + 1], cvT[:, cc, :], ALU.mult, ALU.add),
                 reads=[a, cw, cvT], writes=[cvT])
    pt = PS["z"]
    ut = [sb([128, 512], F32) for _ in range(2)]
    ob = [sb([128, 512], BF16) for _ in range(2)]
    st = sb([128, 6], F32); mv = sb([128, 2], F32); rstd = sb([128, 1], F32)
    for t in range(16):
        p = pt[t % 2]; u = ut[t % 2]; o = ob[t % 2]
        for cc in range(4):
            m.op("pe", lambda e: e.transpose(p[:, cc * 128:(cc + 1) * 128], cvT[:, cc, t * 128:(t + 1) * 128], identf[:]),
                 reads=[cvT, identf], writes=[p], pe_accum=True)
        m.op("act", lambda e: e.copy(u[:], p[:]), reads=[p], writes=[u])
        m.op("dve", lambda e: e.bn_stats(st[:], u[:]), reads=[u], writes=[st])
        m.op("dve", lambda e: e.bn_aggr(mv[:], st[:]), reads=[st], writes=[mv])
        m.op("dve", lambda e: e.tensor_scalar_add(rstd[:], mv[:, 1:2], LN_EPS), reads=[mv], writes=[rstd])
        m.op("act", lambda e: e.activation(rstd[:], rstd[:], AF.Sqrt), reads=[rstd], writes=[rstd])
        m.op("dve", lambda e: e.reciprocal(rstd[:], rstd[:]), reads=[rstd], writes=[rstd])
        m.op("dve", lambda e: e.tensor_scalar(u[:], u[:], mv[:, 0:1], rstd[:, 0:1], ALU.subtract, ALU.mult),
             reads=[u, mv, rstd], writes=[u])
        m.op("pool", lambda e: e.tensor_tensor(u[:], u[:], lg[:], ALU.mult), reads=[u, lg], writes=[u])
        m.op("dve", lambda e: e.tensor_tensor(u[:], u[:], lb[:], ALU.add), reads=[u, lb], writes=[u])
        m.op("act", lambda e: e.activation(o[:], u[:], AF.Silu), reads=[u], writes=[o])
        m.dma(o_cv[t * 128:(t + 1) * 128, :], o[:], reads=[o], is_output=True)


def emit_sb(m, nc, es, sbqT, sbkTr, sbvr, o_sb, identb, PS, nheads=4):
    def sb(shape, dt):
        m.nbuf += 1
        return Buf(es.enter_context(nc.sbuf_tensor("sb%d" % m.nbuf, list(shape), dt)), "sb")
    QT = [sb([64, 4096], BF16) for _ in range(2)]
    KT = [sb([64, 4096], BF16) for _ in range(2)]
    VR = [sb([128, 32, 64], BF16) for _ in range(2)]
    ones = sb([128, 512], F32)
    m.op("pool", lambda e: e.memset(ones[:], 1.0), writes=[ones])
    NB = 2
    e_t = [sb([128, 512], F32) for _ in range(NB)]
    sp_t = [sb([128, 512], F32) for _ in range(NB)]
    r_t = [sb([128, 512], F32) for _ in range(NB)]
    la_t = [sb([128, 512], F32) for _ in range(NB)]
    a_t = [sb([128, 512], BF16) for _ in range(NB)]
    aT_t = [sb([128, 4, 128], BF16) for _ in range(NB)]
    ost = [sb([128, 32, 64], BF16) for _ in range(2)]
    pz = PS["z"]; pT = PS["T"]; po = PS["o"][0]

    def load(h):
        m.dma(QT[h % 2][:], sbqT[h], writes=[QT[h % 2]])
        m.dma(KT[h % 2][:], sbkTr[h], writes=[KT[h % 2]])
        m.dma(VR[h % 2][:], sbvr[:, h * 64:(h + 1) * 64].rearrange("(c p) d -> p c d", p=128), writes=[VR[h % 2]])
    load(0)
    ci = 0
    for h in range(nheads):
        if h + 1 < nheads:
            load(h + 1)
        q = QT[h % 2]; k = KT[h % 2]; v = VR[h % 2]; os_ = ost[h % 2]
        for i in range(32):
            k0 = 128 * (31 - i)
            nkb = i + 1
            nch = (nkb + 3) // 4
            prev_r = None
            for c in range(nch):
                c0 = k0 + 512 * c
                w = min(512, 4096 - c0)
                nb = w // 128
                z = pz[ci % 2]; et = e_t[ci % NB]; spt = sp_t[ci % NB]; rt = r_t[ci % NB]
                lat = la_t[ci % NB]; at_ = a_t[ci % NB]; aTt = aT_t[ci % NB]; pt = pT[ci % 2]
                ci += 1
                m.op("pe", lambda e: e.matmul(z[:, 0:w], q[:, i * 128:(i + 1) * 128], k[:, c0:c0 + w], start=True, stop=True),
                     reads=[q, k], writes=[z])
                m.op("act", lambda e: e.activation(et[:, 0:w], z[:, 0:w], AF.Exp, scale=0.125), reads=[z], writes=[et])
                m.op("act", lambda e: e.activation(spt[:, 0:w], et[:, 0:w], AF.Ln, bias=1.0), reads=[et], writes=[spt])
                if c == 0:
                    m.op("pool", lambda e: e.affine_select(out=spt[:, 0:128], in_=spt[:, 0:128], pattern=[[1, 128]],
                                                           compare_op=ALU.is_gt, fill=0.0, base=-127, channel_multiplier=1),
                         reads=[spt], writes=[spt])
                init = 0.0 if prev_r is None else prev_r[0][:, prev_r[1] - 1:prev_r[1]]
                rd = [ones, spt] + ([] if prev_r is None else [prev_r[0]])
                m.op("dve", lambda e: e.tensor_tensor_scan(rt[:, 0:w], ones[:, 0:w], spt[:, 0:w], init, ALU.mult, ALU.add),
                     reads=rd, writes=[rt])
                prev_r = (rt, w)
                m.op("dve", lambda e: e.scalar_tensor_tensor(lat[:, 0:w], z[:, 0:w], 0.125, rt[:, 0:w], ALU.mult, ALU.subtract),
                     reads=[z, rt], writes=[lat])
                m.op("act", lambda e: e.activation(at_[:, 0:w], lat[:, 0:w], AF.Exp), reads=[lat], writes=[at_])
                if c == 0:
                    m.op("pool", lambda e: e.affine_select(out=at_[:, 0:128], in_=at_[:, 0:128], pattern=[[1, 128]],
                                                           compare_op=ALU.is_gt, fill=0.0, base=-127, channel_multiplier=1),
                         reads=[at_], writes=[at_])
                for bb in range(nb):
                    m.op("pe", lambda e: e.transpose(pt[:, bb, :], at_[:, bb * 128:(bb + 1) * 128], identb[:]),
                         reads=[at_, identb], writes=[pt], pe_accum=True)
                m.op("dve", lambda e: e.tensor_copy(aTt[:, 0:nb, :], pt[:, 0:nb, :]), reads=[pt], writes=[aTt])
                for bb in range(nb):
                    kb = (c0 // 128) + bb
                    first = (c == 0 and bb == 0); last = (c == nch - 1 and bb == nb - 1)
                    m.op("pe", lambda e: e.matmul(po[:, 0:64], aTt[:, bb, :], v[:, kb, :], start=first, stop=last),
                         reads=[aTt, v], writes=[po], pe_accum=True)
            m.op("act", lambda e: e.copy(os_[:, i, :], po[:, 0:64]), reads=[po], writes=[os_])
        m.dma(o_sb[:, h * 64:(h + 1) * 64].rearrange("(i p) d -> p i d", p=128), os_[:], reads=[os_], is_output=True)


def emit_fox(m, nc, es, fxqT, fxkT, fxv, fT, negb, o_fx, identf, PS, nheads=8):
    def sb(shape, dt):
        m.nbuf += 1
        return Buf(es.enter_context(nc.sbuf_tensor("fx%d" % m.nbuf, list(shape), dt)), "fx")
    NH = nheads
    C = sb([NH, 4096], F32); W1 = sb([NH, 4096], F32); nb_t = sb([NH, 1], F32)
    onesr = sb([NH, 4096], F32)
    m.dma(W1[:], fT[0:NH, :], writes=[W1])
    m.dma(nb_t[:], negb[0:NH, :], writes=[nb_t])
    m.op("pool", lambda e: e.memset(onesr[:], 1.0), writes=[onesr])
    m.op("dve", lambda e: e.tensor_scalar_mul(nb_t[:], nb_t[:], -1.0), reads=[nb_t], writes=[nb_t])
    m.op("act", lambda e: e.activation(W1[:], W1[:], AF.Exp, bias=nb_t[:, 0:1], scale=-1.0), reads=[W1, nb_t], writes=[W1])
    m.op("act", lambda e: e.activation(W1[:], W1[:], AF.Ln, bias=1.0), reads=[W1], writes=[W1])
    m.op("dve", lambda e: e.tensor_tensor_scan(C[:], onesr[:], W1[:], 0.0, ALU.mult, ALU.add), reads=[onesr, W1], writes=[C])
    CkT = sb([128, 32, NH], F32)
    pc = PS["misc"]
    for j in range(32):
        m.op("pe", lambda e: e.transpose(pc[:, j * NH:(j + 1) * NH], C[:, j * 128:(j + 1) * 128], identf[0:NH, 0:NH]),
             reads=[C, identf], writes=[pc], pe_accum=True)
    m.op("dve", lambda e: e.tensor_copy(CkT[:].rearrange("p j h -> p (j h)"), pc[:, 0:32 * NH]), reads=[pc], writes=[CkT])
    Rd = sb([NH, NH, 8], F32)
    Cg = C[:].rearrange("h (g q) -> h g q", q=512)
    m.op("dve", lambda e: e.tensor_tensor(Rd[:], Cg[:, :, 511:512].rearrange("h g o -> h o g").to_broadcast([NH, NH, 8]),
                                          identf[0:NH, 0:NH].unsqueeze(2).to_broadcast([NH, NH, 8]), ALU.mult),
         reads=[C, identf], writes=[Rd])
    onesk = sb([NH, 128], F32)
    m.op("pool", lambda e: e.memset(onesk[:], 1.0), writes=[onesk])
    m.op("pe", lambda e: e.matmul(pc[:, 256:256 + NH * 8], onesk[:], Rd[:].rearrange("h a g -> h (a g)"), start=True, stop=True),
         reads=[onesk, Rd, CkT], writes=[pc])
    Rbc = sb([128, NH, 8], F32)
    m.op("dve", lambda e: e.tensor_copy(Rbc[:].rearrange("p h g -> p (h g)"), pc[:, 256:256 + NH * 8]), reads=[pc], writes=[Rbc])
    bias = sb([128, NH, 8, 32], F32)
    for h in range(NH):
        for g in range(8):
            nj = 4 * g + 4
            m.op("dve", lambda e: e.tensor_scalar(bias[:, h, g, 0:nj], CkT[:, 0:nj, h], Rbc[:, h, g:g + 1], None, ALU.subtract),
                 reads=[CkT, Rbc], writes=[bias])
    D8 = sb([NH, 4096], F32); Dhi = sb([NH, 4096], BF16); Dlo = sb([NH, 4096], BF16)
    D8g = D8[:].rearrange("h (g q) -> h g q", q=512)
    m.op("dve", lambda e: e.tensor_tensor(D8g, Cg[:, :, 511:512].to_broadcast([NH, 8, 512]), Cg, ALU.subtract),
         reads=[C], writes=[D8])
    m.op("dve", lambda e: e.tensor_scalar_mul(D8[:], D8[:], 8.0), reads=[D8], writes=[D8])
    m.op("dve", lambda e: e.tensor_copy(Dhi[:], D8[:]), reads=[D8], writes=[Dhi])
    m.op("dve", lambda e: e.tensor_tensor(D8[:], D8[:], Dhi[:], ALU.subtract), reads=[D8, Dhi], writes=[D8])
    m.op("dve", lambda e: e.tensor_copy(Dlo[:], D8[:]), reads=[D8], writes=[Dlo])
    QT = [sb([66, 4096], BF16) for _ in range(2)]
    KT = [sb([66, 4096], BF16) for _ in range(2)]
    V = [sb([128, 32, 65], BF16) for _ in range(2)]
    for i in range(2):
        m.op("pool", lambda e: e.memset(KT[i][64:66, :], 1.0), writes=[KT[i]])
        m.op("pool", lambda e: e.memset(V[i][:, :, 64:65], 1.0), writes=[V[i]])
    NP = 3
    P_t = [sb([128, 512], BF16) for _ in range(NP)]
    ost = [sb([128, 32, 64], BF16) for _ in range(2)]
    rc = sb([128, 4], F32)
    pz = PS["z"]; po = PS["o"]

    def load(h):
        m.dma(QT[h % 2][0:64, :], fxqT[h], writes=[QT[h % 2]])
        m.dma(QT[h % 2][64:65, :], Dhi[h:h + 1, :], reads=[Dhi], writes=[QT[h % 2]])
        m.dma(QT[h % 2][65:66, :], Dlo[h:h + 1, :], reads=[Dlo], writes=[QT[h % 2]])
        m.dma(KT[h % 2][0:64, :], fxkT[h], writes=[KT[h % 2]])
        m.dma(V[h % 2][:, :, 0:64], fxv[:, h * 64:(h + 1) * 64].rearrange("(c p) d -> p c d", p=128), writes=[V[h % 2]])
    load(0)
    ci = 0
    for h in range(NH):
        if h + 1 < NH:
            load(h + 1)
        q = QT[h % 2]; k = KT[h % 2]; v = V[h % 2]; os_ = ost[h % 2]
        for g in range(8):
            for j in range(4 * g + 4):
                md = j - 4 * g
                q0 = 128 * max(0, md)
                w = 512 - q0
                z = pz[ci % 2]; P = P_t[ci % NP]; ci += 1
                m.op("pe", lambda e: e.matmul(z[:, 0:w], k[:, j * 128:(j + 1) * 128], q[:, 512 * g + q0:512 * g + 512], start=True, stop=True),
                     reads=[q, k], writes=[z])
                m.op("act", lambda e: e.activation(P[:, 0:w], z[:, 0:w], AF.Exp, bias=bias[:, h, g, j:j + 1], scale=0.125),
                     reads=[z, bias], writes=[P])
                if md >= 0:
                    m.op("pool", lambda e: e.affine_select(out=P[:, 0:128], in_=P[:, 0:128], pattern=[[1, 128]],
                                                           compare_op=ALU.is_ge, fill=0.0, base=0, channel_multiplier=-1),
                         reads=[P], writes=[P])
                for s in range(max(0, md), 4):
                    lc = (s * 128) - q0
                    m.op("pe", lambda e: e.matmul(po[s][:, 0:65], P[:, lc:lc + 128], v[:, j, :], start=(j == 0), stop=(j == 4 * g + s)),
                         reads=[P, v], writes=[po[s]], pe_accum=True)
            for s in range(4):
                m.op("dve", lambda e: e.reciprocal(rc[:, s:s + 1], po[s][:, 64:65]), reads=[po[s]], writes=[rc])
                m.op("dve", lambda e: e.tensor_scalar(os_[:, 4 * g + s, :], po[s][:, 0:64], rc[:, s:s + 1], None, ALU.mult),
                     reads=[po[s], rc], writes=[os_])
        m.dma(o_fx[:, h * 64:(h + 1) * 64].rearrange("(i p) d -> p i d", p=128), os_[:], reads=[os_], is_output=True)


def build_B(do_conv=True, n_sb=4, n_fx=8):
    from contextlib import ExitStack
    nc = bass.Bass("TRN2", target_bir_lowering=False)
    dt = lambda n, s, d, k="ExternalInput": nc.dram_tensor(n, s, d, kind=k).ap()
    sbqT = dt("sbqT", [4, 64, 4096], BF16); sbkTr = dt("sbkTr", [4, 64, 4096], BF16); sbvr = dt("sbvr", [4096, 256], BF16)
    fxqT = dt("fxqT", [8, 64, 4096], BF16); fxkT = dt("fxkT", [8, 64, 4096], BF16); fxv = dt("fxv", [4096, 512], BF16)
    fT = dt("fT", [8, 4096], F32); negb = dt("bfg", [8, 1], F32)
    aT = dt("aT", [512, 2078], F32); gT = dt("gT", [512, 2078], F32)
    cwT = dt("cwT", [128, 4, 31], F32); cb = dt("cb", [128, 4], F32); lng = dt("lng", [512], F32); lnb = dt("lnb", [512], F32)
    o_sb = dt("o_sb", [4096, 256], BF16, "ExternalOutput"); o_fx = dt("o_fx", [4096, 512], BF16, "ExternalOutput")
    o_cv = dt("o_cv", [2048, 512], BF16, "ExternalOutput")
    m = MK(nc)
    identf = make_ident(m, F32)
    identb = m.sb([128, 128], BF16)
    m.op("dve", lambda e: e.tensor_copy(identb[:], identf[:]), reads=[identf], writes=[identb])
    PS = {"z": [m.ps([128, 512], F32) for _ in range(2)],
          "o": [m.ps([128, 512], F32) for _ in range(4)],
          "T": [m.ps([128, 4, 128], BF16) for _ in range(1)] * 2,
          "misc": m.ps([128, 512], F32)}
    if do_conv:
        with ExitStack() as es:
            emit_conv(m, nc, es, aT, gT, cwT, cb, lng, lnb, o_cv, identf, PS)
            barrier(m)
    if n_sb:
        with ExitStack() as es:
            emit_sb(m, nc, es, sbqT, sbkTr, sbvr, o_sb, identb, PS, n_sb)
            barrier(m)
    if n_fx:
        with ExitStack() as es:
            emit_fox(m, nc, es, fxqT, fxkT, fxv, fT, negb, o_fx, identf, PS, n_fx)
            barrier(m)
    m.finish()
    return nc
def ln_affine(m, r, st, mv, rstd, g_bc, b_bc, out):
    ln_stats(m, r, st, mv, rstd)
    m.op("dve", lambda e: e.tensor_scalar(r[:], r[:], mv[:, 0:1], rstd[:, 0:1], ALU.subtract, ALU.mult),
         reads=[r, mv, rstd], writes=[r])
    m.op("pool", lambda e: e.tensor_tensor(r[:], r[:], g_bc[:], ALU.mult), reads=[r, g_bc], writes=[r])
    m.op("dve", lambda e: e.tensor_tensor(out[:], r[:], b_bc[:], ALU.add), reads=[r, b_bc], writes=[out])


def emit_C1(m, nc, es, x, o, modv, w_out, ln1g, ln1b, rw, rb, x1_d, h2T_d, Wt, identb, identf, PS):
    def sb(shape, dt):
        m.nbuf += 1
        return Buf(es.enter_context(nc.sbuf_tensor("c1_%d" % m.nbuf, list(shape), dt)), "c1")
    wob = sb([128, 16, 2048], BF16)
    wst = [sb([128, 2048], F32) for _ in range(2)]
    bc = {}
    for nm, src in (("gt1", modv[0, :]), ("sh2", modv[1, :]), ("sc2", modv[2, :]), ("l1g", ln1g), ("l1b", ln1b)):
        bc[nm] = sb([128, 2048], F32)
        m.dma(bc[nm][:], src.partition_broadcast(128), writes=[bc[nm]])
    m.op("pool", lambda e: e.tensor_scalar_add(bc["sc2"][:], bc["sc2"][:], 1.0), reads=[bc["sc2"]], writes=[bc["sc2"]])
    rwt = sb([128, 16, 36], F32); rbt = sb([128, 36], F32)
    m.dma(rwt[:], rw.rearrange("(k p) n -> p k n", p=128), writes=[rwt])
    m.dma(rbt[:], rb.partition_broadcast(128), writes=[rbt])
    m.dma(wst[0][:], w_out[0:128, :], writes=[wst[0]])
    for k in range(16):
        if k + 1 < 16:
            m.dma(wst[(k + 1) % 2][:], w_out[(k + 1) * 128:(k + 2) * 128, :], writes=[wst[(k + 1) % 2]])
        if k % 2 == 0:
            m.op("act", lambda e: e.copy(wob[:, k, :], wst[k % 2][:]), reads=[wst[k % 2]], writes=[wob])
        else:
            m.op("pool", lambda e: e.tensor_copy(wob[:, k, :], wst[k % 2][:]), reads=[wst[k % 2]], writes=[wob])
    ot = [sb([128, 2048], BF16) for _ in range(2)]
    xt = [sb([128, 2048], F32) for _ in range(2)]
    oT = sb([128, 16, 128], BF16)
    tmp = sb([128, 2048], F32); x1 = sb([128, 2048], F32); h2f = sb([128, 2048], F32); h2b = sb([128, 2048], BF16)
    h2T = sb([128, 16, 128], BF16); h2fT = sb([128, 16, 128], F32)
    st = sb([128, 24], F32); mv = sb([128, 2], F32); rstd = sb([128, 1], F32)
    lg = sb([128, 36], F32)
    sm = {k: sb([128, n], F32) for k, n in (("m1", 1), ("nm1", 1), ("ohg", 4), ("e1", 4), ("s1", 1), ("pg", 1), ("t48", 32),
                                           ("sel", 8), ("m2a", 1), ("nm2a", 1), ("oh1", 8), ("sel2", 8), ("m2b", 1), ("oh2", 8),
                                           ("r", 1), ("den", 1), ("w1", 1), ("w2", 1), ("wsel", 8))}
    pT = PS["T"]; py = PS["o"]; pz = PS["z"]; pm = PS["misc"]

    def load(t):
        m.dma(ot[t % 2][:], o[t * 128:(t + 1) * 128, :], writes=[ot[t % 2]])
        m.dma(xt[t % 2][:], x[t * 128:(t + 1) * 128, :], writes=[xt[t % 2]])
    load(0)
    for t in range(16):
        if t + 1 < 16:
            load(t + 1)
        ob = ot[t % 2]; xx = xt[t % 2]
        for half in range(2):
            for j in range(8):
                kk = half * 8 + j
                m.op("pe", lambda e: e.transpose(pT[:, j, :], ob[:, kk * 128:(kk + 1) * 128], identb[:]),
                     reads=[ob, identb], writes=[pT], pe_accum=True)
            m.op("act", lambda e: e.copy(oT[:, half * 8:(half + 1) * 8, :], pT[:]), reads=[pT], writes=[oT])
        for n in range(4):
            p = py[n]
            for k in range(16):
                m.op("pe", lambda e: e.matmul(p[:], oT[:, k, :], wob[:, k, n * 512:(n + 1) * 512], start=(k == 0), stop=(k == 15)),
                     reads=[oT, wob], writes=[p], pe_accum=True)
            m.op("dve", lambda e: e.tensor_tensor(tmp[:, n * 512:(n + 1) * 512], p[:], bc["gt1"][:, n * 512:(n + 1) * 512], ALU.mult),
                 reads=[p, bc["gt1"]], writes=[tmp])
        m.op("dve", lambda e: e.scalar_tensor_tensor(tmp[:], xx[:], ALPHA, tmp[:], ALU.mult, ALU.add), reads=[xx, tmp], writes=[tmp])
        ln_affine(m, tmp, st, mv, rstd, bc["l1g"], bc["l1b"], x1)
        m.dma(x1_d[t * 128:(t + 1) * 128, :], x1[:], reads=[x1], writes=[x1_d])
        ln_stats(m, x1, st, mv, rstd)
        m.op("dve", lambda e: e.tensor_scalar(h2f[:], x1[:], mv[:, 0:1], rstd[:, 0:1], ALU.subtract, ALU.mult),
             reads=[x1, mv, rstd], writes=[h2f])
        m.op("pool", lambda e: e.tensor_tensor(h2f[:], h2f[:], bc["sc2"][:], ALU.mult), reads=[h2f, bc["sc2"]], writes=[h2f])
        m.op("dve", lambda e: e.tensor_tensor(h2f[:], h2f[:], bc["sh2"][:], ALU.add), reads=[h2f, bc["sh2"]], writes=[h2f])
        m.op("act", lambda e: e.copy(h2b[:], h2f[:]), reads=[h2f], writes=[h2b])
        for half in range(2):
            for j in range(8):
                kk = half * 8 + j
                m.op("pe", lambda e: e.transpose(pT[:, j, :], h2b[:, kk * 128:(kk + 1) * 128], identb[:]),
                     reads=[h2b, identb], writes=[pT], pe_accum=True)
            m.op("act", lambda e: e.copy(h2T[:, half * 8:(half + 1) * 8, :], pT[:]), reads=[pT], writes=[h2T])
        m.dma(h2T_d[:, :, t * 128:(t + 1) * 128], h2T[:], reads=[h2T], writes=[h2T_d])
        for q4 in range(4):
            p = pz[q4 % 2]
            for j in range(4):
                kk = q4 * 4 + j
                m.op("pe", lambda e: e.transpose(p[:, j * 128:(j + 1) * 128], h2f[:, kk * 128:(kk + 1) * 128], identf[:]),
                     reads=[h2f, identf], writes=[p], pe_accum=True)
            m.op("act", lambda e: e.copy(h2fT[:, q4 * 4:(q4 + 1) * 4, :].rearrange("p a b -> p (a b)"), p[:]), reads=[p], writes=[h2fT])
        for k in range(16):
            m.op("pe", lambda e: e.matmul(pm[:, 0:36], h2fT[:, k, :], rwt[:, k, :], start=(k == 0), stop=(k == 15)),
                 reads=[h2fT, rwt], writes=[pm], pe_accum=True)
        m.op("dve", lambda e: e.tensor_tensor(lg[:], pm[:, 0:36], rbt[:], ALU.add), reads=[pm, rbt], writes=[lg])
        S = sm
        def dv(fn, r, w):
            m.op("dve", fn, reads=r, writes=w)
        dv(lambda e: e.reduce_max(S["m1"][:], lg[:, 0:4], AX.X), [lg], [S["m1"]])
        dv(lambda e: e.tensor_scalar(S["ohg"][:], lg[:, 0:4], S["m1"][:, 0:1], None, ALU.is_equal), [lg, S["m1"]], [S["ohg"]])
        dv(lambda e: e.tensor_scalar_mul(S["nm1"][:], S["m1"][:], -1.0), [S["m1"]], [S["nm1"]])
        m.op("act", lambda e: e.activation(S["e1"][:], lg[:, 0:4], AF.Exp, bias=S["nm1"][:, 0:1], accum_out=S["s1"][:, 0:1]),
             reads=[lg, S["nm1"]], writes=[S["e1"], S["s1"]])
        dv(lambda e: e.reciprocal(S["pg"][:], S["s1"][:]), [S["s1"]], [S["pg"]])
        l2 = lg[:, 4:36].rearrange("p (g e) -> p g e", g=4)
        t48 = S["t48"][:].rearrange("p (g e) -> p g e", g=4)
        dv(lambda e: e.tensor_tensor(t48, l2, S["ohg"][:].unsqueeze(2).to_broadcast([128, 4, 8]), ALU.mult), [lg, S["ohg"]], [S["t48"]])
        dv(lambda e: e.tensor_reduce(S["sel"][:], S["t48"][:].rearrange("p (g e) -> p e g", g=4), AX.X, ALU.add), [S["t48"]], [S["sel"]])
        dv(lambda e: e.reduce_max(S["m2a"][:], S["sel"][:], AX.X), [S["sel"]], [S["m2a"]])
        dv(lambda e: e.tensor_scalar(S["oh1"][:], S["sel"][:], S["m2a"][:, 0:1], None, ALU.is_equal), [S["sel"], S["m2a"]], [S["oh1"]])
        dv(lambda e: e.scalar_tensor_tensor(S["sel2"][:], S["oh1"][:], -1e30, S["sel"][:], ALU.mult, ALU.add), [S["oh1"], S["sel"]], [S["sel2"]])
        dv(lambda e: e.reduce_max(S["m2b"][:], S["sel2"][:], AX.X), [S["sel2"]], [S["m2b"]])
        dv(lambda e: e.tensor_scalar(S["oh2"][:], S["sel2"][:], S["m2b"][:, 0:1], None, ALU.is_equal), [S["sel2"], S["m2b"]], [S["oh2"]])
        dv(lambda e: e.tensor_scalar_mul(S["nm2a"][:], S["m2a"][:], -1.0), [S["m2a"]], [S["nm2a"]])
        m.op("act", lambda e: e.activation(S["r"][:], S["m2b"][:], AF.Exp, bias=S["nm2a"][:, 0:1]), reads=[S["m2b"], S["nm2a"]], writes=[S["r"]])
        dv(lambda e: e.tensor_scalar_add(S["den"][:], S["r"][:], 1.0), [S["r"]], [S["den"]])
        dv(lambda e: e.reciprocal(S["den"][:], S["den"][:]), [S["den"]], [S["den"]])
        dv(lambda e: e.tensor_tensor(S["w1"][:], S["pg"][:], S["den"][:], ALU.mult), [S["pg"], S["den"]], [S["w1"]])
        dv(lambda e: e.tensor_tensor(S["w2"][:], S["w1"][:], S["r"][:], ALU.mult), [S["w1"], S["r"]], [S["w2"]])
        dv(lambda e: e.tensor_scalar(S["wsel"][:], S["oh1"][:], S["w1"][:, 0:1], None, ALU.mult), [S["oh1"], S["w1"]], [S["wsel"]])
        dv(lambda e: e.scalar_tensor_tensor(S["wsel"][:], S["oh2"][:], S["w2"][:, 0:1], S["wsel"][:], ALU.mult, ALU.add),
           [S["oh2"], S["w2"], S["wsel"]], [S["wsel"]])
        dv(lambda e: e.tensor_tensor(Wt[:, t, :].rearrange("p (g e) -> p g e", g=4), S["ohg"][:].unsqueeze(2).to_broadcast([128, 4, 8]),
                                     S["wsel"][:].unsqueeze(1).to_broadcast([128, 4, 8]), ALU.mult), [S["ohg"], S["wsel"]], [Wt])


def emit_C2(m, nc, es, modv, ln2g, ln2b, wg, wu, wd, x1_d, h2T_d, Wt, x_out, PS, n_exp=32):
    def sb(shape, dt):
        m.nbuf += 1
        return Buf(es.enter_context(nc.sbuf_tensor("c2_%d" % m.nbuf, list(shape), dt)), "c2")
    bc = {}
    for nm, src in (("gt2", modv[3, :]), ("l2g", ln2g), ("l2b", ln2b)):
        bc[nm] = sb([128, 2048], F32)
        m.dma(bc[nm][:], src.partition_broadcast(128), writes=[bc[nm]])
    h2T = sb([128, 16, 512], BF16)
    yacc = sb([128, 4, 2048], F32)
    hidT = sb([128, 8, 512], BF16)
    gst = [sb([128, 16, 128], F32) for _ in range(2)]
    ust = [sb([128, 16, 128], F32) for _ in range(2)]
    wgb = [sb([128, 16, 128], BF16) for _ in range(2)]
    wub = [sb([128, 16, 128], BF16) for _ in range(2)]
    dst = [sb([128, 2048], F32) for _ in range(2)]
    wdb = sb([128, 8, 2048], BF16)
    sg = [sb([128, 512], F32) for _ in range(2)]
    xt = sb([128, 2048], F32); outt = sb([128, 2048], F32)
    st = sb([128, 24], F32); mv = sb([128, 2], F32); rstd = sb([128, 1], F32)
    pg = PS["z"]; pu = PS["o"][0:2]; pyb = PS["o"][2:4]
    for qt in range(4):
        m.dma(h2T[:], h2T_d[:, :, qt * 512:(qt + 1) * 512], reads=[h2T_d], writes=[h2T])
        ci = 0
        items = [(e, fc) for e in range(n_exp) for fc in range(8)]

        def load_gu(ix):
            e, fc = items[ix]
            m.dma(gst[ix % 2][:], wg[e, :, fc * 128:(fc + 1) * 128].rearrange("(k p) n -> p k n", p=128), writes=[gst[ix % 2]])
            m.dma(ust[ix % 2][:], wu[e, :, fc * 128:(fc + 1) * 128].rearrange("(k p) n -> p k n", p=128), writes=[ust[ix % 2]])
            m.dma(dst[ix % 2][:], wd[e, fc * 128:(fc + 1) * 128, :], writes=[dst[ix % 2]])
        load_gu(0)
        for ix, (e, fc) in enumerate(items):
            if ix + 1 < len(items):
                load_gu(ix + 1)
            gs = gst[ix % 2]; us = ust[ix % 2]; gb = wgb[ix % 2]; ub = wub[ix % 2]; ds = dst[ix % 2]
            m.op("act", lambda en: en.copy(gb[:], gs[:]), reads=[gs], writes=[gb])
            m.op("pool", lambda en: en.tensor_copy(ub[:], us[:]), reads=[us], writes=[ub])
            m.op("pool", lambda en: en.tensor_copy(wdb[:, fc, :], ds[:]), reads=[ds], writes=[wdb])
            a = pg[ix % 2]; b = pu[ix % 2]; s = sg[ix % 2]
            for k in range(16):
                m.op("pe", lambda en: en.matmul(a[:], gb[:, k, :], h2T[:, k, :], start=(k == 0), stop=(k == 15)),
                     reads=[gb, h2T], writes=[a], pe_accum=True)
            for k in range(16):
                m.op("pe", lambda en: en.matmul(b[:], ub[:, k, :], h2T[:, k, :], start=(k == 0), stop=(k == 15)),
                     reads=[ub, h2T], writes=[b], pe_accum=True)
            m.op("act", lambda en: en.activation(s[:], a[:], AF.Silu), reads=[a], writes=[s])
            m.op("dve", lambda en: en.tensor_tensor(hidT[:, fc, :], s[:], b[:], ALU.mult), reads=[s, b], writes=[hidT])
            if fc == 7:
                for tt in range(4):
                    tile_i = qt * 4 + tt
                    for n in range(4):
                        p = pyb[(tt * 4 + n) % 2]
                        for f2 in range(8):
                            m.op("pe", lambda en: en.matmul(p[:], hidT[:, f2, tt * 128:(tt + 1) * 128], wdb[:, f2, n * 512:(n + 1) * 512],
                                                            start=(f2 == 0), stop=(f2 == 7)),
                                 reads=[hidT, wdb], writes=[p], pe_accum=True)
                        ys = yacc[:, tt, n * 512:(n + 1) * 512]
                        if e == 0:
                            m.op("dve", lambda en: en.tensor_scalar(ys, p[:], Wt[:, tile_i, e:e + 1], None, ALU.mult),
                                 reads=[p, Wt], writes=[yacc])
                        else:
                            m.op("dve", lambda en: en.scalar_tensor_tensor(ys, p[:], Wt[:, tile_i, e:e + 1], ys, ALU.mult, ALU.add),
                                 reads=[p, Wt, yacc], writes=[yacc])
        for tt in range(4):
            tile_i = qt * 4 + tt
            m.dma(xt[:], x1_d[tile_i * 128:(tile_i + 1) * 128, :], reads=[x1_d], writes=[xt])
            m.op("pool", lambda en: en.tensor_tensor(yacc[:, tt, :], yacc[:, tt, :], bc["gt2"][:], ALU.mult), reads=[yacc, bc["gt2"]], writes=[yacc])
            m.op("dve", lambda en: en.scalar_tensor_tensor(xt[:], xt[:], ALPHA, yacc[:, tt, :], ALU.mult, ALU.add), reads=[xt, yacc], writes=[xt])
            ln_affine(m, xt, st, mv, rstd, bc["l2g"], bc["l2b"], outt)
            m.dma(x_out[tile_i * 128:(tile_i + 1) * 128, :], outt[:], reads=[outt], is_output=True)


def build_C(n_exp=32, n_alloc=32):
    from contextlib import ExitStack
    nc = bass.Bass("TRN2", target_bir_lowering=False)
    dt = lambda n, s, d, k="ExternalInput": nc.dram_tensor(n, s, d, kind=k).ap()
    x = dt("x", [2048, 2048], F32); o = dt("o", [2048, 2048], BF16); modv = dt("modv", [4, 2048], F32)
    w_out = dt("w_out", [2048, 2048], F32)
    ln1g = dt("ln1g", [2048], F32); ln1b = dt("ln1b", [2048], F32); ln2g = dt("ln2g", [2048], F32); ln2b = dt("ln2b", [2048], F32)
    rw = dt("rw", [2048, 36], F32); rb = dt("rb", [36], F32)
    wg = dt("wg", [n_alloc, 2048, 1024], F32); wu = dt("wu", [n_alloc, 2048, 1024], F32); wd = dt("wd", [n_alloc, 1024, 2048], F32)
    x_out = dt("x_out", [2048, 2048], F32, "ExternalOutput")
    m = MK(nc)
    x1_d = m.dram("x1_d", [2048, 2048], F32)
    h2T_d = m.dram("h2T_d", [128, 16, 2048], BF16)
    identf = make_ident(m, F32)
    identb = m.sb([128, 128], BF16)
    m.op("dve", lambda e: e.tensor_copy(identb[:], identf[:]), reads=[identf], writes=[identb])
    Wt = m.sb([128, 16, 32], F32)
    PS = {"z": [m.ps([128, 512], F32) for _ in range(2)],
          "o": [m.ps([128, 512], F32) for _ in range(4)],
          "T": m.ps([128, 8, 128], BF16),
          "misc": m.ps([128, 512], F32)}
    with ExitStack() as es:
        emit_C1(m, nc, es, x, o, modv, w_out, ln1g, ln1b, rw, rb, x1_d, h2T_d, Wt, identb, identf, PS)
        barrier(m)
    with ExitStack() as es:
        emit_C2(m, nc, es, modv, ln2g, ln2b, wg, wu, wd, x1_d, h2T_d, Wt, x_out, PS, n_exp)
        barrier(m)
    m.finish()
    return nc
def cc_allgather(m, nc, src_ap, dst_ap):
    barrier(m)
    if "cc" not in m.sem:
        m.sem["cc"] = nc.alloc_semaphore("ccsem"); m.cnt["cc"] = 0
    ins = nc.gpsimd.collective_compute("AllGather", ALU.bypass, replica_groups=[[0, 1], [2, 3], [4, 5], [6, 7]],
                                       ins=[src_ap.opt()], outs=[dst_ap.opt()])
    m.cnt["cc"] += 1
    ins.then_inc(m.sem["cc"], 1)
    for e in m.E:
        m.E[e].wait_ge(m.sem["cc"], m.cnt["cc"])


class GB:
    def __init__(self, nc, name, rows, cols, dt, rows_k):
        self.nk = (rows + rows_k - 1) // rows_k; self.rk = rows_k
        self.src = nc.dram_tensor(name, [self.nk * rows_k, cols], dt, kind="Internal").ap()
        self.dst = nc.dram_tensor(name + "g", [self.nk * 2 * rows_k, cols], dt, kind="Internal").ap()

    def gather(self, m, nc):
        rk = self.rk
        for k in range(self.nk):
            cc_allgather(m, nc, self.src[k * rk:(k + 1) * rk, :], self.dst[k * 2 * rk:(k + 1) * 2 * rk, :])

    def g(self, r, lo, hi):
        rk = self.rk
        k = lo // rk
        assert (hi - 1) // rk == k, (lo, hi, rk)
        base = k * 2 * rk + r * rk + (lo - k * rk)
        return self.dst[base:base + (hi - lo), :]


FM_BLOCKS_F = ([(c0, "qk", o0) for c0, o0 in zip(range(0, 512, 256), range(0, 512, 256))] +
               [(c0, "qk", o0) for c0, o0 in zip(range(1536, 3584, 256), range(512, 2560, 256))] +
               [(c0, "fag", o0) for c0, o0 in zip(range(4624, 5648, 256), range(0, 1024, 256))])
TM_BLOCKS_F = ([(c0, o0) for c0, o0 in zip(range(512, 1536, 256), range(0, 1024, 256))] +
               [(c0, o0) for c0, o0 in zip(range(3584, 4608, 256), range(1024, 2048, 256))])


def emit_A2(m, nc, es, x, sh_ap, sc_ap, w_in, qkT, fagT, vtm, ident, PS):
    def sb(shape, dt):
        m.nbuf += 1
        return Buf(es.enter_context(nc.sbuf_tensor("a_%d" % m.nbuf, list(shape), dt)), "a")
    NT = 16
    hT = sb([128, 16, 2048], BF16)
    scb = sb([128, 2048], F32); shb = sb([128, 2048], F32)
    xts = [sb([128, 2048], F32) for _ in range(2)]
    hb = [sb([128, 2048], BF16) for _ in range(2)]
    st = sb([128, 24], F32); mv = sb([128, 2], F32); rstd = sb([128, 1], F32)
    pT = PS["T"]
    m.dma(shb[:], sh_ap.partition_broadcast(128), writes=[shb])
    m.dma(scb[:], sc_ap.partition_broadcast(128), writes=[scb])
    m.op("pool", lambda e: e.tensor_scalar_add(scb[:], scb[:], 1.0), reads=[scb], writes=[scb])
    m.dma(xts[0][:], x[0:128, :], writes=[xts[0]])
    for t in range(NT):
        xt = xts[t % 2]; h = hb[t % 2]
        if t + 1 < NT:
            m.dma(xts[(t + 1) % 2][:], x[(t + 1) * 128:(t + 2) * 128, :], writes=[xts[(t + 1) % 2]])
        ln_stats(m, xt, st, mv, rstd)
        m.op("dve", lambda e: e.tensor_scalar(xt[:], xt[:], mv[:, 0:1], rstd[:, 0:1], ALU.subtract, ALU.mult),
             reads=[xt, mv, rstd], writes=[xt])
        m.op("pool", lambda e: e.tensor_tensor(xt[:], xt[:], scb[:], ALU.mult), reads=[xt, scb], writes=[xt])
        m.op("dve", lambda e: e.tensor_tensor(h[:], xt[:], shb[:], ALU.add), reads=[xt, shb], writes=[h])
        for half in range(2):
            for j in range(8):
                k = half * 8 + j
                m.op("pe", lambda e: e.transpose(pT[:, j, :], h[:, k * 128:(k + 1) * 128], ident[:]),
                     reads=[h, ident], writes=[pT], pe_accum=True)
            m.op("act", lambda e: e.copy(hT[:, half * 8:(half + 1) * 8, t * 128:(t + 1) * 128], pT[:]),
                 reads=[pT], writes=[hT])
    wst = [sb([128, 16, 256], F32) for _ in range(2)]
    wbf = [sb([128, 16, 256], BF16) for _ in range(2)]
    pm = PS["o"]
    oq = [sb([128, 2048], BF16) for _ in range(2)]
    of = [sb([128, 2048], F32) for _ in range(2)]
    ov = [sb([128, 256], BF16) for _ in range(2)]
    blocks = [("fm", c0, kind, o0) for (c0, kind, o0) in FM_BLOCKS_F] + [("tm", c0, None, o0) for (c0, o0) in TM_BLOCKS_F]
    blocks.append(("fm16", 4608, "fag", 1024))
    pi = 0; oi = 0

    def load_w(bi):
        typ, c0, kind, o0 = blocks[bi]
        cw = 16 if typ == "fm16" else 256
        m.dma(wst[bi % 2][:, :, 0:cw], w_in[:, c0:c0 + cw].rearrange("(k p) n -> p k n", p=128), writes=[wst[bi % 2]])
    load_w(0)
    for bi, (typ, c0, kind, o0) in enumerate(blocks):
        cw = 16 if typ == "fm16" else 256
        ws = wst[bi % 2]; wb = wbf[bi % 2]
        if bi + 1 < len(blocks):
            load_w(bi + 1)
        m.op("act", lambda e: e.copy(wb[:, 0:8, 0:cw], ws[:, 0:8, 0:cw]), reads=[ws], writes=[wb])
        m.op("pool", lambda e: e.tensor_copy(wb[:, 8:16, 0:cw], ws[:, 8:16, 0:cw]), reads=[ws], writes=[wb])
        if typ in ("fm", "fm16"):
            for cc in range(0, cw, 128):
                cn = min(128, cw - cc)
                o = (oq if kind == "qk" else of)[oi % 2]; oi += 1
                for tg in range(4):
                    p = pm[pi % 4]; pi += 1
                    for k in range(16):
                        m.op("pe", lambda e: e.matmul(p[0:cn, :], wb[:, k, cc:cc + cn], hT[:, k, tg * 512:(tg + 1) * 512],
                                                      start=(k == 0), stop=(k == 15)),
                             reads=[wb, hT], writes=[p], pe_accum=True)
                    if tg % 2 == 0:
                        m.op("act", lambda e: e.copy(o[0:cn, tg * 512:(tg + 1) * 512], p[0:cn, :]), reads=[p], writes=[o])
                    else:
                        m.op("dve", lambda e: e.tensor_copy(o[0:cn, tg * 512:(tg + 1) * 512], p[0:cn, :]), reads=[p], writes=[o])
                dst = qkT if kind == "qk" else fagT
                m.dma(dst[o0 + cc:o0 + cc + cn, :], o[0:cn, :], reads=[o])
        else:
            for tt in range(16):
                p = pm[pi % 4]; pi += 1
                o = ov[oi % 2]; oi += 1
                for k in range(16):
                    m.op("pe", lambda e: e.matmul(p[:, 0:256], hT[:, k, tt * 128:(tt + 1) * 128], wb[:, k, :],
                                                  start=(k == 0), stop=(k == 15)),
                         reads=[wb, hT], writes=[p], pe_accum=True)
                if tt % 2 == 0:
                    m.op("act", lambda e: e.copy(o[:], p[:, 0:256]), reads=[p], writes=[o])
                else:
                    m.op("dve", lambda e: e.tensor_copy(o[:], p[:, 0:256]), reads=[p], writes=[o])
                m.dma(vtm[tt * 128:(tt + 1) * 128, o0:o0 + 256], o[:], reads=[o])


def blend_tiles(m, sb_pool, sel, dst_fn, srcA_fn, srcB_fn, ntiles, shape, dt, post=None):
    ta, tb = (sb_pool["a"], sb_pool["b"]) if dt == BF16 else (sb_pool["a32"], sb_pool["b32"])
    np_ = shape[0]
    for i in range(ntiles):
        a = ta[i % 2]; b = tb[i % 2]
        av = a[0:np_, 0:shape[1]]; bv = b[0:np_, 0:shape[1]]
        m.dma(av, srcA_fn(i), writes=[a])
        m.dma(bv, srcB_fn(i), writes=[b])
        m.op("dve", lambda e: e.tensor_scalar(av, av, sel[0:np_, 0:1], None, ALU.mult), reads=[a, sel], writes=[a])
        m.op("dve", lambda e: e.scalar_tensor_tensor(av, bv, sel[0:np_, 1:2], av, ALU.mult, ALU.add), reads=[a, b, sel], writes=[a])
        if post is None:
            m.dma(dst_fn(i), av, reads=[a])
        else:
            post(i, a, av)


def emit_select1(m, nc, es, sel, G1, G2, G3, S, identb, jmat, PS):
    def sb(shape, dt):
        m.nbuf += 1
        return Buf(es.enter_context(nc.sbuf_tensor("s_%d" % m.nbuf, list(shape), dt)), "s")
    pool = {"a": [sb([128, 2048], BF16) for _ in range(2)], "b": [sb([128, 2048], BF16) for _ in range(2)],
            "a32": [sb([128, 2048], F32) for _ in range(2)], "b32": [sb([128, 2048], F32) for _ in range(2)]}
    for r in range(2):
        tk = slice(r * 2048, (r + 1) * 2048)
        for (dst, a0, b0, nrows) in ((S["sbqT"], 0, 256, 256), (S["fxqT"], 512, 1024, 512), (S["fxkT"], 1536, 2048, 512)):
            blend_tiles(m, pool, sel, lambda i: dst[i * 128:(i + 1) * 128, tk], lambda i: G1.g(r, a0 + i * 128, a0 + (i + 1) * 128),
                        lambda i: G1.g(r, b0 + i * 128, b0 + (i + 1) * 128), nrows // 128, [128, 2048], BF16)
        blend_tiles(m, pool, sel, lambda i: S["fxv"][r * 2048 + i * 128:r * 2048 + (i + 1) * 128, :],
                    lambda i: G2.g(r, i * 128, (i + 1) * 128)[:, 1024:1536], lambda i: G2.g(r, i * 128, (i + 1) * 128)[:, 1536:2048], 16, [128, 512], BF16)
        blend_tiles(m, pool, sel, lambda i: S["fT"][:, tk], lambda i: G3.g(r, 1024, 1032), lambda i: G3.g(r, 1032, 1040), 1, [8, 2048], F32)
        blend_tiles(m, pool, sel, lambda i: S["aT"][i * 128:(i + 1) * 128, 30 + r * 2048:30 + (r + 1) * 2048],
                    lambda i: G3.g(r, i * 128, (i + 1) * 128), lambda i: G3.g(r, 256 + i * 128, 256 + (i + 1) * 128), 2, [128, 2048], F32)
        blend_tiles(m, pool, sel, lambda i: S["gT"][i * 128:(i + 1) * 128, 30 + r * 2048:30 + (r + 1) * 2048],
                    lambda i: G3.g(r, 512 + i * 128, 512 + (i + 1) * 128), lambda i: G3.g(r, 768 + i * 128, 768 + (i + 1) * 128), 2, [128, 2048], F32)
        pk = PS["z"]; pv = PS["misc"]
        ko = [sb([64, 4, 128], BF16) for _ in range(2)]
        vo = [sb([128, 256], BF16) for _ in range(2)]

        def post_k(i, a, av):
            gt = r * 16 + i; rb = 31 - gt
            p = pk[i % 2]; o = ko[i % 2]
            for h in range(4):
                m.op("pe", lambda e: e.matmul(p[0:64, h * 128:(h + 1) * 128], a[:, h * 64:(h + 1) * 64], jmat[:], start=True, stop=True),
                     reads=[a, jmat], writes=[p], pe_accum=True)
            m.op("act", lambda e: e.copy(o[:].rearrange("p h t -> p (h t)"), p[0:64, 0:512]), reads=[p], writes=[o])
            m.dma(S["sbkTr"][:, :, rb * 128:(rb + 1) * 128].rearrange("h d t -> d h t"), o[:], reads=[o])

        def post_v(i, a, av):
            gt = r * 16 + i; rb = 31 - gt
            o = vo[i % 2]
            m.op("pe", lambda e: e.matmul(pv[:, 0:256], jmat[:], a[:, 0:256], start=True, stop=True), reads=[a, jmat], writes=[pv])
            m.op("act", lambda e: e.copy(o[:], pv[:, 0:256]), reads=[pv], writes=[o])
            m.dma(S["sbvr"][rb * 128:(rb + 1) * 128, :], o[:], reads=[o])
        blend_tiles(m, pool, sel, None, lambda i: G2.g(r, i * 128, (i + 1) * 128)[:, 0:256], lambda i: G2.g(r, i * 128, (i + 1) * 128)[:, 256:512],
                    16, [128, 256], BF16, post=post_k)
        blend_tiles(m, pool, sel, None, lambda i: G2.g(r, i * 128, (i + 1) * 128)[:, 512:768], lambda i: G2.g(r, i * 128, (i + 1) * 128)[:, 768:1024],
                    16, [128, 256], BF16, post=post_v)
    z = sb([128, 30], F32)
    m.op("pool", lambda e: e.memset(z[:], 0.0), writes=[z])
    for i in range(2):
        m.dma(S["aT"][i * 128:(i + 1) * 128, 0:30], z[:], reads=[z])
        m.dma(S["gT"][i * 128:(i + 1) * 128, 0:30], z[:], reads=[z])


def emit_select2(m, nc, es, sel, G5, ocv, So):
    def sb(shape, dt):
        m.nbuf += 1
        return Buf(es.enter_context(nc.sbuf_tensor("s2_%d" % m.nbuf, list(shape), dt)), "s2")
    pool = {"a": [sb([128, 512], BF16) for _ in range(2)], "b": [sb([128, 512], BF16) for _ in range(2)]}
    rows = lambda hf, i: (hf * 2048 + i * 128, hf * 2048 + (i + 1) * 128)
    for (src_fn, c0, cw) in ((lambda hf, i: G5.g(0, *rows(hf, i))[:, 0:256], 0, 256), (lambda hf, i: G5.g(1, *rows(hf, i))[:, 0:256], 256, 256),
                             (lambda hf, i: G5.g(0, *rows(hf, i))[:, 256:768], 512, 512), (lambda hf, i: G5.g(1, *rows(hf, i))[:, 256:768], 1024, 512),
                             (lambda hf, i: ocv[rows(hf, i)[0]:rows(hf, i)[1], :], 1536, 512)):
        blend_tiles(m, pool, sel, lambda i: So[i * 128:(i + 1) * 128, c0:c0 + cw], lambda i: src_fn(0, i), lambda i: src_fn(1, i),
                    16, [128, cw], BF16)


def emit_conv2(m, nc, es, aT, gT, cwT, cb, G4):
    def sb(shape, dt):
        m.nbuf += 1
        return Buf(es.enter_context(nc.sbuf_tensor("cv%d" % m.nbuf, list(shape), dt)), "cv")
    a = sb([128, 4126], F32); g = sb([128, 4126], F32); acc = sb([128, 4096], F32)
    cw = sb([128, 2, 31], F32); cbt = sb([128, 2], F32)
    m.dma(cw[:], cwT, writes=[cw])
    m.dma(cbt[:], cb, writes=[cbt])
    for cc in range(2):
        m.dma(a[:], aT[cc * 128:(cc + 1) * 128, :], writes=[a])
        m.dma(g[:], gT[cc * 128:(cc + 1) * 128, :], writes=[g])
        m.op("act", lambda e: e.activation(g[:], g[:], AF.Sigmoid), reads=[g], writes=[g])
        m.op("dve", lambda e: e.tensor_tensor(a[:], a[:], g[:], ALU.mult), reads=[a, g], writes=[a])
        m.op("dve", lambda e: e.tensor_scalar(acc[:], a[:, 0:4096], cw[:, cc, 0:1], cbt[:, cc:cc + 1], ALU.mult, ALU.add),
             reads=[a, cw, cbt], writes=[acc])
        for w in range(1, 31):
            m.op("dve", lambda e: e.scalar_tensor_tensor(acc[:], a[:, w:w + 4096], cw[:, cc, w:w + 1], acc[:], ALU.mult, ALU.add),
                 reads=[a, cw, acc], writes=[acc])
        m.dma(G4[cc * 128:(cc + 1) * 128, :], acc[:], reads=[acc])


def emit_convln(m, nc, es, G4, lng, lnb, o_cv, identf, PS):
    def sb(shape, dt):
        m.nbuf += 1
        return Buf(es.enter_context(nc.sbuf_tensor("cl%d" % m.nbuf, list(shape), dt)), "cl")
    cvT = sb([128, 4, 4096], F32)
    lg = sb([128, 512], F32); lb = sb([128, 512], F32)
    m.dma(lg[:], lng.partition_broadcast(128), writes=[lg])
    m.dma(lb[:], lnb.partition_broadcast(128), writes=[lb])
    for cc in range(4):
        m.dma(cvT[:, cc, :], G4.g(cc // 2, (cc % 2) * 128, (cc % 2 + 1) * 128), writes=[cvT])
    pt = PS["z"]
    ut = [sb([128, 512], F32) for _ in range(2)]
    ob = [sb([128, 512], BF16) for _ in range(2)]
    st = sb([128, 6], F32); mv = sb([128, 2], F32); rstd = sb([128, 1], F32)
    for t in range(32):
        p = pt[t % 2]; u = ut[t % 2]; o = ob[t % 2]
        for cc in range(4):
            m.op("pe", lambda e: e.transpose(p[:, cc * 128:(cc + 1) * 128], cvT[:, cc, t * 128:(t + 1) * 128], identf[:]),
                 reads=[cvT, identf], writes=[p], pe_accum=True)
        m.op("act", lambda e: e.copy(u[:], p[:]), reads=[p], writes=[u])
        m.op("dve", lambda e: e.bn_stats(st[:], u[:]), reads=[u], writes=[st])
        m.op("dve", lambda e: e.bn_aggr(mv[:], st[:]), reads=[st], writes=[mv])
        m.op("dve", lambda e: e.tensor_scalar_add(rstd[:], mv[:, 1:2], LN_EPS), reads=[mv], writes=[rstd])
        m.op("act", lambda e: e.activation(rstd[:], rstd[:], AF.Sqrt), reads=[rstd], writes=[rstd])
        m.op("dve", lambda e: e.reciprocal(rstd[:], rstd[:]), reads=[rstd], writes=[rstd])
        m.op("dve", lambda e: e.tensor_scalar(u[:], u[:], mv[:, 0:1], rstd[:, 0:1], ALU.subtract, ALU.mult),
             reads=[u, mv, rstd], writes=[u])
        m.op("pool", lambda e: e.tensor_tensor(u[:], u[:], lg[:], ALU.mult), reads=[u, lg], writes=[u])
        m.op("dve", lambda e: e.tensor_tensor(u[:], u[:], lb[:], ALU.add), reads=[u, lb], writes=[u])
        m.op("act", lambda e: e.activation(o[:], u[:], AF.Silu), reads=[u], writes=[o])
        m.dma(o_cv[t * 128:(t + 1) * 128, :], o[:], reads=[o])


def emit_mod(m, nc, es, cT, aw, ab, Gm, PS):
    def sb(shape, dt):
        m.nbuf += 1
        return Buf(es.enter_context(nc.sbuf_tensor("mo%d" % m.nbuf, list(shape), dt)), "mo")
    ct = sb([128, 16], F32); sc = sb([128, 16], F32)
    wst = [sb([128, 16, 512], F32) for _ in range(2)]
    bt = [sb([1, 512], F32) for _ in range(2)]
    ot = [sb([1, 512], F32) for _ in range(2)]
    pp = PS["z"]
    m.dma(ct[:], cT, writes=[ct])
    m.op("act", lambda e: e.activation(sc[:], ct[:], AF.Silu), reads=[ct], writes=[sc])
    items = [(l, n) for l in range(2) for n in range(12)]

    def load(i):
        l, n = items[i]
        m.dma(wst[i % 2][:], aw[l, :, n * 512:(n + 1) * 512].rearrange("(k p) n -> p k n", p=128), writes=[wst[i % 2]])
        m.dma(bt[i % 2][:], ab[l:l + 1, n * 512:(n + 1) * 512], writes=[bt[i % 2]])
    load(0)
    for i, (l, n) in enumerate(items):
        if i + 1 < len(items):
            load(i + 1)
        w = wst[i % 2]; b = bt[i % 2]; o = ot[i % 2]; p = pp[i % 2]
        for k in range(16):
            m.op("pe", lambda e: e.matmul(p[0:1, :], sc[:, k:k + 1], w[:, k, :], start=(k == 0), stop=(k == 15)),
                 reads=[sc, w], writes=[p], pe_accum=True)
        m.op("dve", lambda e: e.tensor_tensor(o[:], p[0:1, :], b[:], ALU.add), reads=[p, b], writes=[o])
        m.dma(Gm[l:l + 1, n * 512:(n + 1) * 512], o[:], reads=[o])


def build_F(n_exp=32, depth=DEPTH, n_alloc=32):
    from contextlib import ExitStack
    nc = bass.Bass("TRN2", target_bir_lowering=False)
    dt = lambda n, s, d, k="ExternalInput": nc.dram_tensor(n, s, d, kind=k).ap()
    x_in = dt("x", [2048, 2048], F32); cT = dt("cT", [128, 16], F32); sel_d = dt("sel", [128, 2], F32)
    aw = dt("aw", [2, 2048, 6144], F32); ab = dt("ab", [2, 6144], F32)
    w_in = dt("w_in", [2, 2048, MIX_COLS], F32); w_out = dt("w_out", [2, 2048, 2048], F32)
    bfg = dt("bfg", [2, 8, 1], F32); cwT = dt("cwT", [2, 128, 2, 31], F32); cb = dt("cb", [2, 128, 2], F32)
    lng = dt("lng", [2, 512], F32); lnb = dt("lnb", [2, 512], F32)
    ln1g = dt("ln1g", [2, 2048], F32); ln1b = dt("ln1b", [2, 2048], F32); ln2g = dt("ln2g", [2, 2048], F32); ln2b = dt("ln2b", [2, 2048], F32)
    rw = dt("rw", [2, 2048, 36], F32); rb = dt("rb", [2, 36], F32)
    wg = dt("wg", [2, n_alloc, 2048, 1024], F32); wu = dt("wu", [2, n_alloc, 2048, 1024], F32); wd = dt("wd", [2, n_alloc, 1024, 2048], F32)
    x_out = dt("x_out", [2048, 2048], F32, "ExternalOutput")
    m = MK(nc)
    D = lambda n, s, d: nc.dram_tensor(n, list(s), d, kind="Internal").ap()
    Gm = GB(nc, "Gm", 2, 6144, F32, 2)
    G1 = GB(nc, "G1", 2560, 2048, BF16, 512); G2 = GB(nc, "G2", 2048, 2048, BF16, 512)
    G3 = GB(nc, "G3", 1040, 2048, F32, 256); G4 = GB(nc, "G4", 256, 4096, F32, 128); G5 = GB(nc, "G5", 4096, 768, BF16, 1024)
    S = {"sbqT": D("S_sbqT", [256, 4096], BF16), "fxqT": D("S_fxqT", [512, 4096], BF16), "fxkT": D("S_fxkT", [512, 4096], BF16),
         "sbkTr": D("S_sbkTr", [4, 64, 4096], BF16), "sbvr": D("S_sbvr", [4096, 256], BF16), "fxv": D("S_fxv", [4096, 512], BF16),
         "fT": D("S_fT", [8, 4096], F32), "aT": D("S_aT", [256, 4126], F32), "gT": D("S_gT", [256, 4126], F32)}
    ocv = D("ocv", [4096, 512], BF16); So = D("So", [2048, 2048], BF16)
    x_mid = D("x_mid", [2048, 2048], F32)
    x1_d = m.dram("x1_d", [2048, 2048], F32)
    h2T_d = m.dram("h2T_d", [128, 16, 2048], BF16)
    identf = make_ident(m, F32)
    identb = m.sb([128, 128], BF16)
    m.op("dve", lambda e: e.tensor_copy(identb[:], identf[:]), reads=[identf], writes=[identb])
    jf = m.sb([128, 128], F32); jmat = m.sb([128, 128], BF16)
    m.op("pool", lambda e: e.memset(jf[:], 1.0), writes=[jf])
    m.op("pool", lambda e: e.affine_select(out=jf[:], in_=jf[:], pattern=[[1, 128]], compare_op=ALU.is_equal, fill=0.0,
                                           base=-127, channel_multiplier=1), reads=[jf], writes=[jf])
    m.op("dve", lambda e: e.tensor_copy(jmat[:], jf[:]), reads=[jf], writes=[jmat])
    sel = m.sb([128, 2], F32)
    m.dma(sel[:], sel_d, writes=[sel])
    Wt = m.sb([128, 16, 32], F32)
    PS = {"z": [m.ps([128, 512], F32) for _ in range(2)],
          "o": [m.ps([128, 512], F32) for _ in range(4)],
          "T": m.ps([128, 8, 128], BF16),
          "misc": m.ps([128, 512], F32)}
    with ExitStack() as es:
        emit_mod(m, nc, es, cT, aw, ab, Gm.src, PS)
    Gm.gather(m, nc)
    modsec = lambda l, s: Gm.dst[2 * (s // 3) + l, (s % 3) * 2048:(s % 3 + 1) * 2048]
    for l in range(depth):
        xin = x_in if l == 0 else x_mid
        xo = x_out if l == depth - 1 else x_mid
        with ExitStack() as es:
            emit_A2(m, nc, es, xin, modsec(l, 0), modsec(l, 1), w_in[l], G1.src, G3.src, G2.src, identb, PS)
        G1.gather(m, nc); G2.gather(m, nc); G3.gather(m, nc)
        with ExitStack() as es:
            emit_select1(m, nc, es, sel, G1, G2, G3, S, identb, jmat, PS)
            barrier(m)
        with ExitStack() as es:
            emit_conv2(m, nc, es, S["aT"], S["gT"], cwT[l], cb[l], G4.src)
        G4.gather(m, nc)
        with ExitStack() as es:
            emit_convln(m, nc, es, G4, lng[l], lnb[l], ocv, identf, PS)
            barrier(m)
        with ExitStack() as es:
            emit_sb(m, nc, es, S["sbqT"].rearrange("(h d) t -> h d t", d=64), S["sbkTr"], S["sbvr"], G5.src[:, 0:256], identb, PSB_fix(PS), 4)
            barrier(m)
        with ExitStack() as es:
            emit_fox(m, nc, es, S["fxqT"].rearrange("(h d) t -> h d t", d=64), S["fxkT"].rearrange("(h d) t -> h d t", d=64),
                     S["fxv"], S["fT"], bfg[l], G5.src[:, 256:768], identf, PS, 8)
        G5.gather(m, nc)
        with ExitStack() as es:
            emit_select2(m, nc, es, sel, G5, ocv, So)
            barrier(m)
        modv4 = [modsec(l, 2), modsec(l, 3), modsec(l, 4), modsec(l, 5)]
        with ExitStack() as es:
            emit_C1(m, nc, es, xin, So, ModV(modv4), w_out[l], ln1g[l], ln1b[l], rw[l], rb[l], x1_d, h2T_d, Wt, identb, identf, PS)
            barrier(m)
        with ExitStack() as es:
            emit_C2(m, nc, es, ModV(modv4), ln2g[l], ln2b[l], wg[l], wu[l], wd[l], x1_d, h2T_d, Wt, xo, PS, n_exp)
            barrier(m)
    m.finish()
    return nc


class ModV:
    def __init__(self, aps):
        self.aps = aps

    def __getitem__(self, idx):
        return self.aps[idx[0]]


def PSB_fix(PS):
    d = dict(PS)
    d["T"] = [PS["T"], PS["T"]]
    return d
import ml_dtypes
from concourse.bass_utils import run_bass_kernel_spmd

_PROGS = {}


def _fused_in_maps(x, c, ada_w, ada_b, w_in, b_forget, conv_w, conv_b, conv_ln_g, conv_ln_b,
                   w_out, ln1_g, ln1_b, r1_w, r1_b, r2_w, r2_b, w_gate, w_up, w_down, ln2_g, ln2_b, n_alloc=32):
    f = lambda a: np.ascontiguousarray(np.asarray(a, dtype=np.float32))
    A = lambda a: np.asarray(a)
    rw = f(np.concatenate([A(r1_w), A(r2_w).transpose(0, 2, 1, 3).reshape(2, 2048, 32)], axis=2))
    rb = f(np.concatenate([A(r1_b), A(r2_b).reshape(2, 32)], axis=1))
    shared = {"w_in": f(w_in), "w_out": f(w_out), "lng": f(conv_ln_g), "lnb": f(conv_ln_b), "ln1g": f(ln1_g), "ln1b": f(ln1_b),
              "ln2g": f(ln2_g), "ln2b": f(ln2_b), "rw": rw, "rb": rb,
              "wg": f(A(w_gate)[:, :n_alloc]), "wu": f(A(w_up)[:, :n_alloc]), "wd": f(A(w_down)[:, :n_alloc])}
    maps = []
    for i in range(8):
        b, j = divmod(i, 2)
        sel = np.zeros((128, 2), np.float32); sel[:, j] = 1.0
        d = dict(shared)
        d["x"] = f(A(x)[b, j * 2048:(j + 1) * 2048])
        d["cT"] = f(A(c)[b].reshape(16, 128).T)
        d["sel"] = sel
        d["aw"] = f(A(ada_w)[:, :, j * 6144:(j + 1) * 6144]); d["ab"] = f(A(ada_b)[:, j * 6144:(j + 1) * 6144])
        d["bfg"] = f(A(b_forget)[:, 8 * j:8 * j + 8].reshape(2, 8, 1))
        d["cwT"] = f(A(conv_w)[:, :, j * 256:(j + 1) * 256].transpose(0, 2, 1).reshape(2, 2, 128, 31).transpose(0, 2, 1, 3))
        d["cb"] = f(A(conv_b)[:, j * 256:(j + 1) * 256].reshape(2, 2, 128).transpose(0, 2, 1))
        maps.append(d)
    return maps


def kernel(**inputs):
    if "F" not in _PROGS:
        _PROGS["F"] = build_F()
    maps = _fused_in_maps(**inputs)
    res = run_bass_kernel_spmd(_PROGS["F"], maps, core_ids=list(range(8)))
    xs = [np.asarray(r["x_out"]) for r in res.results]
    out = np.stack([np.concatenate([xs[2 * b], xs[2 * b + 1]], axis=0) for b in range(4)], axis=0)
    return out.astype(np.float32)
```

```python
D_MODEL = 2048; BATCH = 4; SEQ = 4096; DEPTH = 2
MIX_COLS = 5648
LN_EPS = 1e-5
ALPHA = (2 * DEPTH) ** 0.25
import numpy as np
import concourse.bass as bass
import concourse.mybir as mybir

F32 = mybir.dt.float32
BF16 = mybir.dt.bfloat16
I32 = mybir.dt.int32
ALU = mybir.AluOpType
AF = mybir.ActivationFunctionType
AX = mybir.AxisListType


class Buf:
    __slots__ = ("t", "name", "w", "r")

    def __init__(self, t, name):
        self.t = t
        self.name = name
        self.w = None
        self.r = []

    def __getitem__(self, idx):
        return self.t[idx]


class MK:
    def __init__(self, nc, n_dma_sems=24):
        self.nc = nc
        self.E = {"pe": nc.tensor, "act": nc.scalar, "dve": nc.vector,
                  "pool": nc.gpsimd, "sp": nc.sync}
        self.sem = {k: nc.alloc_semaphore("c_" + k) for k in self.E}
        self.cnt = {k: 0 for k in self.E}
        self.dsem = [nc.alloc_semaphore("d%d" % i) for i in range(n_dma_sems)]
        self.dcnt = [0] * n_dma_sems
        self.dnext = 0
        self.seen = {k: {} for k in self.E}
        self.nbuf = 0
        self.out_tokens = []

    def sb(self, shape, dtype, name=None):
        self.nbuf += 1
        name = name or "b%d" % self.nbuf
        return Buf(self.nc.alloc_sbuf_tensor(name, list(shape), dtype), name)

    def ps(self, shape, dtype, name=None):
        self.nbuf += 1
        name = name or "p%d" % self.nbuf
        return Buf(self.nc.alloc_psum_tensor(name, list(shape), dtype), name)

    def dram(self, name, shape, dtype, kind="Internal"):
        t = self.nc.dram_tensor(name, list(shape), dtype, kind=kind)
        return Buf(t.ap(), name)

    def _semobj(self, key):
        return self.sem[key] if isinstance(key, str) else self.dsem[key]

    def _wait(self, eng, tok):
        if tok is None:
            return
        key, val, _ = tok
        if self.seen[eng].get(key, 0) >= val:
            return
        self.E[eng].wait_ge(self._semobj(key), val)
        self.seen[eng][key] = val

    def _deps(self, eng, reads, writes, pe_accum=False):
        for b in reads:
            self._wait(eng, b.w)
        for b in writes:
            if not (pe_accum and b.w is not None and b.w[2] == "pe" and eng == "pe"):
                self._wait(eng, b.w)
            for tok in b.r:
                self._wait(eng, tok)

    def _commit(self, tok, reads, writes):
        for b in reads:
            b.r.append(tok)
            if len(b.r) > 6:
                d = {}
                for t in b.r:
                    if t[0] not in d or d[t[0]][1] < t[1]:
                        d[t[0]] = t
                b.r = list(d.values())
        for b in writes:
            b.w = tok
            b.r = []

    def op(self, eng, fn, reads=(), writes=(), pe_accum=False):
        self._deps(eng, reads, writes, pe_accum)
        ins = fn(self.E[eng])
        self.cnt[eng] += 1
        ins.then_inc(self.sem[eng], 1)
        tok = (eng, self.cnt[eng], eng)
        self._commit(tok, reads, writes)
        return tok

    def dma(self, out, in_, reads=(), writes=(), eng="sp", is_output=False, **kw):
        i = self.dnext
        self.dnext = (self.dnext + 1) % len(self.dsem)
        if self.dcnt[i] > 0:
            self._wait(eng, (i, self.dcnt[i], "dma"))
        self._deps(eng, reads, writes)
        ins = self.E[eng].dma_start(out=out, in_=in_, **kw)
        self.dcnt[i] += 16
        ins.then_inc(self.dsem[i], 16)
        tok = (i, self.dcnt[i], "dma")
        self._commit(tok, reads, writes)
        if is_output:
            self.out_tokens.append(tok)
        return tok

    def finish(self, eng="sp"):
        for tok in self.out_tokens:
            key, val, _ = tok
            self.E[eng].wait_ge(self._semobj(key), val)
        for i, c in enumerate(self.dcnt):
            if c > 0:
                self.E[eng].wait_ge(self.dsem[i], c)
def build_M():
    nc = bass.Bass("TRN2", target_bir_lowering=False)
    cT = nc.dram_tensor("cT", [128, 16, 4], F32, kind="ExternalInput").ap()
    aw = nc.dram_tensor("aw", [2, 2048, 1536], F32, kind="ExternalInput").ap()
    ab = nc.dram_tensor("ab", [2, 1536], F32, kind="ExternalInput").ap()
    mo = nc.dram_tensor("mo", [2, 4, 1536], F32, kind="ExternalOutput").ap()
    m = MK(nc)
    ct = m.sb([128, 16, 4], F32)
    sc = m.sb([128, 16, 4], F32)
    wst = [m.sb([128, 16, 512], F32) for _ in range(2)]
    bt = [m.sb([4, 512], F32) for _ in range(2)]
    ot = [m.sb([4, 512], F32) for _ in range(2)]
    pp = [m.ps([4, 512], F32) for _ in range(2)]
    m.dma(ct[:], cT, writes=[ct])
    m.op("act", lambda e: e.activation(sc[:], ct[:], AF.Silu), reads=[ct], writes=[sc])
    i = 0
    for l in range(2):
        for n in range(3):
            w = wst[i % 2]; b = bt[i % 2]; o = ot[i % 2]; p = pp[i % 2]
            m.dma(w[:], aw[l, :, n * 512:(n + 1) * 512].rearrange("(k p) n -> p k n", p=128), writes=[w])
            m.dma(b[:], ab[l, n * 512:(n + 1) * 512].partition_broadcast(4), writes=[b])
            for k in range(16):
                m.op("pe", lambda e: e.matmul(p[:], sc[:, k, :], w[:, k, :], start=(k == 0), stop=(k == 15)),
                     reads=[sc, w], writes=[p], pe_accum=True)
            m.op("dve", lambda e: e.tensor_tensor(o[:], p[:], b[:], ALU.add), reads=[p, b], writes=[o])
            m.dma(mo[l, :, n * 512:(n + 1) * 512], o[:], reads=[o], is_output=True)
            i += 1
    m.finish()
    return nc


def make_ident(m, dtype):
    idf = m.sb([128, 128], F32)
    m.op("pool", lambda e: e.memset(idf[:], 1.0), writes=[idf])
    m.op("pool", lambda e: e.affine_select(out=idf[:], in_=idf[:], pattern=[[1, 128]], compare_op=ALU.is_equal,
                                           fill=0.0, base=0, channel_multiplier=-1), reads=[idf], writes=[idf])
    if dtype == F32:
        return idf
    idb = m.sb([128, 128], dtype)
    m.op("dve", lambda e: e.tensor_copy(idb[:], idf[:]), reads=[idf], writes=[idb])
    return idb


def ln_stats(m, xt, st, mv, rstd):
    for q in range(4):
        m.op("dve", lambda e: e.bn_stats(st[:, q * 6:(q + 1) * 6], xt[:, q * 512:(q + 1) * 512]), reads=[xt], writes=[st])
    m.op("dve", lambda e: e.bn_aggr(mv[:], st[:]), reads=[st], writes=[mv])
    m.op("dve", lambda e: e.tensor_scalar_add(rstd[:], mv[:, 1:2], LN_EPS), reads=[mv], writes=[rstd])
    m.op("act", lambda e: e.activation(rstd[:], rstd[:], AF.Sqrt), reads=[rstd], writes=[rstd])
    m.op("dve", lambda e: e.reciprocal(rstd[:], rstd[:]), reads=[rstd], writes=[rstd])


FM_BLOCKS = [(0, "qk", 0), (256, "qk", 256), (512, "qk", 512), (768, "qk", 768),
             (1536, "qk", 1024), (1792, "qk", 1280), (2048, "qk", 1536), (2304, "qk", 1792),
             (2560, "qk", 2048), (2816, "qk", 2304), (3072, "qk", 2560), (3328, "qk", 2816),
             (4608, "fag", 0), (4864, "fag", 256), (5120, "fag", 512), (5376, "fag", 768)]
TM_BLOCKS = [(1024, 0), (1280, 256), (3584, 512), (3840, 768), (4096, 1024), (4352, 1280)]


def emit_A(m, nc, x, modv, w_in, qkT, fagT, vtm, ident):
    NT = 16
    hT = m.sb([128, 16, 2048], BF16, "hT")
    scb = m.sb([128, 2048], F32, "scb")
    shb = m.sb([128, 2048], F32, "shb")
    xts = [m.sb([128, 2048], F32) for _ in range(2)]
    hb = [m.sb([128, 2048], BF16) for _ in range(2)]
    st = m.sb([128, 24], F32); mv = m.sb([128, 2], F32); rstd = m.sb([128, 1], F32)
    pT = [m.ps([128, 8, 128], BF16) for _ in range(2)]
    m.dma(shb[:], modv[0, :].partition_broadcast(128), writes=[shb])
    m.dma(scb[:], modv[1, :].partition_broadcast(128), writes=[scb])
    m.op("pool", lambda e: e.tensor_scalar_add(scb[:], scb[:], 1.0), reads=[scb], writes=[scb])
    m.dma(xts[0][:], x[0:128, :], writes=[xts[0]])
    for t in range(NT):
        xt = xts[t % 2]; h = hb[t % 2]
        if t + 1 < NT:
            m.dma(xts[(t + 1) % 2][:], x[(t + 1) * 128:(t + 2) * 128, :], writes=[xts[(t + 1) % 2]])
        ln_stats(m, xt, st, mv, rstd)
        m.op("dve", lambda e: e.tensor_scalar(xt[:], xt[:], mv[:, 0:1], rstd[:, 0:1], ALU.subtract, ALU.mult),
             reads=[xt, mv, rstd], writes=[xt])
        m.op("pool", lambda e: e.tensor_tensor(xt[:], xt[:], scb[:], ALU.mult), reads=[xt, scb], writes=[xt])
        m.op("dve", lambda e: e.tensor_tensor(h[:], xt[:], shb[:], ALU.add), reads=[xt, shb], writes=[h])
        for half in range(2):
            p = pT[half]
            for j in range(8):
                k = half * 8 + j
                m.op("pe", lambda e: e.transpose(p[:, j, :], h[:, k * 128:(k + 1) * 128], ident[:]),
                     reads=[h, ident], writes=[p], pe_accum=True)
            m.op("act", lambda e: e.copy(hT[:, half * 8:(half + 1) * 8, t * 128:(t + 1) * 128], p[:]),
                 reads=[p], writes=[hT])
    wst = [m.sb([128, 16, 256], F32) for _ in range(2)]
    wbf = [m.sb([128, 16, 256], BF16) for _ in range(2)]
    pm = [m.ps([128, 512], F32) for _ in range(4)]
    oq = [m.sb([128, 2048], BF16) for _ in range(2)]
    of = [m.sb([128, 2048], F32) for _ in range(2)]
    ov = [m.sb([128, 256], BF16) for _ in range(2)]
    blocks = [("fm", c0, kind, o0) for (c0, kind, o0) in FM_BLOCKS] + [("tm", c0, None, o0) for (c0, o0) in TM_BLOCKS]
    blocks.append(("fm16", 5632, "fag", 1024))
    pi = 0; oi = 0
    def load_w(bi):
        typ, c0, kind, o0 = blocks[bi]
        cw = 16 if typ == "fm16" else 256
        m.dma(wst[bi % 2][:, :, 0:cw], w_in[:, c0:c0 + cw].rearrange("(k p) n -> p k n", p=128), writes=[wst[bi % 2]])
    load_w(0)
    for bi, (typ, c0, kind, o0) in enumerate(blocks):
        cw = 16 if typ == "fm16" else 256
        ws = wst[bi % 2]; wb = wbf[bi % 2]
        if bi + 1 < len(blocks):
            load_w(bi + 1)
        m.op("act", lambda e: e.copy(wb[:, 0:8, 0:cw], ws[:, 0:8, 0:cw]), reads=[ws], writes=[wb])
        m.op("pool", lambda e: e.tensor_copy(wb[:, 8:16, 0:cw], ws[:, 8:16, 0:cw]), reads=[ws], writes=[wb])
        if typ in ("fm", "fm16"):
            for cc in range(0, cw, 128):
                cn = min(128, cw - cc)
                o = (oq if kind == "qk" else of)[oi % 2]; oi += 1
                for tg in range(4):
                    p = pm[pi % 4]; pi += 1
                    for k in range(16):
                        m.op("pe", lambda e: e.matmul(p[0:cn, :], wb[:, k, cc:cc + cn], hT[:, k, tg * 512:(tg + 1) * 512],
                                                      start=(k == 0), stop=(k == 15)),
                             reads=[wb, hT], writes=[p], pe_accum=True)
                    if tg % 2 == 0:
                        m.op("act", lambda e: e.copy(o[0:cn, tg * 512:(tg + 1) * 512], p[0:cn, :]), reads=[p], writes=[o])
                    else:
                        m.op("dve", lambda e: e.tensor_copy(o[0:cn, tg * 512:(tg + 1) * 512], p[0:cn, :]), reads=[p], writes=[o])
                dst = qkT if kind == "qk" else fagT
                m.dma(dst[o0 + cc:o0 + cc + cn, :], o[0:cn, :], reads=[o], is_output=True)
        else:
            for tt in range(16):
                p = pm[pi % 4]; pi += 1
                o = ov[oi % 2]; oi += 1
                for k in range(16):
                    m.op("pe", lambda e: e.matmul(p[:, 0:256], hT[:, k, tt * 128:(tt + 1) * 128], wb[:, k, :],
                                                  start=(k == 0), stop=(k == 15)),
                         reads=[wb, hT], writes=[p], pe_accum=True)
                if tt % 2 == 0:
                    m.op("act", lambda e: e.copy(o[:], p[:, 0:256]), reads=[p], writes=[o])
                else:
                    m.op("dve", lambda e: e.tensor_copy(o[:], p[:, 0:256]), reads=[p], writes=[o])
                m.dma(vtm[tt * 128:(tt + 1) * 128, o0:o0 + 256], o[:], reads=[o], is_output=True)


def build_A():
    nc = bass.Bass("TRN2", target_bir_lowering=False)
    x = nc.dram_tensor("x", [2048, 2048], F32, kind="ExternalInput").ap()
    modv = nc.dram_tensor("modv", [2, 2048], F32, kind="ExternalInput").ap()
    w_in = nc.dram_tensor("w_in", [2048, MIX_COLS], F32, kind="ExternalInput").ap()
    qkT = nc.dram_tensor("qkT", [3072, 2048], BF16, kind="ExternalOutput").ap()
    fagT = nc.dram_tensor("fagT", [1040, 2048], F32, kind="ExternalOutput").ap()
    vtm = nc.dram_tensor("vtm", [2048, 1536], BF16, kind="ExternalOutput").ap()
    m = MK(nc)
    ident = make_ident(m, BF16)
    emit_A(m, nc, x, modv, w_in, qkT, fagT, vtm, ident)
    m.finish()
    return nc
def barrier(m):
    for e in m.E:
        for f in m.E:
            if m.cnt[f] > 0:
                m._wait(e, (f, m.cnt[f], f))
        for i, c in enumerate(m.dcnt):
            if c > 0:
                m._wait(e, (i, c, "dma"))


def emit_conv(m, nc, es, aT, gT, cwT, cb, lng, lnb, o_cv, identf, PS):
    def sb(shape, dt):
        m.nbuf += 1
        return Buf(es.enter_context(nc.sbuf_tensor("cv%d" % m.nbuf, list(shape), dt)), "cv")
    cvT = sb([128, 4, 2048], F32)
    at = [sb([128, 2078], F32) for _ in range(2)]
    gt = [sb([128, 2078], F32) for _ in range(2)]
    cw = sb([128, 4, 31], F32); cbt = sb([128, 4], F32)
    lg = sb([128, 512], F32); lb = sb([128, 512], F32)
    m.dma(cw[:], cwT, writes=[cw])
    m.dma(cbt[:], cb, writes=[cbt])
    m.dma(lg[:], lng.partition_broadcast(128), writes=[lg])
    m.dma(lb[:], lnb.partition_broadcast(128), writes=[lb])
    for cc in range(4):
        a = at[cc % 2]; g = gt[cc % 2]
        m.dma(a[:], aT[cc * 128:(cc + 1) * 128, :], writes=[a])
        m.dma(g[:], gT[cc * 128:(cc + 1) * 128, :], writes=[g])
        m.op("act", lambda e: e.activation(g[:], g[:], AF.Sigmoid), reads=[g], writes=[g])
        m.op("dve", lambda e: e.tensor_tensor(a[:], a[:], g[:], ALU.mult), reads=[a, g], writes=[a])
        m.op("dve", lambda e: e.tensor_scalar(cvT[:, cc, :], a[:, 0:2048], cw[:, cc, 0:1], cbt[:, cc:cc + 1], ALU.mult, ALU.add),
             reads=[a, cw, cbt], writes=[cvT])
        for w in range(1, 31):
            m.op("dve", lambda e: e.scalar_tensor_tensor(cvT[:, cc, :], a[:, w:w + 2048], cw[:, cc, w:w + 1], cvT[:, cc, :], ALU.mult, ALU.add),
                 reads=[a, cw, cvT], writes=[cvT])
    pt = PS["z"]
    ut = [sb([128, 512], F32) for _ in range(2)]
    ob = [sb([128, 512], BF16) for _ in range(2)]
    st = sb([128, 6], F32); mv = sb([128, 2], F32); rstd = sb([128, 1], F32)
    for t in range(16):
        p = pt[t % 2]; u = ut[t % 2]; o = ob[t % 2]
        for cc in range(4):
            m.op("pe", lambda e: e.transpose(p[:, cc * 128:(cc + 1) * 128], cvT[:, cc, t * 128:(t + 1) * 128], identf[:]),
                 reads=[cvT, identf], writes=[p], pe_accum=True)
        m.op("act", lambda e: e.copy(u[:], p[:]), reads=[p], writes=[u])
        m.op("dve", lambda e: e.bn_stats(st[:], u[:]), reads=[u], writes=[st])
        m.op("dve", lambda e: e.bn_aggr(mv[:], st[:]), reads=[st], writes=[mv])
        m.op("dve", lambda e: e.tensor_scalar_add(rstd[:], mv[:, 1:2], LN_EPS), reads=[mv], writes=[rstd])
        m.op("act", lambda e: e.activation(rstd[:], rstd[:], AF.Sqrt), reads=[rstd], writes=[rstd])
        m.op("dve", lambda e: e.reciprocal(rstd[:], rstd[:]), reads=[rstd], writes=[rstd])
        m.op("dve", lambda e: e.tensor_scalar(u[:], u[:], mv[:, 0:1], rstd[:, 0:1], ALU.subtract, ALU.mult),
             reads=[u, mv, rstd], writes=[u])
        m.op("pool", lambda e: e.tensor_tensor(u[:], u[:], lg[:], ALU.mult), reads=[u, lg], writes=[u])
        m.op("dve", lambda e: e.tensor_tensor(u[:], u[:], lb[:], ALU.add), reads=[u, lb], writes=[u])
        m.op("act", lambda e: e.activation(o[:], u[:], AF.Silu), reads=[u], writes=[o])
        m.dma(o_cv[t * 128:(t + 1) * 128, :], o[:], reads=[o], is_output=True)


def run_pipeline(n, stages):
    S = len(stages)
    for step in range(n + S - 1):
        for st in range(S - 1, -1, -1):
            i = step - st
            if 0 <= i < n:
                stages[st](i)


def emit_sb(m, nc, es, sbqT, sbkTr, sbvr, o_sb, identb, PS, nheads=4):
    def sb(shape, dt):
        m.nbuf += 1
        return Buf(es.enter_context(nc.sbuf_tensor("sb%d" % m.nbuf, list(shape), dt)), "sb")
    QT = [sb([64, 4096], BF16) for _ in range(2)]
    KT = [sb([64, 4096], BF16) for _ in range(2)]
    VR = [sb([128, 32, 64], BF16) for _ in range(2)]
    ones = sb([128, 512], F32)
    m.op("pool", lambda e: e.memset(ones[:], 1.0), writes=[ones])
    NB = 4
    e_t = [sb([128, 512], F32) for _ in range(NB)]
    sp_t = [sb([128, 512], F32) for _ in range(NB)]
    r_t = [sb([128, 512], F32) for _ in range(NB)]
    la_t = [sb([128, 512], F32) for _ in range(NB)]
    a_t = [sb([128, 512], BF16) for _ in range(NB)]
    aT_t = [sb([128, 4, 128], BF16) for _ in range(2)]
    ost = [sb([128, 32, 64], BF16) for _ in range(2)]
    pzs = list(PS["z"]) + [PS["misc"]]
    NZ = len(pzs)
    pT2 = [Buf(PS["T"][0].t[:, 0:4, :], "pTa"), Buf(PS["T"][0].t[:, 4:8, :], "pTb")]
    po = PS["o"][0]

    def load(h):
        m.dma(QT[h % 2][:], sbqT[h], writes=[QT[h % 2]])
        m.dma(KT[h % 2][:], sbkTr[h], writes=[KT[h % 2]])
        m.dma(VR[h % 2][:], sbvr[:, h * 64:(h + 1) * 64].rearrange("(c p) d -> p c d", p=128), writes=[VR[h % 2]])
    items = []
    for h in range(nheads):
        for i in range(32):
            nch = (i + 1 + 3) // 4
            for c in range(nch):
                items.append((h, i, c, nch))
    load(0)

    def geom(ix):
        h, i, c, nch = items[ix]
        c0 = 128 * (31 - i) + 512 * c
        w = min(512, 4096 - c0)
        return h, i, c, nch, c0, w

    def s0(ix):
        h, i, c, nch, c0, w = geom(ix)
        q = QT[h % 2]; k = KT[h % 2]
        z = pzs[ix % NZ]; et = e_t[ix % NB]; spt = sp_t[ix % NB]
        m.op("pe", lambda e: e.matmul(z[:, 0:w], q[:, i * 128:(i + 1) * 128], k[:, c0:c0 + w], start=True, stop=True),
             reads=[q, k], writes=[z])
        m.op("act", lambda e: e.activation(et[:, 0:w], z[:, 0:w], AF.Exp, scale=0.125), reads=[z], writes=[et])
        m.op("act", lambda e: e.activation(spt[:, 0:w], et[:, 0:w], AF.Ln, bias=1.0), reads=[et], writes=[spt])
        if c == 0:
            m.op("pool", lambda e: e.affine_select(out=spt[:, 0:128], in_=spt[:, 0:128], pattern=[[1, 128]],
                                                   compare_op=ALU.is_gt, fill=0.0, base=-127, channel_multiplier=1),
                 reads=[spt], writes=[spt])

    def s1(ix):
        h, i, c, nch, c0, w = geom(ix)
        z = pzs[ix % NZ]; spt = sp_t[ix % NB]; rt = r_t[ix % NB]; lat = la_t[ix % NB]
        if c == 0:
            init = 0.0; rd = [ones, spt]
        else:
            prt = r_t[(ix - 1) % NB]
            init = prt[:, 511:512]; rd = [ones, spt, prt]
        m.op("dve", lambda e: e.tensor_tensor_scan(rt[:, 0:w], ones[:, 0:w], spt[:, 0:w], init, ALU.mult, ALU.add),
             reads=rd, writes=[rt])
        m.op("dve", lambda e: e.scalar_tensor_tensor(lat[:, 0:w], z[:, 0:w], 0.125, rt[:, 0:w], ALU.mult, ALU.subtract),
             reads=[z, rt], writes=[lat])

    def s2(ix):
        h, i, c, nch, c0, w = geom(ix)
        lat = la_t[ix % NB]; at_ = a_t[ix % NB]
        m.op("act", lambda e: e.activation(at_[:, 0:w], lat[:, 0:w], AF.Exp), reads=[lat], writes=[at_])
        if c == 0:
            m.op("pool", lambda e: e.affine_select(out=at_[:, 0:128], in_=at_[:, 0:128], pattern=[[1, 128]],
                                                   compare_op=ALU.is_gt, fill=0.0, base=-127, channel_multiplier=1),
                 reads=[at_], writes=[at_])

    def s3(ix):
        h, i, c, nch, c0, w = geom(ix)
        nb = w // 128
        v = VR[h % 2]; os_ = ost[h % 2]
        at_ = a_t[ix % NB]; aTt = aT_t[ix % 2]
        pTb = pT2[ix % 2]
        if i == 0 and c == 0 and h + 1 < nheads:
            load(h + 1)
        for bb in range(nb):
            m.op("pe", lambda e: e.transpose(pTb[:, bb, :], at_[:, bb * 128:(bb + 1) * 128], identb[:]),
                 reads=[at_, identb], writes=[pTb], pe_accum=True)
        m.op("dve", lambda e: e.tensor_copy(aTt[:, 0:nb, :], pTb[:, 0:nb, :]), reads=[pTb], writes=[aTt])
        for bb in range(nb):
            kb = (c0 // 128) + bb
            first = (c == 0 and bb == 0); last = (c == nch - 1 and bb == nb - 1)
            m.op("pe", lambda e: e.matmul(po[:, 0:64], aTt[:, bb, :], v[:, kb, :], start=first, stop=last),
                 reads=[aTt, v], writes=[po], pe_accum=True)
        if c == nch - 1:
            m.op("act", lambda e: e.copy(os_[:, i, :], po[:, 0:64]), reads=[po], writes=[os_])
            if i == 31:
                m.dma(o_sb[:, h * 64:(h + 1) * 64].rearrange("(i p) d -> p i d", p=128), os_[:], reads=[os_], is_output=True)
    run_pipeline(len(items), [s0, s1, s2, s3])


def emit_fox(m, nc, es, fxqT, fxkT, fxv, fT, negb, o_fx, identf, PS, nheads=8):
    def sb(shape, dt):
        m.nbuf += 1
        return Buf(es.enter_context(nc.sbuf_tensor("fx%d" % m.nbuf, list(shape), dt)), "fx")
    NH = nheads
    C = sb([NH, 4096], F32); W1 = sb([NH, 4096], F32); nb_t = sb([NH, 1], F32)
    onesr = sb([NH, 4096], F32)
    m.dma(W1[:], fT[0:NH, :], writes=[W1])
    m.dma(nb_t[:], negb[0:NH, :], writes=[nb_t])
    m.op("pool", lambda e: e.memset(onesr[:], 1.0), writes=[onesr])
    m.op("dve", lambda e: e.tensor_scalar_mul(nb_t[:], nb_t[:], -1.0), reads=[nb_t], writes=[nb_t])
    m.op("act", lambda e: e.activation(W1[:], W1[:], AF.Exp, bias=nb_t[:, 0:1], scale=-1.0), reads=[W1, nb_t], writes=[W1])
    m.op("act", lambda e: e.activation(W1[:], W1[:], AF.Ln, bias=1.0), reads=[W1], writes=[W1])
    m.op("dve", lambda e: e.tensor_tensor_scan(C[:], onesr[:], W1[:], 0.0, ALU.mult, ALU.add), reads=[onesr, W1], writes=[C])
    CkT = sb([128, 32, NH], F32)
    pc = PS["misc"]
    for j in range(32):
        m.op("pe", lambda e: e.transpose(pc[:, j * NH:(j + 1) * NH], C[:, j * 128:(j + 1) * 128], identf[0:NH, 0:NH]),
             reads=[C, identf], writes=[pc], pe_accum=True)
    m.op("dve", lambda e: e.tensor_copy(CkT[:].rearrange("p j h -> p (j h)"), pc[:, 0:32 * NH]), reads=[pc], writes=[CkT])
    Rd = sb([NH, NH, 8], F32)
    Cg = C[:].rearrange("h (g q) -> h g q", q=512)
    m.op("dve", lambda e: e.tensor_tensor(Rd[:], Cg[:, :, 511:512].rearrange("h g o -> h o g").to_broadcast([NH, NH, 8]),
                                          identf[0:NH, 0:NH].unsqueeze(2).to_broadcast([NH, NH, 8]), ALU.mult),
         reads=[C, identf], writes=[Rd])
    onesk = sb([NH, 128], F32)
    m.op("pool", lambda e: e.memset(onesk[:], 1.0), writes=[onesk])
    m.op("pe", lambda e: e.matmul(pc[:, 256:256 + NH * 8], onesk[:], Rd[:].rearrange("h a g -> h (a g)"), start=True, stop=True),
         reads=[onesk, Rd, CkT], writes=[pc])
    Rbc = sb([128, NH, 8], F32)
    m.op("dve", lambda e: e.tensor_copy(Rbc[:].rearrange("p h g -> p (h g)"), pc[:, 256:256 + NH * 8]), reads=[pc], writes=[Rbc])
    bias = sb([128, NH, 8, 32], F32)
    for h in range(NH):
        for g in range(8):
            nj = 4 * g + 4
            m.op("dve", lambda e: e.tensor_scalar(bias[:, h, g, 0:nj], CkT[:, 0:nj, h], Rbc[:, h, g:g + 1], None, ALU.subtract),
                 reads=[CkT, Rbc], writes=[bias])
    D8 = sb([NH, 4096], F32); Dhi = sb([NH, 4096], BF16); Dlo = sb([NH, 4096], BF16)
    D8g = D8[:].rearrange("h (g q) -> h g q", q=512)
    m.op("dve", lambda e: e.tensor_tensor(D8g, Cg[:, :, 511:512].to_broadcast([NH, 8, 512]), Cg, ALU.subtract),
         reads=[C], writes=[D8])
    m.op("dve", lambda e: e.tensor_scalar_mul(D8[:], D8[:], 8.0), reads=[D8], writes=[D8])
    m.op("dve", lambda e: e.tensor_copy(Dhi[:], D8[:]), reads=[D8], writes=[Dhi])
    m.op("dve", lambda e: e.tensor_tensor(D8[:], D8[:], Dhi[:], ALU.subtract), reads=[D8, Dhi], writes=[D8])
    m.op("dve", lambda e: e.tensor_copy(Dlo[:], D8[:]), reads=[D8], writes=[Dlo])
    QT = [sb([66, 4096], BF16) for _ in range(2)]
    KT = [sb([66, 4096], BF16) for _ in range(2)]
    V = [sb([128, 32, 65], BF16) for _ in range(2)]
    for i in range(2):
        m.op("pool", lambda e: e.memset(KT[i][64:66, :], 1.0), writes=[KT[i]])
        m.op("pool", lambda e: e.memset(V[i][:, :, 64:65], 1.0), writes=[V[i]])
    NP = 4
    P_t = [sb([128, 512], BF16) for _ in range(NP)]
    ost = [sb([128, 32, 64], BF16) for _ in range(2)]
    rc = sb([128, 4], F32)
    pzs = list(PS["z"]) + [PS["misc"]]
    NZ = len(pzs)
    po = PS["o"]

    def load(h):
        m.dma(QT[h % 2][0:64, :], fxqT[h], writes=[QT[h % 2]])
        m.dma(QT[h % 2][64:65, :], Dhi[h:h + 1, :], reads=[Dhi], writes=[QT[h % 2]])
        m.dma(QT[h % 2][65:66, :], Dlo[h:h + 1, :], reads=[Dlo], writes=[QT[h % 2]])
        m.dma(KT[h % 2][0:64, :], fxkT[h], writes=[KT[h % 2]])
        m.dma(V[h % 2][:, :, 0:64], fxv[:, h * 64:(h + 1) * 64].rearrange("(c p) d -> p c d", p=128), writes=[V[h % 2]])
    load(0)
    items = [(h, g, j) for h in range(NH) for g in range(8) for j in range(4 * g + 4)]

    def geom(ix):
        h, g, j = items[ix]
        md = j - 4 * g
        q0 = 128 * max(0, md)
        return h, g, j, md, q0, 512 - q0

    def s0(ix):
        h, g, j, md, q0, w = geom(ix)
        q = QT[h % 2]; k = KT[h % 2]; z = pzs[ix % NZ]
        m.op("pe", lambda e: e.matmul(z[:, 0:w], k[:, j * 128:(j + 1) * 128], q[:, 512 * g + q0:512 * g + 512], start=True, stop=True),
             reads=[q, k], writes=[z])

    def s1(ix):
        h, g, j, md, q0, w = geom(ix)
        z = pzs[ix % NZ]; P = P_t[ix % NP]
        m.op("act", lambda e: e.activation(P[:, 0:w], z[:, 0:w], AF.Exp, bias=bias[:, h, g, j:j + 1], scale=0.125),
             reads=[z, bias], writes=[P])
        if md >= 0:
            m.op("pool", lambda e: e.affine_select(out=P[:, 0:128], in_=P[:, 0:128], pattern=[[1, 128]],
                                                   compare_op=ALU.is_ge, fill=0.0, base=0, channel_multiplier=-1),
                 reads=[P], writes=[P])

    def s2(ix):
        h, g, j, md, q0, w = geom(ix)
        v = V[h % 2]; os_ = ost[h % 2]; P = P_t[ix % NP]
        if g == 0 and j == 0 and h + 1 < NH:
            load(h + 1)
        for s_ in range(max(0, md), 4):
            lc = (s_ * 128) - q0
            m.op("pe", lambda e: e.matmul(po[s_][:, 0:65], P[:, lc:lc + 128], v[:, j, :], start=(j == 0), stop=(j == 4 * g + s_)),
                 reads=[P, v], writes=[po[s_]], pe_accum=True)
        if j == 4 * g + 3:
            for s_ in range(4):
                m.op("dve", lambda e: e.reciprocal(rc[:, s_:s_ + 1], po[s_][:, 64:65]), reads=[po[s_]], writes=[rc])
                m.op("dve", lambda e: e.tensor_scalar(os_[:, 4 * g + s_, :], po[s_][:, 0:64], rc[:, s_:s_ + 1], None, ALU.mult),
                     reads=[po[s_], rc], writes=[os_])
            if g == 7:
                m.dma(o_fx[:, h * 64:(h + 1) * 64].rearrange("(i p) d -> p i d", p=128), os_[:], reads=[os_], is_output=True)
    run_pipeline(len(items), [s0, s1, s2])


def build_B(do_conv=True, n_sb=4, n_fx=8):
    from contextlib import ExitStack
    nc = bass.Bass("TRN2", target_bir_lowering=False)
    dt = lambda n, s, d, k="ExternalInput": nc.dram_tensor(n, s, d, kind=k).ap()
    sbqT = dt("sbqT", [4, 64, 4096], BF16); sbkTr = dt("sbkTr", [4, 64, 4096], BF16); sbvr = dt("sbvr", [4096, 256], BF16)
    fxqT = dt("fxqT", [8, 64, 4096], BF16); fxkT = dt("fxkT", [8, 64, 4096], BF16); fxv = dt("fxv", [4096, 512], BF16)
    fT = dt("fT", [8, 4096], F32); negb = dt("bfg", [8, 1], F32)
    aT = dt("aT", [512, 2078], F32); gT = dt("gT", [512, 2078], F32)
    cwT = dt("cwT", [128, 4, 31], F32); cb = dt("cb", [128, 4], F32); lng = dt("lng", [512], F32); lnb = dt("lnb", [512], F32)
    o_sb = dt("o_sb", [4096, 256], BF16, "ExternalOutput"); o_fx = dt("o_fx", [4096, 512], BF16, "ExternalOutput")
    o_cv = dt("o_cv", [2048, 512], BF16, "ExternalOutput")
    m = MK(nc)
    identf = make_ident(m, F32)
    identb = m.sb([128, 128], BF16)
    m.op("dve", lambda e: e.tensor_copy(identb[:], identf[:]), reads=[identf], writes=[identb])
    PS = {"z": [m.ps([128, 512], F32) for _ in range(2)],
          "o": [m.ps([128, 512], F32) for _ in range(4)],
          "T": [m.ps([128, 8, 128], BF16)] * 2,
          "misc": m.ps([128, 512], F32)}
    if do_conv:
        with ExitStack() as es:
            emit_conv(m, nc, es, aT, gT, cwT, cb, lng, lnb, o_cv, identf, PS)
            barrier(m)
    if n_sb:
        with ExitStack() as es:
            emit_sb(m, nc, es, sbqT, sbkTr, sbvr, o_sb, identb, PS, n_sb)
            barrier(m)
    if n_fx:
        with ExitStack() as es:
            emit_fox(m, nc, es, fxqT, fxkT, fxv, fT, negb, o_fx, identf, PS, n_fx)
            barrier(m)
    m.finish()
    return nc
def ln_affine(m, r, st, mv, rstd, g_bc, b_bc, out):
    ln_stats(m, r, st, mv, rstd)
    m.op("dve", lambda e: e.tensor_scalar(r[:], r[:], mv[:, 0:1], rstd[:, 0:1], ALU.subtract, ALU.mult),
         reads=[r, mv, rstd], writes=[r])
    m.op("pool", lambda e: e.tensor_tensor(r[:], r[:], g_bc[:], ALU.mult), reads=[r, g_bc], writes=[r])
    m.op("dve", lambda e: e.tensor_tensor(out[:], r[:], b_bc[:], ALU.add), reads=[r, b_bc], writes=[out])


def emit_C1(m, nc, es, x, o, modv, w_out, ln1g, ln1b, rw, rb, x1_d, h2T_d, Wt, identb, identf, PS):
    def sb(shape, dt):
        m.nbuf += 1
        return Buf(es.enter_context(nc.sbuf_tensor("c1_%d" % m.nbuf, list(shape), dt)), "c1")
    wob = sb([128, 16, 2048], BF16)
    wst = [sb([128, 2048], F32) for _ in range(2)]
    bc = {}
    for nm, src in (("gt1", modv[0, :]), ("sh2", modv[1, :]), ("sc2", modv[2, :]), ("l1g", ln1g), ("l1b", ln1b)):
        bc[nm] = sb([128, 2048], F32)
        m.dma(bc[nm][:], src.partition_broadcast(128), writes=[bc[nm]])
    m.op("pool", lambda e: e.tensor_scalar_add(bc["sc2"][:], bc["sc2"][:], 1.0), reads=[bc["sc2"]], writes=[bc["sc2"]])
    rwt = sb([128, 16, 36], F32); rbt = sb([128, 36], F32)
    m.dma(rwt[:], rw.rearrange("(k p) n -> p k n", p=128), writes=[rwt])
    m.dma(rbt[:], rb.partition_broadcast(128), writes=[rbt])
    m.dma(wst[0][:], w_out[0:128, :], writes=[wst[0]])
    for k in range(16):
        if k + 1 < 16:
            m.dma(wst[(k + 1) % 2][:], w_out[(k + 1) * 128:(k + 2) * 128, :], writes=[wst[(k + 1) % 2]])
        if k % 2 == 0:
            m.op("act", lambda e: e.copy(wob[:, k, :], wst[k % 2][:]), reads=[wst[k % 2]], writes=[wob])
        else:
            m.op("pool", lambda e: e.tensor_copy(wob[:, k, :], wst[k % 2][:]), reads=[wst[k % 2]], writes=[wob])
    ot = [sb([128, 2048], BF16) for _ in range(2)]
    xt = [sb([128, 2048], F32) for _ in range(2)]
    oT = sb([128, 16, 128], BF16)
    tmp = sb([128, 2048], F32); x1 = sb([128, 2048], F32); h2f = sb([128, 2048], F32); h2b = sb([128, 2048], BF16)
    h2T = sb([128, 16, 128], BF16); h2fT = sb([128, 16, 128], F32)
    st = sb([128, 24], F32); mv = sb([128, 2], F32); rstd = sb([128, 1], F32)
    lg = sb([128, 36], F32)
    sm = {k: sb([128, n], F32) for k, n in (("m1", 1), ("nm1", 1), ("ohg", 4), ("e1", 4), ("s1", 1), ("pg", 1), ("t48", 32),
                                           ("sel", 8), ("m2a", 1), ("nm2a", 1), ("oh1", 8), ("sel2", 8), ("m2b", 1), ("oh2", 8),
                                           ("r", 1), ("den", 1), ("w1", 1), ("w2", 1), ("wsel", 8))}
    pT = PS["T"]; py = PS["o"]; pz = PS["z"]; pm = PS["misc"]

    def load(t):
        m.dma(ot[t % 2][:], o[t * 128:(t + 1) * 128, :], writes=[ot[t % 2]])
        m.dma(xt[t % 2][:], x[t * 128:(t + 1) * 128, :], writes=[xt[t % 2]])
    load(0)
    for t in range(16):
        if t + 1 < 16:
            load(t + 1)
        ob = ot[t % 2]; xx = xt[t % 2]
        for half in range(2):
            for j in range(8):
                kk = half * 8 + j
                m.op("pe", lambda e: e.transpose(pT[:, j, :], ob[:, kk * 128:(kk + 1) * 128], identb[:]),
                     reads=[ob, identb], writes=[pT], pe_accum=True)
            m.op("act", lambda e: e.copy(oT[:, half * 8:(half + 1) * 8, :], pT[:]), reads=[pT], writes=[oT])
        for n in range(4):
            p = py[n]
            for k in range(16):
                m.op("pe", lambda e: e.matmul(p[:], oT[:, k, :], wob[:, k, n * 512:(n + 1) * 512], start=(k == 0), stop=(k == 15)),
                     reads=[oT, wob], writes=[p], pe_accum=True)
            m.op("dve", lambda e: e.tensor_tensor(tmp[:, n * 512:(n + 1) * 512], p[:], bc["gt1"][:, n * 512:(n + 1) * 512], ALU.mult),
                 reads=[p, bc["gt1"]], writes=[tmp])
        m.op("dve", lambda e: e.scalar_tensor_tensor(tmp[:], xx[:], ALPHA, tmp[:], ALU.mult, ALU.add), reads=[xx, tmp], writes=[tmp])
        ln_affine(m, tmp, st, mv, rstd, bc["l1g"], bc["l1b"], x1)
        m.dma(x1_d[t * 128:(t + 1) * 128, :], x1[:], reads=[x1], writes=[x1_d])
        ln_stats(m, x1, st, mv, rstd)
        m.op("dve", lambda e: e.tensor_scalar(h2f[:], x1[:], mv[:, 0:1], rstd[:, 0:1], ALU.subtract, ALU.mult),
             reads=[x1, mv, rstd], writes=[h2f])
        m.op("pool", lambda e: e.tensor_tensor(h2f[:], h2f[:], bc["sc2"][:], ALU.mult), reads=[h2f, bc["sc2"]], writes=[h2f])
        m.op("dve", lambda e: e.tensor_tensor(h2f[:], h2f[:], bc["sh2"][:], ALU.add), reads=[h2f, bc["sh2"]], writes=[h2f])
        m.op("act", lambda e: e.copy(h2b[:], h2f[:]), reads=[h2f], writes=[h2b])
        for half in range(2):
            for j in range(8):
                kk = half * 8 + j
                m.op("pe", lambda e: e.transpose(pT[:, j, :], h2b[:, kk * 128:(kk + 1) * 128], identb[:]),
                     reads=[h2b, identb], writes=[pT], pe_accum=True)
            m.op("act", lambda e: e.copy(h2T[:, half * 8:(half + 1) * 8, :], pT[:]), reads=[pT], writes=[h2T])
        m.dma(h2T_d[:, :, t * 128:(t + 1) * 128], h2T[:], reads=[h2T], writes=[h2T_d])
        for q4 in range(4):
            p = pz[q4 % 2]
            for j in range(4):
                kk = q4 * 4 + j
                m.op("pe", lambda e: e.transpose(p[:, j * 128:(j + 1) * 128], h2f[:, kk * 128:(kk + 1) * 128], identf[:]),
                     reads=[h2f, identf], writes=[p], pe_accum=True)
            m.op("act", lambda e: e.copy(h2fT[:, q4 * 4:(q4 + 1) * 4, :].rearrange("p a b -> p (a b)"), p[:]), reads=[p], writes=[h2fT])
        for k in range(16):
            m.op("pe", lambda e: e.matmul(pm[:, 0:36], h2fT[:, k, :], rwt[:, k, :], start=(k == 0), stop=(k == 15)),
                 reads=[h2fT, rwt], writes=[pm], pe_accum=True)
        m.op("dve", lambda e: e.tensor_tensor(lg[:], pm[:, 0:36], rbt[:], ALU.add), reads=[pm, rbt], writes=[lg])
        S = sm
        def dv(fn, r, w):
            m.op("dve", fn, reads=r, writes=w)
        dv(lambda e: e.reduce_max(S["m1"][:], lg[:, 0:4], AX.X), [lg], [S["m1"]])
        dv(lambda e: e.tensor_scalar(S["ohg"][:], lg[:, 0:4], S["m1"][:, 0:1], None, ALU.is_equal), [lg, S["m1"]], [S["ohg"]])
        dv(lambda e: e.tensor_scalar_mul(S["nm1"][:], S["m1"][:], -1.0), [S["m1"]], [S["nm1"]])
        m.op("act", lambda e: e.activation(S["e1"][:], lg[:, 0:4], AF.Exp, bias=S["nm1"][:, 0:1], accum_out=S["s1"][:, 0:1]),
             reads=[lg, S["nm1"]], writes=[S["e1"], S["s1"]])
        dv(lambda e: e.reciprocal(S["pg"][:], S["s1"][:]), [S["s1"]], [S["pg"]])
        l2 = lg[:, 4:36].rearrange("p (g e) -> p g e", g=4)
        t48 = S["t48"][:].rearrange("p (g e) -> p g e", g=4)
        dv(lambda e: e.tensor_tensor(t48, l2, S["ohg"][:].unsqueeze(2).to_broadcast([128, 4, 8]), ALU.mult), [lg, S["ohg"]], [S["t48"]])
        dv(lambda e: e.tensor_reduce(S["sel"][:], S["t48"][:].rearrange("p (g e) -> p e g", g=4), AX.X, ALU.add), [S["t48"]], [S["sel"]])
        dv(lambda e: e.reduce_max(S["m2a"][:], S["sel"][:], AX.X), [S["sel"]], [S["m2a"]])
        dv(lambda e: e.tensor_scalar(S["oh1"][:], S["sel"][:], S["m2a"][:, 0:1], None, ALU.is_equal), [S["sel"], S["m2a"]], [S["oh1"]])
        dv(lambda e: e.scalar_tensor_tensor(S["sel2"][:], S["oh1"][:], -1e30, S["sel"][:], ALU.mult, ALU.add), [S["oh1"], S["sel"]], [S["sel2"]])
        dv(lambda e: e.reduce_max(S["m2b"][:], S["sel2"][:], AX.X), [S["sel2"]], [S["m2b"]])
        dv(lambda e: e.tensor_scalar(S["oh2"][:], S["sel2"][:], S["m2b"][:, 0:1], None, ALU.is_equal), [S["sel2"], S["m2b"]], [S["oh2"]])
        dv(lambda e: e.tensor_scalar_mul(S["nm2a"][:], S["m2a"][:], -1.0), [S["m2a"]], [S["nm2a"]])
        m.op("act", lambda e: e.activation(S["r"][:], S["m2b"][:], AF.Exp, bias=S["nm2a"][:, 0:1]), reads=[S["m2b"], S["nm2a"]], writes=[S["r"]])
        dv(lambda e: e.tensor_scalar_add(S["den"][:], S["r"][:], 1.0), [S["r"]], [S["den"]])
        dv(lambda e: e.reciprocal(S["den"][:], S["den"][:]), [S["den"]], [S["den"]])
        dv(lambda e: e.tensor_tensor(S["w1"][:], S["pg"][:], S["den"][:], ALU.mult), [S["pg"], S["den"]], [S["w1"]])
        dv(lambda e: e.tensor_tensor(S["w2"][:], S["w1"][:], S["r"][:], ALU.mult), [S["w1"], S["r"]], [S["w2"]])
        dv(lambda e: e.tensor_scalar(S["wsel"][:], S["oh1"][:], S["w1"][:, 0:1], None, ALU.mult), [S["oh1"], S["w1"]], [S["wsel"]])
        dv(lambda e: e.scalar_tensor_tensor(S["wsel"][:], S["oh2"][:], S["w2"][:, 0:1], S["wsel"][:], ALU.mult, ALU.add),
           [S["oh2"], S["w2"], S["wsel"]], [S["wsel"]])
        dv(lambda e: e.tensor_tensor(Wt[:, t, :].rearrange("p (g e) -> p g e", g=4), S["ohg"][:].unsqueeze(2).to_broadcast([128, 4, 8]),
                                     S["wsel"][:].unsqueeze(1).to_broadcast([128, 4, 8]), ALU.mult), [S["ohg"], S["wsel"]], [Wt])


def emit_C2(m, nc, es, modv, ln2g, ln2b, wg, wu, wd, x1_d, h2T_d, Wt, x_out, PS, n_exp=32):
    from contextlib import ExitStack

    def mk_sb(stack, tag):
        def sb(shape, dt):
            m.nbuf += 1
            return Buf(stack.enter_context(nc.sbuf_tensor("%s_%d" % (tag, m.nbuf), list(shape), dt)), tag)
        return sb
    sbo = mk_sb(es, "c2o")
    NTOK = 1024; NTT = NTOK // 128; NTG = NTOK // 512
    h2T = sbo([128, 16, NTOK], BF16)
    yacc = sbo([128, NTT, 2048], F32)
    gbanks = PS["z"]; ubanks = PS["o"][0:2]; pyb = PS["o"][2:4]
    for hp in range(2048 // NTOK):
        m.dma(h2T[:], h2T_d[:, :, hp * NTOK:(hp + 1) * NTOK], reads=[h2T_d], writes=[h2T])
        with ExitStack() as es2:
            sb = mk_sb(es2, "c2w")
            hidT = sb([128, 8, NTOK], BF16)
            gst = sb([128, 16, 128], F32); ust = sb([128, 16, 128], F32)
            wgb = [sb([128, 16, 128], BF16) for _ in range(2)]
            wub = [sb([128, 16, 128], BF16) for _ in range(2)]
            dst = sb([128, 2048], F32)
            wdb = sb([128, 8, 2048], BF16)
            sg = [sb([128, 512], F32) for _ in range(2)]
            items = [(e, fc) for e in range(n_exp) for fc in range(8)]

            def load_gu(ix):
                e, fc = items[ix]
                m.dma(gst[:], wg[e, :, fc * 128:(fc + 1) * 128].rearrange("(k p) n -> p k n", p=128), writes=[gst])
                m.dma(ust[:], wu[e, :, fc * 128:(fc + 1) * 128].rearrange("(k p) n -> p k n", p=128), writes=[ust])
                m.dma(dst[:], wd[e, fc * 128:(fc + 1) * 128, :], writes=[dst])

            def convert(ix):
                e, fc = items[ix]
                m.op("act", lambda en: en.copy(wgb[ix % 2][:], gst[:]), reads=[gst], writes=[wgb[ix % 2]])
                m.op("pool", lambda en: en.tensor_copy(wub[ix % 2][:], ust[:]), reads=[ust], writes=[wub[ix % 2]])
            load_gu(0)
            convert(0)
            si = 0
            for ix, (e, fc) in enumerate(items):
                gb = wgb[ix % 2]; ub = wub[ix % 2]
                if fc == 0:
                    pass
                m.op("pool", lambda en: en.tensor_copy(wdb[:, fc, :], dst[:]), reads=[dst], writes=[wdb])
                if ix + 1 < len(items):
                    load_gu(ix + 1)
                for tg in range(NTG):
                    a = gbanks[tg]; b = ubanks[tg]; s_ = sg[si % 2]; si += 1
                    for k in range(16):
                        m.op("pe", lambda en: en.matmul(a[:], gb[:, k, :], h2T[:, k, tg * 512:(tg + 1) * 512], start=(k == 0), stop=(k == 15)),
                             reads=[gb, h2T], writes=[a], pe_accum=True)
                    for k in range(16):
                        m.op("pe", lambda en: en.matmul(b[:], ub[:, k, :], h2T[:, k, tg * 512:(tg + 1) * 512], start=(k == 0), stop=(k == 15)),
                             reads=[ub, h2T], writes=[b], pe_accum=True)
                    m.op("act", lambda en: en.activation(s_[:], a[:], AF.Silu), reads=[a], writes=[s_])
                    m.op("dve", lambda en: en.tensor_tensor(hidT[:, fc, tg * 512:(tg + 1) * 512], s_[:], b[:], ALU.mult),
                         reads=[s_, b], writes=[hidT])
                if ix + 1 < len(items):
                    convert(ix + 1)
                if fc == 7:
                    for tt in range(NTT):
                        tile_i = hp * NTT + tt
                        for n in range(4):
                            p = pyb[(tt * 4 + n) % 2]
                            for f2 in range(8):
                                m.op("pe", lambda en: en.matmul(p[:], hidT[:, f2, tt * 128:(tt + 1) * 128], wdb[:, f2, n * 512:(n + 1) * 512],
                                                                start=(f2 == 0), stop=(f2 == 7)),
                                     reads=[hidT, wdb], writes=[p], pe_accum=True)
                            ys = yacc[:, tt, n * 512:(n + 1) * 512]
                            if e == 0:
                                m.op("dve", lambda en: en.tensor_scalar(ys, p[:], Wt[:, tile_i, e:e + 1], None, ALU.mult),
                                     reads=[p, Wt], writes=[yacc])
                            else:
                                m.op("dve", lambda en: en.scalar_tensor_tensor(ys, p[:], Wt[:, tile_i, e:e + 1], ys, ALU.mult, ALU.add),
                                     reads=[p, Wt, yacc], writes=[yacc])
            barrier(m)
        with ExitStack() as es3:
            sb = mk_sb(es3, "c2e")
            bc = {}
            for nm, src in (("gt2", modv[3, :]), ("l2g", ln2g), ("l2b", ln2b)):
                bc[nm] = sb([128, 2048], F32)
                m.dma(bc[nm][:], src.partition_broadcast(128), writes=[bc[nm]])
            xt = [sb([128, 2048], F32) for _ in range(2)]; outt = [sb([128, 2048], F32) for _ in range(2)]
            st = sb([128, 24], F32); mv = sb([128, 2], F32); rstd = sb([128, 1], F32)
            for tt in range(NTT):
                tile_i = hp * NTT + tt
                x_ = xt[tt % 2]; o_ = outt[tt % 2]
                m.dma(x_[:], x1_d[tile_i * 128:(tile_i + 1) * 128, :], reads=[x1_d], writes=[x_])
                m.op("pool", lambda en: en.tensor_tensor(yacc[:, tt, :], yacc[:, tt, :], bc["gt2"][:], ALU.mult), reads=[yacc, bc["gt2"]], writes=[yacc])
                m.op("dve", lambda en: en.scalar_tensor_tensor(x_[:], x_[:], ALPHA, yacc[:, tt, :], ALU.mult, ALU.add), reads=[x_, yacc], writes=[x_])
                ln_affine(m, x_, st, mv, rstd, bc["l2g"], bc["l2b"], o_)
                m.dma(x_out[tile_i * 128:(tile_i + 1) * 128, :], o_[:], reads=[o_], is_output=True)
            barrier(m)


def build_C(n_exp=32, n_alloc=32):
    from contextlib import ExitStack
    nc = bass.Bass("TRN2", target_bir_lowering=False)
    dt = lambda n, s, d, k="ExternalInput": nc.dram_tensor(n, s, d, kind=k).ap()
    x = dt("x", [2048, 2048], F32); o = dt("o", [2048, 2048], BF16); modv = dt("modv", [4, 2048], F32)
    w_out = dt("w_out", [2048, 2048], F32)
    ln1g = dt("ln1g", [2048], F32); ln1b = dt("ln1b", [2048], F32); ln2g = dt("ln2g", [2048], F32); ln2b = dt("ln2b", [2048], F32)
    rw = dt("rw", [2048, 36], F32); rb = dt("rb", [36], F32)
    wg = dt("wg", [n_alloc, 2048, 1024], F32); wu = dt("wu", [n_alloc, 2048, 1024], F32); wd = dt("wd", [n_alloc, 1024, 2048], F32)
    x_out = dt("x_out", [2048, 2048], F32, "ExternalOutput")
    m = MK(nc)
    x1_d = m.dram("x1_d", [2048, 2048], F32)
    h2T_d = m.dram("h2T_d", [128, 16, 2048], BF16)
    identf = make_ident(m, F32)
    identb = m.sb([128, 128], BF16)
    m.op("dve", lambda e: e.tensor_copy(identb[:], identf[:]), reads=[identf], writes=[identb])
    Wt = m.sb([128, 16, 32], F32)
    PS = {"z": [m.ps([128, 512], F32) for _ in range(2)],
          "o": [m.ps([128, 512], F32) for _ in range(4)],
          "T": m.ps([128, 8, 128], BF16),
          "misc": m.ps([128, 512], F32)}
    with ExitStack() as es:
        emit_C1(m, nc, es, x, o, modv, w_out, ln1g, ln1b, rw, rb, x1_d, h2T_d, Wt, identb, identf, PS)
        barrier(m)
    with ExitStack() as es:
        emit_C2(m, nc, es, modv, ln2g, ln2b, wg, wu, wd, x1_d, h2T_d, Wt, x_out, PS, n_exp)
        barrier(m)
    m.finish()
    return nc
def cc_allgather(m, nc, src_ap, dst_ap):
    barrier(m)
    if "cc" not in m.sem:
        m.sem["cc"] = nc.alloc_semaphore("ccsem"); m.cnt["cc"] = 0
    ins = nc.gpsimd.collective_compute("AllGather", ALU.bypass, replica_groups=[[0, 1], [2, 3], [4, 5], [6, 7]],
                                       ins=[src_ap.opt()], outs=[dst_ap.opt()])
    m.cnt["cc"] += 1
    ins.then_inc(m.sem["cc"], 1)
    for e in m.E:
        m.E[e].wait_ge(m.sem["cc"], m.cnt["cc"])


class GB:
    def __init__(self, nc, name, rows, cols, dt, rows_k):
        self.nk = (rows + rows_k - 1) // rows_k; self.rk = rows_k
        self.src = nc.dram_tensor(name, [self.nk * rows_k, cols], dt, kind="Internal").ap()
        self.dst = nc.dram_tensor(name + "g", [self.nk * 2 * rows_k, cols], dt, kind="Internal").ap()

    def gather(self, m, nc):
        rk = self.rk
        for k in range(self.nk):
            cc_allgather(m, nc, self.src[k * rk:(k + 1) * rk, :], self.dst[k * 2 * rk:(k + 1) * 2 * rk, :])

    def g(self, r, lo, hi):
        rk = self.rk
        k = lo // rk
        assert (hi - 1) // rk == k, (lo, hi, rk)
        base = k * 2 * rk + r * rk + (lo - k * rk)
        return self.dst[base:base + (hi - lo), :]


FM_BLOCKS_F = ([(c0, "qk", o0) for c0, o0 in zip(range(0, 512, 256), range(0, 512, 256))] +
               [(c0, "qk", o0) for c0, o0 in zip(range(1536, 3584, 256), range(512, 2560, 256))] +
               [(c0, "fag", o0) for c0, o0 in zip(range(4624, 5648, 256), range(0, 1024, 256))])
TM_BLOCKS_F = ([(c0, o0) for c0, o0 in zip(range(512, 1536, 256), range(0, 1024, 256))] +
               [(c0, o0) for c0, o0 in zip(range(3584, 4608, 256), range(1024, 2048, 256))])


def emit_A2(m, nc, es, x, sh_ap, sc_ap, w_in, qkT, fagT, vtm, ident, PS):
    def sb(shape, dt):
        m.nbuf += 1
        return Buf(es.enter_context(nc.sbuf_tensor("a_%d" % m.nbuf, list(shape), dt)), "a")
    NT = 16
    hT = sb([128, 16, 2048], BF16)
    scb = sb([128, 2048], F32); shb = sb([128, 2048], F32)
    xts = [sb([128, 2048], F32) for _ in range(2)]
    hb = [sb([128, 2048], BF16) for _ in range(2)]
    st = sb([128, 24], F32); mv = sb([128, 2], F32); rstd = sb([128, 1], F32)
    pT = PS["T"]
    m.dma(shb[:], sh_ap.partition_broadcast(128), writes=[shb])
    m.dma(scb[:], sc_ap.partition_broadcast(128), writes=[scb])
    m.op("pool", lambda e: e.tensor_scalar_add(scb[:], scb[:], 1.0), reads=[scb], writes=[scb])
    m.dma(xts[0][:], x[0:128, :], writes=[xts[0]])
    for t in range(NT):
        xt = xts[t % 2]; h = hb[t % 2]
        if t + 1 < NT:
            m.dma(xts[(t + 1) % 2][:], x[(t + 1) * 128:(t + 2) * 128, :], writes=[xts[(t + 1) % 2]])
        ln_stats(m, xt, st, mv, rstd)
        m.op("dve", lambda e: e.tensor_scalar(xt[:], xt[:], mv[:, 0:1], rstd[:, 0:1], ALU.subtract, ALU.mult),
             reads=[xt, mv, rstd], writes=[xt])
        m.op("pool", lambda e: e.tensor_tensor(xt[:], xt[:], scb[:], ALU.mult), reads=[xt, scb], writes=[xt])
        m.op("dve", lambda e: e.tensor_tensor(h[:], xt[:], shb[:], ALU.add), reads=[xt, shb], writes=[h])
        for half in range(2):
            for j in range(8):
                k = half * 8 + j
                m.op("pe", lambda e: e.transpose(pT[:, j, :], h[:, k * 128:(k + 1) * 128], ident[:]),
                     reads=[h, ident], writes=[pT], pe_accum=True)
            m.op("act", lambda e: e.copy(hT[:, half * 8:(half + 1) * 8, t * 128:(t + 1) * 128], pT[:]),
                 reads=[pT], writes=[hT])
    wst = [sb([128, 16, 256], F32) for _ in range(2)]
    wbf = [sb([128, 16, 256], BF16) for _ in range(2)]
    pm = PS["o"]
    oq = [sb([128, 2048], BF16) for _ in range(2)]
    of = [sb([128, 2048], F32) for _ in range(2)]
    ov = [sb([128, 256], BF16) for _ in range(2)]
    blocks = [("fm", c0, kind, o0) for (c0, kind, o0) in FM_BLOCKS_F] + [("tm", c0, None, o0) for (c0, o0) in TM_BLOCKS_F]
    blocks.append(("fm16", 4608, "fag", 1024))
    pi = 0; oi = 0

    def load_w(bi):
        typ, c0, kind, o0 = blocks[bi]
        cw = 16 if typ == "fm16" else 256
        m.dma(wst[bi % 2][:, :, 0:cw], w_in[:, c0:c0 + cw].rearrange("(k p) n -> p k n", p=128), writes=[wst[bi % 2]])
    load_w(0)
    for bi, (typ, c0, kind, o0) in enumerate(blocks):
        cw = 16 if typ == "fm16" else 256
        ws = wst[bi % 2]; wb = wbf[bi % 2]
        if bi + 1 < len(blocks):
            load_w(bi + 1)
        m.op("act", lambda e: e.copy(wb[:, 0:8, 0:cw], ws[:, 0:8, 0:cw]), reads=[ws], writes=[wb])
        m.op("pool", lambda e: e.tensor_copy(wb[:, 8:16, 0:cw], ws[:, 8:16, 0:cw]), reads=[ws], writes=[wb])
        if typ in ("fm", "fm16"):
            for cc in range(0, cw, 128):
                cn = min(128, cw - cc)
                o = (oq if kind == "qk" else of)[oi % 2]; oi += 1
                for tg in range(4):
                    p = pm[pi % 4]; pi += 1
                    for k in range(16):
                        m.op("pe", lambda e: e.matmul(p[0:cn, :], wb[:, k, cc:cc + cn], hT[:, k, tg * 512:(tg + 1) * 512],
                                                      start=(k == 0), stop=(k == 15)),
                             reads=[wb, hT], writes=[p], pe_accum=True)
                    if tg % 2 == 0:
                        m.op("act", lambda e: e.copy(o[0:cn, tg * 512:(tg + 1) * 512], p[0:cn, :]), reads=[p], writes=[o])
                    else:
                        m.op("dve", lambda e: e.tensor_copy(o[0:cn, tg * 512:(tg + 1) * 512], p[0:cn, :]), reads=[p], writes=[o])
                dst = qkT if kind == "qk" else fagT
                m.dma(dst[o0 + cc:o0 + cc + cn, :], o[0:cn, :], reads=[o])
        else:
            for tt in range(16):
                p = pm[pi % 4]; pi += 1
                o = ov[oi % 2]; oi += 1
                for k in range(16):
                    m.op("pe", lambda e: e.matmul(p[:, 0:256], hT[:, k, tt * 128:(tt + 1) * 128], wb[:, k, :],
                                                  start=(k == 0), stop=(k == 15)),
                         reads=[wb, hT], writes=[p], pe_accum=True)
                if tt % 2 == 0:
                    m.op("act", lambda e: e.copy(o[:], p[:, 0:256]), reads=[p], writes=[o])
                else:
                    m.op("dve", lambda e: e.tensor_copy(o[:], p[:, 0:256]), reads=[p], writes=[o])
                m.dma(vtm[tt * 128:(tt + 1) * 128, o0:o0 + 256], o[:], reads=[o])


def blend_tiles(m, sb_pool, sel, dst_fn, srcA_fn, srcB_fn, ntiles, shape, dt, post=None):
    ta, tb = (sb_pool["a"], sb_pool["b"]) if dt == BF16 else (sb_pool["a32"], sb_pool["b32"])
    np_ = shape[0]
    for i in range(ntiles):
        a = ta[i % 2]; b = tb[i % 2]
        av = a[0:np_, 0:shape[1]]; bv = b[0:np_, 0:shape[1]]
        m.dma(av, srcA_fn(i), writes=[a])
        m.dma(bv, srcB_fn(i), writes=[b])
        m.op("dve", lambda e: e.tensor_scalar(av, av, sel[0:np_, 0:1], None, ALU.mult), reads=[a, sel], writes=[a])
        m.op("dve", lambda e: e.scalar_tensor_tensor(av, bv, sel[0:np_, 1:2], av, ALU.mult, ALU.add), reads=[a, b, sel], writes=[a])
        if post is None:
            m.dma(dst_fn(i), av, reads=[a])
        else:
            post(i, a, av)


def emit_select1(m, nc, es, sel, G1, G2, G3, S, identb, jmat, PS):
    def sb(shape, dt):
        m.nbuf += 1
        return Buf(es.enter_context(nc.sbuf_tensor("s_%d" % m.nbuf, list(shape), dt)), "s")
    pool = {"a": [sb([128, 2048], BF16) for _ in range(2)], "b": [sb([128, 2048], BF16) for _ in range(2)],
            "a32": [sb([128, 2048], F32) for _ in range(2)], "b32": [sb([128, 2048], F32) for _ in range(2)]}
    for r in range(2):
        tk = slice(r * 2048, (r + 1) * 2048)
        for (dst, a0, b0, nrows) in ((S["sbqT"], 0, 256, 256), (S["fxqT"], 512, 1024, 512), (S["fxkT"], 1536, 2048, 512)):
            blend_tiles(m, pool, sel, lambda i: dst[i * 128:(i + 1) * 128, tk], lambda i: G1.g(r, a0 + i * 128, a0 + (i + 1) * 128),
                        lambda i: G1.g(r, b0 + i * 128, b0 + (i + 1) * 128), nrows // 128, [128, 2048], BF16)
        blend_tiles(m, pool, sel, lambda i: S["fxv"][r * 2048 + i * 128:r * 2048 + (i + 1) * 128, :],
                    lambda i: G2.g(r, i * 128, (i + 1) * 128)[:, 1024:1536], lambda i: G2.g(r, i * 128, (i + 1) * 128)[:, 1536:2048], 16, [128, 512], BF16)
        blend_tiles(m, pool, sel, lambda i: S["fT"][:, tk], lambda i: G3.g(r, 1024, 1032), lambda i: G3.g(r, 1032, 1040), 1, [8, 2048], F32)
        blend_tiles(m, pool, sel, lambda i: S["aT"][i * 128:(i + 1) * 128, 30 + r * 2048:30 + (r + 1) * 2048],
                    lambda i: G3.g(r, i * 128, (i + 1) * 128), lambda i: G3.g(r, 256 + i * 128, 256 + (i + 1) * 128), 2, [128, 2048], F32)
        blend_tiles(m, pool, sel, lambda i: S["gT"][i * 128:(i + 1) * 128, 30 + r * 2048:30 + (r + 1) * 2048],
                    lambda i: G3.g(r, 512 + i * 128, 512 + (i + 1) * 128), lambda i: G3.g(r, 768 + i * 128, 768 + (i + 1) * 128), 2, [128, 2048], F32)
        pk = PS["z"]; pv = PS["misc"]
        ko = [sb([64, 4, 128], BF16) for _ in range(2)]
        vo = [sb([128, 256], BF16) for _ in range(2)]

        def post_k(i, a, av):
            gt = r * 16 + i; rb = 31 - gt
            p = pk[i % 2]; o = ko[i % 2]
            for h in range(4):
                m.op("pe", lambda e: e.matmul(p[0:64, h * 128:(h + 1) * 128], a[:, h * 64:(h + 1) * 64], jmat[:], start=True, stop=True),
                     reads=[a, jmat], writes=[p], pe_accum=True)
            m.op("act", lambda e: e.copy(o[:].rearrange("p h t -> p (h t)"), p[0:64, 0:512]), reads=[p], writes=[o])
            m.dma(S["sbkTr"][:, :, rb * 128:(rb + 1) * 128].rearrange("h d t -> d h t"), o[:], reads=[o])

        def post_v(i, a, av):
            gt = r * 16 + i; rb = 31 - gt
            o = vo[i % 2]
            m.op("pe", lambda e: e.matmul(pv[:, 0:256], jmat[:], a[:, 0:256], start=True, stop=True), reads=[a, jmat], writes=[pv])
            m.op("act", lambda e: e.copy(o[:], pv[:, 0:256]), reads=[pv], writes=[o])
            m.dma(S["sbvr"][rb * 128:(rb + 1) * 128, :], o[:], reads=[o])
        blend_tiles(m, pool, sel, None, lambda i: G2.g(r, i * 128, (i + 1) * 128)[:, 0:256], lambda i: G2.g(r, i * 128, (i + 1) * 128)[:, 256:512],
                    16, [128, 256], BF16, post=post_k)
        blend_tiles(m, pool, sel, None, lambda i: G2.g(r, i * 128, (i + 1) * 128)[:, 512:768], lambda i: G2.g(r, i * 128, (i + 1) * 128)[:, 768:1024],
                    16, [128, 256], BF16, post=post_v)
    z = sb([128, 30], F32)
    m.op("pool", lambda e: e.memset(z[:], 0.0), writes=[z])
    for i in range(2):
        m.dma(S["aT"][i * 128:(i + 1) * 128, 0:30], z[:], reads=[z])
        m.dma(S["gT"][i * 128:(i + 1) * 128, 0:30], z[:], reads=[z])


def emit_select2(m, nc, es, sel, G5, ocv, So):
    def sb(shape, dt):
        m.nbuf += 1
        return Buf(es.enter_context(nc.sbuf_tensor("s2_%d" % m.nbuf, list(shape), dt)), "s2")
    pool = {"a": [sb([128, 512], BF16) for _ in range(2)], "b": [sb([128, 512], BF16) for _ in range(2)]}
    rows = lambda hf, i: (hf * 2048 + i * 128, hf * 2048 + (i + 1) * 128)
    for (src_fn, c0, cw) in ((lambda hf, i: G5.g(0, *rows(hf, i))[:, 0:256], 0, 256), (lambda hf, i: G5.g(1, *rows(hf, i))[:, 0:256], 256, 256),
                             (lambda hf, i: G5.g(0, *rows(hf, i))[:, 256:768], 512, 512), (lambda hf, i: G5.g(1, *rows(hf, i))[:, 256:768], 1024, 512),
                             (lambda hf, i: ocv[rows(hf, i)[0]:rows(hf, i)[1], :], 1536, 512)):
        blend_tiles(m, pool, sel, lambda i: So[i * 128:(i + 1) * 128, c0:c0 + cw], lambda i: src_fn(0, i), lambda i: src_fn(1, i),
                    16, [128, cw], BF16)


def emit_conv2(m, nc, es, aT, gT, cwT, cb, G4):
    def sb(shape, dt):
        m.nbuf += 1
        return Buf(es.enter_context(nc.sbuf_tensor("cv%d" % m.nbuf, list(shape), dt)), "cv")
    a = sb([128, 4126], F32); g = sb([128, 4126], F32); acc = sb([128, 4096], F32)
    cw = sb([128, 2, 31], F32); cbt = sb([128, 2], F32)
    m.dma(cw[:], cwT, writes=[cw])
    m.dma(cbt[:], cb, writes=[cbt])
    for cc in range(2):
        m.dma(a[:], aT[cc * 128:(cc + 1) * 128, :], writes=[a])
        m.dma(g[:], gT[cc * 128:(cc + 1) * 128, :], writes=[g])
        m.op("act", lambda e: e.activation(g[:], g[:], AF.Sigmoid), reads=[g], writes=[g])
        m.op("dve", lambda e: e.tensor_tensor(a[:], a[:], g[:], ALU.mult), reads=[a, g], writes=[a])
        m.op("dve", lambda e: e.tensor_scalar(acc[:], a[:, 0:4096], cw[:, cc, 0:1], cbt[:, cc:cc + 1], ALU.mult, ALU.add),
             reads=[a, cw, cbt], writes=[acc])
        for w in range(1, 31):
            m.op("dve", lambda e: e.scalar_tensor_tensor(acc[:], a[:, w:w + 4096], cw[:, cc, w:w + 1], acc[:], ALU.mult, ALU.add),
                 reads=[a, cw, acc], writes=[acc])
        m.dma(G4[cc * 128:(cc + 1) * 128, :], acc[:], reads=[acc])


def emit_convln(m, nc, es, G4, lng, lnb, o_cv, identf, PS):
    def sb(shape, dt):
        m.nbuf += 1
        return Buf(es.enter_context(nc.sbuf_tensor("cl%d" % m.nbuf, list(shape), dt)), "cl")
    cvT = sb([128, 4, 4096], F32)
    lg = sb([128, 512], F32); lb = sb([128, 512], F32)
    m.dma(lg[:], lng.partition_broadcast(128), writes=[lg])
    m.dma(lb[:], lnb.partition_broadcast(128), writes=[lb])
    for cc in range(4):
        m.dma(cvT[:, cc, :], G4.g(cc // 2, (cc % 2) * 128, (cc % 2 + 1) * 128), writes=[cvT])
    pt = PS["z"]
    ut = [sb([128, 512], F32) for _ in range(2)]
    ob = [sb([128, 512], BF16) for _ in range(2)]
    st = sb([128, 6], F32); mv = sb([128, 2], F32); rstd = sb([128, 1], F32)
    for t in range(32):
        p = pt[t % 2]; u = ut[t % 2]; o = ob[t % 2]
        for cc in range(4):
            m.op("pe", lambda e: e.transpose(p[:, cc * 128:(cc + 1) * 128], cvT[:, cc, t * 128:(t + 1) * 128], identf[:]),
                 reads=[cvT, identf], writes=[p], pe_accum=True)
        m.op("act", lambda e: e.copy(u[:], p[:]), reads=[p], writes=[u])
        m.op("dve", lambda e: e.bn_stats(st[:], u[:]), reads=[u], writes=[st])
        m.op("dve", lambda e: e.bn_aggr(mv[:], st[:]), reads=[st], writes=[mv])
        m.op("dve", lambda e: e.tensor_scalar_add(rstd[:], mv[:, 1:2], LN_EPS), reads=[mv], writes=[rstd])
        m.op("act", lambda e: e.activation(rstd[:], rstd[:], AF.Sqrt), reads=[rstd], writes=[rstd])
        m.op("dve", lambda e: e.reciprocal(rstd[:], rstd[:]), reads=[rstd], writes=[rstd])
        m.op("dve", lambda e: e.tensor_scalar(u[:], u[:], mv[:, 0:1], rstd[:, 0:1], ALU.subtract, ALU.mult),
             reads=[u, mv, rstd], writes=[u])
        m.op("pool", lambda e: e.tensor_tensor(u[:], u[:], lg[:], ALU.mult), reads=[u, lg], writes=[u])
        m.op("dve", lambda e: e.tensor_tensor(u[:], u[:], lb[:], ALU.add), reads=[u, lb], writes=[u])
        m.op("act", lambda e: e.activation(o[:], u[:], AF.Silu), reads=[u], writes=[o])
        m.dma(o_cv[t * 128:(t + 1) * 128, :], o[:], reads=[o])


def emit_mod(m, nc, es, cT, aw, ab, Gm, PS):
    def sb(shape, dt):
        m.nbuf += 1
        return Buf(es.enter_context(nc.sbuf_tensor("mo%d" % m.nbuf, list(shape), dt)), "mo")
    ct = sb([128, 16], F32); sc = sb([128, 16], F32)
    wst = [sb([128, 16, 512], F32) for _ in range(2)]
    bt = [sb([1, 512], F32) for _ in range(2)]
    ot = [sb([1, 512], F32) for _ in range(2)]
    pp = PS["z"]
    m.dma(ct[:], cT, writes=[ct])
    m.op("act", lambda e: e.activation(sc[:], ct[:], AF.Silu), reads=[ct], writes=[sc])
    items = [(l, n) for l in range(2) for n in range(12)]

    def load(i):
        l, n = items[i]
        m.dma(wst[i % 2][:], aw[l, :, n * 512:(n + 1) * 512].rearrange("(k p) n -> p k n", p=128), writes=[wst[i % 2]])
        m.dma(bt[i % 2][:], ab[l:l + 1, n * 512:(n + 1) * 512], writes=[bt[i % 2]])
    load(0)
    for i, (l, n) in enumerate(items):
        if i + 1 < len(items):
            load(i + 1)
        w = wst[i % 2]; b = bt[i % 2]; o = ot[i % 2]; p = pp[i % 2]
        for k in range(16):
            m.op("pe", lambda e: e.matmul(p[0:1, :], sc[:, k:k + 1], w[:, k, :], start=(k == 0), stop=(k == 15)),
                 reads=[sc, w], writes=[p], pe_accum=True)
        m.op("dve", lambda e: e.tensor_tensor(o[:], p[0:1, :], b[:], ALU.add), reads=[p, b], writes=[o])
        m.dma(Gm[l:l + 1, n * 512:(n + 1) * 512], o[:], reads=[o])


def build_F(n_exp=32, depth=DEPTH, n_alloc=32):
    from contextlib import ExitStack
    nc = bass.Bass("TRN2", target_bir_lowering=False)
    dt = lambda n, s, d, k="ExternalInput": nc.dram_tensor(n, s, d, kind=k).ap()
    x_in = dt("x", [2048, 2048], F32); cT = dt("cT", [128, 16], F32); sel_d = dt("sel", [128, 2], F32)
    aw = dt("aw", [2, 2048, 6144], F32); ab = dt("ab", [2, 6144], F32)
    w_in = dt("w_in", [2, 2048, MIX_COLS], F32); w_out = dt("w_out", [2, 2048, 2048], F32)
    bfg = dt("bfg", [2, 8, 1], F32); cwT = dt("cwT", [2, 128, 2, 31], F32); cb = dt("cb", [2, 128, 2], F32)
    lng = dt("lng", [2, 512], F32); lnb = dt("lnb", [2, 512], F32)
    ln1g = dt("ln1g", [2, 2048], F32); ln1b = dt("ln1b", [2, 2048], F32); ln2g = dt("ln2g", [2, 2048], F32); ln2b = dt("ln2b", [2, 2048], F32)
    rw = dt("rw", [2, 2048, 36], F32); rb = dt("rb", [2, 36], F32)
    wg = dt("wg", [2, n_alloc, 2048, 1024], F32); wu = dt("wu", [2, n_alloc, 2048, 1024], F32); wd = dt("wd", [2, n_alloc, 1024, 2048], F32)
    x_out = dt("x_out", [2048, 2048], F32, "ExternalOutput")
    m = MK(nc)
    D = lambda n, s, d: nc.dram_tensor(n, list(s), d, kind="Internal").ap()
    Gm = GB(nc, "Gm", 2, 6144, F32, 2)
    G1 = GB(nc, "G1", 2560, 2048, BF16, 512); G2 = GB(nc, "G2", 2048, 2048, BF16, 512)
    G3 = GB(nc, "G3", 1040, 2048, F32, 256); G4 = GB(nc, "G4", 256, 4096, F32, 128); G5 = GB(nc, "G5", 4096, 768, BF16, 1024)
    S = {"sbqT": D("S_sbqT", [256, 4096], BF16), "fxqT": D("S_fxqT", [512, 4096], BF16), "fxkT": D("S_fxkT", [512, 4096], BF16),
         "sbkTr": D("S_sbkTr", [4, 64, 4096], BF16), "sbvr": D("S_sbvr", [4096, 256], BF16), "fxv": D("S_fxv", [4096, 512], BF16),
         "fT": D("S_fT", [8, 4096], F32), "aT": D("S_aT", [256, 4126], F32), "gT": D("S_gT", [256, 4126], F32)}
    ocv = D("ocv", [4096, 512], BF16); So = D("So", [2048, 2048], BF16)
    x_mid = D("x_mid", [2048, 2048], F32)
    x1_d = m.dram("x1_d", [2048, 2048], F32)
    h2T_d = m.dram("h2T_d", [128, 16, 2048], BF16)
    identf = make_ident(m, F32)
    identb = m.sb([128, 128], BF16)
    m.op("dve", lambda e: e.tensor_copy(identb[:], identf[:]), reads=[identf], writes=[identb])
    jf = m.sb([128, 128], F32); jmat = m.sb([128, 128], BF16)
    m.op("pool", lambda e: e.memset(jf[:], 1.0), writes=[jf])
    m.op("pool", lambda e: e.affine_select(out=jf[:], in_=jf[:], pattern=[[1, 128]], compare_op=ALU.is_equal, fill=0.0,
                                           base=-127, channel_multiplier=1), reads=[jf], writes=[jf])
    m.op("dve", lambda e: e.tensor_copy(jmat[:], jf[:]), reads=[jf], writes=[jmat])
    sel = m.sb([128, 2], F32)
    m.dma(sel[:], sel_d, writes=[sel])
    Wt = m.sb([128, 16, 32], F32)
    PS = {"z": [m.ps([128, 512], F32) for _ in range(2)],
          "o": [m.ps([128, 512], F32) for _ in range(4)],
          "T": m.ps([128, 8, 128], BF16),
          "misc": m.ps([128, 512], F32)}
    with ExitStack() as es:
        emit_mod(m, nc, es, cT, aw, ab, Gm.src, PS)
    Gm.gather(m, nc)
    modsec = lambda l, s: Gm.dst[2 * (s // 3) + l, (s % 3) * 2048:(s % 3 + 1) * 2048]
    for l in range(depth):
        xin = x_in if l == 0 else x_mid
        xo = x_out if l == depth - 1 else x_mid
        with ExitStack() as es:
            emit_A2(m, nc, es, xin, modsec(l, 0), modsec(l, 1), w_in[l], G1.src, G3.src, G2.src, identb, PS)
        G1.gather(m, nc); G2.gather(m, nc); G3.gather(m, nc)
        with ExitStack() as es:
            emit_select1(m, nc, es, sel, G1, G2, G3, S, identb, jmat, PS)
            barrier(m)
        with ExitStack() as es:
            emit_conv2(m, nc, es, S["aT"], S["gT"], cwT[l], cb[l], G4.src)
        G4.gather(m, nc)
        with ExitStack() as es:
            emit_convln(m, nc, es, G4, lng[l], lnb[l], ocv, identf, PS)
            barrier(m)
        with ExitStack() as es:
            emit_sb(m, nc, es, S["sbqT"].rearrange("(h d) t -> h d t", d=64), S["sbkTr"], S["sbvr"], G5.src[:, 0:256], identb, PSB_fix(PS), 4)
            barrier(m)
        with ExitStack() as es:
            emit_fox(m, nc, es, S["fxqT"].rearrange("(h d) t -> h d t", d=64), S["fxkT"].rearrange("(h d) t -> h d t", d=64),
                     S["fxv"], S["fT"], bfg[l], G5.src[:, 256:768], identf, PS, 8)
        G5.gather(m, nc)
        with ExitStack() as es:
            emit_select2(m, nc, es, sel, G5, ocv, So)
            barrier(m)
        modv4 = [modsec(l, 2), modsec(l, 3), modsec(l, 4), modsec(l, 5)]
        with ExitStack() as es:
            emit_C1(m, nc, es, xin, So, ModV(modv4), w_out[l], ln1g[l], ln1b[l], rw[l], rb[l], x1_d, h2T_d, Wt, identb, identf, PS)
            barrier(m)
        with ExitStack() as es:
            emit_C2(m, nc, es, ModV(modv4), ln2g[l], ln2b[l], wg[l], wu[l], wd[l], x1_d, h2T_d, Wt, xo, PS, n_exp)
            barrier(m)
    m.finish()
    return nc


class ModV:
    def __init__(self, aps):
        self.aps = aps

    def __getitem__(self, idx):
        return self.aps[idx[0]]


def PSB_fix(PS):
    d = dict(PS)
    d["T"] = [PS["T"], PS["T"]]
    return d
import ml_dtypes
from concourse.bass_utils import run_bass_kernel_spmd

_PROGS = {}


def _fused_in_maps(x, c, ada_w, ada_b, w_in, b_forget, conv_w, conv_b, conv_ln_g, conv_ln_b,
                   w_out, ln1_g, ln1_b, r1_w, r1_b, r2_w, r2_b, w_gate, w_up, w_down, ln2_g, ln2_b, n_alloc=32):
    f = lambda a: np.ascontiguousarray(np.asarray(a, dtype=np.float32))
    A = lambda a: np.asarray(a)
    rw = f(np.concatenate([A(r1_w), A(r2_w).transpose(0, 2, 1, 3).reshape(2, 2048, 32)], axis=2))
    rb = f(np.concatenate([A(r1_b), A(r2_b).reshape(2, 32)], axis=1))
    shared = {"w_in": f(w_in), "w_out": f(w_out), "lng": f(conv_ln_g), "lnb": f(conv_ln_b), "ln1g": f(ln1_g), "ln1b": f(ln1_b),
              "ln2g": f(ln2_g), "ln2b": f(ln2_b), "rw": rw, "rb": rb,
              "wg": f(A(w_gate)[:, :n_alloc]), "wu": f(A(w_up)[:, :n_alloc]), "wd": f(A(w_down)[:, :n_alloc])}
    maps = []
    for i in range(8):
        b, j = divmod(i, 2)
        sel = np.zeros((128, 2), np.float32); sel[:, j] = 1.0
        d = dict(shared)
        d["x"] = f(A(x)[b, j * 2048:(j + 1) * 2048])
        d["cT"] = f(A(c)[b].reshape(16, 128).T)
        d["sel"] = sel
        d["aw"] = f(A(ada_w)[:, :, j * 6144:(j + 1) * 6144]); d["ab"] = f(A(ada_b)[:, j * 6144:(j + 1) * 6144])
        d["bfg"] = f(A(b_forget)[:, 8 * j:8 * j + 8].reshape(2, 8, 1))
        d["cwT"] = f(A(conv_w)[:, :, j * 256:(j + 1) * 256].transpose(0, 2, 1).reshape(2, 2, 128, 31).transpose(0, 2, 1, 3))
        d["cb"] = f(A(conv_b)[:, j * 256:(j + 1) * 256].reshape(2, 2, 128).transpose(0, 2, 1))
        maps.append(d)
    return maps


def kernel(**inputs):
    if "F" not in _PROGS:
        _PROGS["F"] = build_F()
    maps = _fused_in_maps(**inputs)
    res = run_bass_kernel_spmd(_PROGS["F"], maps, core_ids=list(range(8)))
    xs = [np.asarray(r["x_out"]) for r in res.results]
    out = np.stack([np.concatenate([xs[2 * b], xs[2 * b + 1]], axis=0) for b in range(4)], axis=0)
    return out.astype(np.float32)
```

```python
D_MODEL = 2048; BATCH = 4; SEQ = 4096; DEPTH = 2
MIX_COLS = 5648
LN_EPS = 1e-5
ALPHA = (2 * DEPTH) ** 0.25
import numpy as np
import concourse.bass as bass
import concourse.mybir as mybir

F32 = mybir.dt.float32
BF16 = mybir.dt.bfloat16
I32 = mybir.dt.int32
ALU = mybir.AluOpType
AF = mybir.ActivationFunctionType
AX = mybir.AxisListType


class Buf:
    __slots__ = ("t", "name", "w", "r")

    def __init__(self, t, name):
        self.t = t
        self.name = name
        self.w = None
        self.r = []

    def __getitem__(self, idx):
        return self.t[idx]


class MK:
    def __init__(self, nc, n_dma_sems=24):
        self.nc = nc
        self.E = {"pe": nc.tensor, "act": nc.scalar, "dve": nc.vector,
                  "pool": nc.gpsimd, "sp": nc.sync}
        self.sem = {k: nc.alloc_semaphore("c_" + k) for k in self.E}
        self.cnt = {k: 0 for k in self.E}
        self.dsem = [nc.alloc_semaphore("d%d" % i) for i in range(n_dma_sems)]
        self.dcnt = [0] * n_dma_sems
        self.dnext = 0
        self.seen = {k: {} for k in self.E}
        self.nbuf = 0
        self.out_tokens = []

    def sb(self, shape, dtype, name=None):
        self.nbuf += 1
        name = name or "b%d" % self.nbuf
        return Buf(self.nc.alloc_sbuf_tensor(name, list(shape), dtype), name)

    def ps(self, shape, dtype, name=None):
        self.nbuf += 1
        name = name or "p%d" % self.nbuf
        return Buf(self.nc.alloc_psum_tensor(name, list(shape), dtype), name)

    def dram(self, name, shape, dtype, kind="Internal"):
        t = self.nc.dram_tensor(name, list(shape), dtype, kind=kind)
        return Buf(t.ap(), name)

    def _semobj(self, key):
        return self.sem[key] if isinstance(key, str) else self.dsem[key]

    def _wait(self, eng, tok):
        if tok is None:
            return
        key, val, _ = tok
        if self.seen[eng].get(key, 0) >= val:
            return
        self.E[eng].wait_ge(self._semobj(key), val)
        self.seen[eng][key] = val

    def _deps(self, eng, reads, writes, pe_accum=False):
        for b in reads:
            self._wait(eng, b.w)
        for b in writes:
            if not (pe_accum and b.w is not None and b.w[2] == "pe" and eng == "pe"):
                self._wait(eng, b.w)
            for tok in b.r:
                self._wait(eng, tok)

    def _commit(self, tok, reads, writes):
        for b in reads:
            b.r.append(tok)
            if len(b.r) > 6:
                d = {}
                for t in b.r:
                    if t[0] not in d or d[t[0]][1] < t[1]:
                        d[t[0]] = t
                b.r = list(d.values())
        for b in writes:
            b.w = tok
            b.r = []

    def op(self, eng, fn, reads=(), writes=(), pe_accum=False):
        self._deps(eng, reads, writes, pe_accum)
        ins = fn(self.E[eng])
        self.cnt[eng] += 1
        ins.then_inc(self.sem[eng], 1)
        tok = (eng, self.cnt[eng], eng)
        self._commit(tok, reads, writes)
        return tok

    def dma(self, out, in_, reads=(), writes=(), eng="sp", is_output=False, **kw):
        i = self.dnext
        self.dnext = (self.dnext + 1) % len(self.dsem)
        if self.dcnt[i] > 0:
            self._wait(eng, (i, self.dcnt[i], "dma"))
        self._deps(eng, reads, writes)
        ins = self.E[eng].dma_start(out=out, in_=in_, **kw)
        self.dcnt[i] += 16
        ins.then_inc(self.dsem[i], 16)
        tok = (i, self.dcnt[i], "dma")
        self._commit(tok, reads, writes)
        if is_output:
            self.out_tokens.append(tok)
        return tok

    def finish(self, eng="sp"):
        for tok in self.out_tokens:
            key, val, _ = tok
            self.E[eng].wait_ge(self._semobj(key), val)
        for i, c in enumerate(self.dcnt):
            if c > 0:
                self.E[eng].wait_ge(self.dsem[i], c)
def build_M():
    nc = bass.Bass("TRN2", target_bir_lowering=False)
    cT = nc.dram_tensor("cT", [128, 16, 4], F32, kind="ExternalInput").ap()
    aw = nc.dram_tensor("aw", [2, 2048, 1536], F32, kind="ExternalInput").ap()
    ab = nc.dram_tensor("ab", [2, 1536], F32, kind="ExternalInput").ap()
    mo = nc.dram_tensor("mo", [2, 4, 1536], F32, kind="ExternalOutput").ap()
    m = MK(nc)
    ct = m.sb([128, 16, 4], F32)
    sc = m.sb([128, 16, 4], F32)
    wst = [m.sb([128, 16, 512], F32) for _ in range(2)]
    bt = [m.sb([4, 512], F32) for _ in range(2)]
    ot = [m.sb([4, 512], F32) for _ in range(2)]
    pp = [m.ps([4, 512], F32) for _ in range(2)]
    m.dma(ct[:], cT, writes=[ct])
    m.op("act", lambda e: e.activation(sc[:], ct[:], AF.Silu), reads=[ct], writes=[sc])
    i = 0
    for l in range(2):
        for n in range(3):
            w = wst[i % 2]; b = bt[i % 2]; o = ot[i % 2]; p = pp[i % 2]
            m.dma(w[:], aw[l, :, n * 512:(n + 1) * 512].rearrange("(k p) n -> p k n", p=128), writes=[w])
            m.dma(b[:], ab[l, n * 512:(n + 1) * 512].partition_broadcast(4), writes=[b])
            for k in range(16):
                m.op("pe", lambda e: e.matmul(p[:], sc[:, k, :], w[:, k, :], start=(k == 0), stop=(k == 15)),
                     reads=[sc, w], writes=[p], pe_accum=True)
            m.op("dve", lambda e: e.tensor_tensor(o[:], p[:], b[:], ALU.add), reads=[p, b], writes=[o])
            m.dma(mo[l, :, n * 512:(n + 1) * 512], o[:], reads=[o], is_output=True)
            i += 1
    m.finish()
    return nc


def make_ident(m, dtype):
    idf = m.sb([128, 128], F32)
    m.op("pool", lambda e: e.memset(idf[:], 1.0), writes=[idf])
    m.op("pool", lambda e: e.affine_select(out=idf[:], in_=idf[:], pattern=[[1, 128]], compare_op=ALU.is_equal,
                                           fill=0.0, base=0, channel_multiplier=-1), reads=[idf], writes=[idf])
    if dtype == F32:
        return idf
    idb = m.sb([128, 128], dtype)
    m.op("dve", lambda e: e.tensor_copy(idb[:], idf[:]), reads=[idf], writes=[idb])
    return idb


def ln_stats(m, xt, st, mv, rstd):
    for q in range(4):
        m.op("dve", lambda e: e.bn_stats(st[:, q * 6:(q + 1) * 6], xt[:, q * 512:(q + 1) * 512]), reads=[xt], writes=[st])
    m.op("dve", lambda e: e.bn_aggr(mv[:], st[:]), reads=[st], writes=[mv])
    m.op("dve", lambda e: e.tensor_scalar_add(rstd[:], mv[:, 1:2], LN_EPS), reads=[mv], writes=[rstd])
    m.op("act", lambda e: e.activation(rstd[:], rstd[:], AF.Sqrt), reads=[rstd], writes=[rstd])
    m.op("dve", lambda e: e.reciprocal(rstd[:], rstd[:]), reads=[rstd], writes=[rstd])


FM_BLOCKS = [(0, "qk", 0), (256, "qk", 256), (512, "qk", 512), (768, "qk", 768),
             (1536, "qk", 1024), (1792, "qk", 1280), (2048, "qk", 1536), (2304, "qk", 1792),
             (2560, "qk", 2048), (2816, "qk", 2304), (3072, "qk", 2560), (3328, "qk", 2816),
             (4608, "fag", 0), (4864, "fag", 256), (5120, "fag", 512), (5376, "fag", 768)]
TM_BLOCKS = [(1024, 0), (1280, 256), (3584, 512), (3840, 768), (4096, 1024), (4352, 1280)]


def emit_A(m, nc, x, modv, w_in, qkT, fagT, vtm, ident):
    NT = 16
    hT = m.sb([128, 16, 2048], BF16, "hT")
    scb = m.sb([128, 2048], F32, "scb")
    shb = m.sb([128, 2048], F32, "shb")
    xts = [m.sb([128, 2048], F32) for _ in range(2)]
    hb = [m.sb([128, 2048], BF16) for _ in range(2)]
    st = m.sb([128, 24], F32); mv = m.sb([128, 2], F32); rstd = m.sb([128, 1], F32)
    pT = [m.ps([128, 8, 128], BF16) for _ in range(2)]
    m.dma(shb[:], modv[0, :].partition_broadcast(128), writes=[shb])
    m.dma(scb[:], modv[1, :].partition_broadcast(128), writes=[scb])
    m.op("pool", lambda e: e.tensor_scalar_add(scb[:], scb[:], 1.0), reads=[scb], writes=[scb])
    m.dma(xts[0][:], x[0:128, :], writes=[xts[0]])
    for t in range(NT):
        xt = xts[t % 2]; h = hb[t % 2]
        if t + 1 < NT:
            m.dma(xts[(t + 1) % 2][:], x[(t + 1) * 128:(t + 2) * 128, :], writes=[xts[(t + 1) % 2]])
        ln_stats(m, xt, st, mv, rstd)
        m.op("dve", lambda e: e.tensor_scalar(xt[:], xt[:], mv[:, 0:1], rstd[:, 0:1], ALU.subtract, ALU.mult),
             reads=[xt, mv, rstd], writes=[xt])
        m.op("pool", lambda e: e.tensor_tensor(xt[:], xt[:], scb[:], ALU.mult), reads=[xt, scb], writes=[xt])
        m.op("dve", lambda e: e.tensor_tensor(h[:], xt[:], shb[:], ALU.add), reads=[xt, shb], writes=[h])
        for half in range(2):
            p = pT[half]
            for j in range(8):
                k = half * 8 + j
                m.op("pe", lambda e: e.transpose(p[:, j, :], h[:, k * 128:(k + 1) * 128], ident[:]),
                     reads=[h, ident], writes=[p], pe_accum=True)
            m.op("act", lambda e: e.copy(hT[:, half * 8:(half + 1) * 8, t * 128:(t + 1) * 128], p[:]),
                 reads=[p], writes=[hT])
    wst = [m.sb([128, 16, 256], F32) for _ in range(2)]
    wbf = [m.sb([128, 16, 256], BF16) for _ in range(2)]
    pm = [m.ps([128, 512], F32) for _ in range(4)]
    oq = [m.sb([128, 2048], BF16) for _ in range(2)]
    of = [m.sb([128, 2048], F32) for _ in range(2)]
    ov = [m.sb([128, 256], BF16) for _ in range(2)]
    blocks = [("fm", c0, kind, o0) for (c0, kind, o0) in FM_BLOCKS] + [("tm", c0, None, o0) for (c0, o0) in TM_BLOCKS]
    blocks.append(("fm16", 5632, "fag", 1024))
    pi = 0; oi = 0
    def load_w(bi):
        typ, c0, kind, o0 = blocks[bi]
        cw = 16 if typ == "fm16" else 256
        m.dma(wst[bi % 2][:, :, 0:cw], w_in[:, c0:c0 + cw].rearrange("(k p) n -> p k n", p=128), writes=[wst[bi % 2]])
    load_w(0)
    for bi, (typ, c0, kind, o0) in enumerate(blocks):
        cw = 16 if typ == "fm16" else 256
        ws = wst[bi % 2]; wb = wbf[bi % 2]
        if bi + 1 < len(blocks):
            load_w(bi + 1)
        m.op("act", lambda e: e.copy(wb[:, 0:8, 0:cw], ws[:, 0:8, 0:cw]), reads=[ws], writes=[wb])
        m.op("pool", lambda e: e.tensor_copy(wb[:, 8:16, 0:cw], ws[:, 8:16, 0:cw]), reads=[ws], writes=[wb])
        if typ in ("fm", "fm16"):
            for cc in range(0, cw, 128):
                cn = min(128, cw - cc)
                o = (oq if kind == "qk" else of)[oi % 2]; oi += 1
                for tg in range(4):
                    p = pm[pi % 4]; pi += 1
                    for k in range(16):
                        m.op("pe", lambda e: e.matmul(p[0:cn, :], wb[:, k, cc:cc + cn], hT[:, k, tg * 512:(tg + 1) * 512],
                                                      start=(k == 0), stop=(k == 15)),
                             reads=[wb, hT], writes=[p], pe_accum=True)
                    if tg % 2 == 0:
                        m.op("act", lambda e: e.copy(o[0:cn, tg * 512:(tg + 1) * 512], p[0:cn, :]), reads=[p], writes=[o])
                    else:
                        m.op("dve", lambda e: e.tensor_copy(o[0:cn, tg * 512:(tg + 1) * 512], p[0:cn, :]), reads=[p], writes=[o])
                dst = qkT if kind == "qk" else fagT
                m.dma(dst[o0 + cc:o0 + cc + cn, :], o[0:cn, :], reads=[o], is_output=True)
        else:
            for tt in range(16):
                p = pm[pi % 4]; pi += 1
                o = ov[oi % 2]; oi += 1
                for k in range(16):
                    m.op("pe", lambda e: e.matmul(p[:, 0:256], hT[:, k, tt * 128:(tt + 1) * 128], wb[:, k, :],
                                                  start=(k == 0), stop=(k == 15)),
                         reads=[wb, hT], writes=[p], pe_accum=True)
                if tt % 2 == 0:
                    m.op("act", lambda e: e.copy(o[:], p[:, 0:256]), reads=[p], writes=[o])
                else:
                    m.op("dve", lambda e: e.tensor_copy(o[:], p[:, 0:256]), reads=[p], writes=[o])
                m.dma(vtm[tt * 128:(tt + 1) * 128, o0:o0 + 256], o[:], reads=[o], is_output=True)


def build_A():
    nc = bass.Bass("TRN2", target_bir_lowering=False)
    x = nc.dram_tensor("x", [2048, 2048], F32, kind="ExternalInput").ap()
    modv = nc.dram_tensor("modv", [2, 2048], F32, kind="ExternalInput").ap()
    w_in = nc.dram_tensor("w_in", [2048, MIX_COLS], F32, kind="ExternalInput").ap()
    qkT = nc.dram_tensor("qkT", [3072, 2048], BF16, kind="ExternalOutput").ap()
    fagT = nc.dram_tensor("fagT", [1040, 2048], F32, kind="ExternalOutput").ap()
    vtm = nc.dram_tensor("vtm", [2048, 1536], BF16, kind="ExternalOutput").ap()
    m = MK(nc)
    ident = make_ident(m, BF16)
    emit_A(m, nc, x, modv, w_in, qkT, fagT, vtm, ident)
    m.finish()
    return nc
def barrier(m):
    for e in m.E:
        for f in m.E:
            if m.cnt[f] > 0:
                m._wait(e, (f, m.cnt[f], f))
        for i, c in enumerate(m.dcnt):
            if c > 0:
                m._wait(e, (i, c, "dma"))


def emit_conv(m, nc, es, aT, gT, cwT, cb, lng, lnb, o_cv, identf, PS):
    def sb(shape, dt):
        m.nbuf += 1
        return Buf(es.enter_context(nc.sbuf_tensor("cv%d" % m.nbuf, list(shape), dt)), "cv")
    cvT = sb([128, 4, 2048], F32)
    at = [sb([128, 2078], F32) for _ in range(2)]
    gt = [sb([128, 2078], F32) for _ in range(2)]
    cw = sb([128, 4, 31], F32); cbt = sb([128, 4], F32)
    lg = sb([128, 512], F32); lb = sb([128, 512], F32)
    m.dma(cw[:], cwT, writes=[cw])
    m.dma(cbt[:], cb, writes=[cbt])
    m.dma(lg[:], lng.partition_broadcast(128), writes=[lg])
    m.dma(lb[:], lnb.partition_broadcast(128), writes=[lb])
    for cc in range(4):
        a = at[cc % 2]; g = gt[cc % 2]
        m.dma(a[:], aT[cc * 128:(cc + 1) * 128, :], writes=[a])
        m.dma(g[:], gT[cc * 128:(cc + 1) * 128, :], writes=[g])
        m.op("act", lambda e: e.activation(g[:], g[:], AF.Sigmoid), reads=[g], writes=[g])
        m.op("dve", lambda e: e.tensor_tensor(a[:], a[:], g[:], ALU.mult), reads=[a, g], writes=[a])
        m.op("dve", lambda e: e.tensor_scalar(cvT[:, cc, :], a[:, 0:2048], cw[:, cc, 0:1], cbt[:, cc:cc + 1], ALU.mult, ALU.add),
             reads=[a, cw, cbt], writes=[cvT])
        for w in range(1, 31):
            m.op("dve", lambda e: e.scalar_tensor_tensor(cvT[:, cc, :], a[:, w:w + 2048], cw[:, cc, w:w + 1], cvT[:, cc, :], ALU.mult, ALU.add),
                 reads=[a, cw, cvT], writes=[cvT])
    pt = PS["z"]
    ut = [sb([128, 512], F32) for _ in range(2)]
    ob = [sb([128, 512], BF16) for _ in range(2)]
    st = sb([128, 6], F32); mv = sb([128, 2], F32); rstd = sb([128, 1], F32)
    for t in range(16):
        p = pt[t % 2]; u = ut[t % 2]; o = ob[t % 2]
        for cc in range(4):
            m.op("pe", lambda e: e.transpose(p[:, cc * 128:(cc + 1) * 128], cvT[:, cc, t * 128:(t + 1) * 128], identf[:]),
                 reads=[cvT, identf], writes=[p], pe_accum=True)
        m.op("act", lambda e: e.copy(u[:], p[:]), reads=[p], writes=[u])
        m.op("dve", lambda e: e.bn_stats(st[:], u[:]), reads=[u], writes=[st])
        m.op("dve", lambda e: e.bn_aggr(mv[:], st[:]), reads=[st], writes=[mv])
        m.op("dve", lambda e: e.tensor_scalar_add(rstd[:], mv[:, 1:2], LN_EPS), reads=[mv], writes=[rstd])
        m.op("act", lambda e: e.activation(rstd[:], rstd[:], AF.Sqrt), reads=[rstd], writes=[rstd])
        m.op("dve", lambda e: e.reciprocal(rstd[:], rstd[:]), reads=[rstd], writes=[rstd])
        m.op("dve", lambda e: e.tensor_scalar(u[:], u[:], mv[:, 0:1], rstd[:, 0:1], ALU.subtract, ALU.mult),
             reads=[u, mv, rstd], writes=[u])
        m.op("pool", lambda e: e.tensor_tensor(u[:], u[:], lg[:], ALU.mult), reads=[u, lg], writes=[u])
        m.op("dve", lambda e: e.tensor_tensor(u[:], u[:], lb[:], ALU.add), reads=[u, lb], writes=[u])
        m.op("act", lambda e: e.activation(o[:], u[:], AF.Silu), reads=[u], writes=[o])
        m.dma(o_cv[t * 128:(t + 1) * 128, :], o[:], reads=[o], is_output=True)


def run_pipeline(n, stages):
    S = len(stages)
    for step in range(n + S - 1):
        for st in range(S - 1, -1, -1):
            i = step - st
            if 0 <= i < n:
                stages[st](i)


def emit_sb(m, nc, es, sbqT, sbkTr, sbvr, o_sb, identb, PS, nheads=4):
    def sb(shape, dt):
        m.nbuf += 1
        return Buf(es.enter_context(nc.sbuf_tensor("sb%d" % m.nbuf, list(shape), dt)), "sb")
    QT = [sb([64, 4096], BF16) for _ in range(2)]
    KT = [sb([64, 4096], BF16) for _ in range(2)]
    VR = [sb([128, 32, 64], BF16) for _ in range(2)]
    ones = sb([128, 512], F32)
    m.op("pool", lambda e: e.memset(ones[:], 1.0), writes=[ones])
    NB = 4
    e_t = [sb([128, 512], F32) for _ in range(NB)]
    sp_t = [sb([128, 512], F32) for _ in range(NB)]
    r_t = [sb([128, 512], F32) for _ in range(NB)]
    la_t = [sb([128, 512], F32) for _ in range(NB)]
    a_t = [sb([128, 512], BF16) for _ in range(NB)]
    aT_t = [sb([128, 4, 128], BF16) for _ in range(2)]
    ost = [sb([128, 32, 64], BF16) for _ in range(2)]
    pzs = list(PS["z"]) + [PS["misc"]]
    NZ = len(pzs)
    pT2 = [Buf(PS["T"][0].t[:, 0:4, :], "pTa"), Buf(PS["T"][0].t[:, 4:8, :], "pTb")]
    po = PS["o"][0]

    def load(h):
        m.dma(QT[h % 2][:], sbqT[h], writes=[QT[h % 2]])
        m.dma(KT[h % 2][:], sbkTr[h], writes=[KT[h % 2]])
        m.dma(VR[h % 2][:], sbvr[:, h * 64:(h + 1) * 64].rearrange("(c p) d -> p c d", p=128), writes=[VR[h % 2]])
    items = []
    for h in range(nheads):
        for i in range(32):
            nch = (i + 1 + 3) // 4
            for c in range(nch):
                items.append((h, i, c, nch))
    load(0)

    def geom(ix):
        h, i, c, nch = items[ix]
        c0 = 128 * (31 - i) + 512 * c
        w = min(512, 4096 - c0)
        return h, i, c, nch, c0, w

    def s0(ix):
        h, i, c, nch, c0, w = geom(ix)
        q = QT[h % 2]; k = KT[h % 2]
        z = pzs[ix % NZ]; et = e_t[ix % NB]; spt = sp_t[ix % NB]
        m.op("pe", lambda e: e.matmul(z[:, 0:w], q[:, i * 128:(i + 1) * 128], k[:, c0:c0 + w], start=True, stop=True),
             reads=[q, k], writes=[z])
        m.op("act", lambda e: e.activation(et[:, 0:w], z[:, 0:w], AF.Exp, scale=0.125), reads=[z], writes=[et])
        m.op("act", lambda e: e.activation(spt[:, 0:w], et[:, 0:w], AF.Ln, bias=1.0), reads=[et], writes=[spt])
        if c == 0:
            m.op("pool", lambda e: e.affine_select(out=spt[:, 0:128], in_=spt[:, 0:128], pattern=[[1, 128]],
                                                   compare_op=ALU.is_gt, fill=0.0, base=-127, channel_multiplier=1),
                 reads=[spt], writes=[spt])

    def s1(ix):
        h, i, c, nch, c0, w = geom(ix)
        z = pzs[ix % NZ]; spt = sp_t[ix % NB]; rt = r_t[ix % NB]; lat = la_t[ix % NB]
        if c == 0:
            init = 0.0; rd = [ones, spt]
        else:
            prt = r_t[(ix - 1) % NB]
            init = prt[:, 511:512]; rd = [ones, spt, prt]
        m.op("dve", lambda e: e.tensor_tensor_scan(rt[:, 0:w], ones[:, 0:w], spt[:, 0:w], init, ALU.mult, ALU.add),
             reads=rd, writes=[rt])
        m.op("dve", lambda e: e.scalar_tensor_tensor(lat[:, 0:w], z[:, 0:w], 0.125, rt[:, 0:w], ALU.mult, ALU.subtract),
             reads=[z, rt], writes=[lat])

    def s2(ix):
        h, i, c, nch, c0, w = geom(ix)
        lat = la_t[ix % NB]; at_ = a_t[ix % NB]
        m.op("act", lambda e: e.activation(at_[:, 0:w], lat[:, 0:w], AF.Exp), reads=[lat], writes=[at_])
        if c == 0:
            m.op("pool", lambda e: e.affine_select(out=at_[:, 0:128], in_=at_[:, 0:128], pattern=[[1, 128]],
                                                   compare_op=ALU.is_gt, fill=0.0, base=-127, channel_multiplier=1),
                 reads=[at_], writes=[at_])

    def s3(ix):
        h, i, c, nch, c0, w = geom(ix)
        nb = w // 128
        v = VR[h % 2]; os_ = ost[h % 2]
        at_ = a_t[ix % NB]; aTt = aT_t[ix % 2]
        pTb = pT2[ix % 2]
        if i == 0 and c == 0 and h + 1 < nheads:
            load(h + 1)
        for bb in range(nb):
            m.op("pe", lambda e: e.transpose(pTb[:, bb, :], at_[:, bb * 128:(bb + 1) * 128], identb[:]),
                 reads=[at_, identb], writes=[pTb], pe_accum=True)
        m.op("dve", lambda e: e.tensor_copy(aTt[:, 0:nb, :], pTb[:, 0:nb, :]), reads=[pTb], writes=[aTt])
        for bb in range(nb):
            kb = (c0 // 128) + bb
            first = (c == 0 and bb == 0); last = (c == nch - 1 and bb == nb - 1)
            m.op("pe", lambda e: e.matmul(po[:, 0:64], aTt[:, bb, :], v[:, kb, :], start=first, stop=last),
                 reads=[aTt, v], writes=[po], pe_accum=True)
        if c == nch - 1:
            m.op("act", lambda e: e.copy(os_[:, i, :], po[:, 0:64]), reads=[po], writes=[os_])
            if i == 31:
                m.dma(o_sb[:, h * 64:(h + 1) * 64].rearrange("(i p) d -> p i d", p=128), os_[:], reads=[os_], is_output=True)
    run_pipeline(len(items), [s0, s1, s2, s3])


def emit_fox(m, nc, es, fxqT, fxkT, fxv, fT, negb, o_fx, identf, PS, nheads=8):
    def sb(shape, dt):
        m.nbuf += 1
        return Buf(es.enter_context(nc.sbuf_tensor("fx%d" % m.nbuf, list(shape), dt)), "fx")
    NH = nheads
    C = sb([NH, 4096], F32); W1 = sb([NH, 4096], F32); nb_t = sb([NH, 1], F32)
    onesr = sb([NH, 4096], F32)
    m.dma(W1[:], fT[0:NH, :], writes=[W1])
    m.dma(nb_t[:], negb[0:NH, :], writes=[nb_t])
    m.op("pool", lambda e: e.memset(onesr[:], 1.0), writes=[onesr])
    m.op("dve", lambda e: e.tensor_scalar_mul(nb_t[:], nb_t[:], -1.0), reads=[nb_t], writes=[nb_t])
    m.op("act", lambda e: e.activation(W1[:], W1[:], AF.Exp, bias=nb_t[:, 0:1], scale=-1.0), reads=[W1, nb_t], writes=[W1])
    m.op("act", lambda e: e.activation(W1[:], W1[:], AF.Ln, bias=1.0), reads=[W1], writes=[W1])
    m.op("dve", lambda e: e.tensor_tensor_scan(C[:], onesr[:], W1[:], 0.0, ALU.mult, ALU.add), reads=[onesr, W1], writes=[C])
    CkT = sb([128, 32, NH], F32)
    pc = PS["misc"]
    for j in range(32):
        m.op("pe", lambda e: e.transpose(pc[:, j * NH:(j + 1) * NH], C[:, j * 128:(j + 1) * 128], identf[0:NH, 0:NH]),
             reads=[C, identf], writes=[pc], pe_accum=True)
    m.op("dve", lambda e: e.tensor_copy(CkT[:].rearrange("p j h -> p (j h)"), pc[:, 0:32 * NH]), reads=[pc], writes=[CkT])
    Rd = sb([NH, NH, 8], F32)
    Cg = C[:].rearrange("h (g q) -> h g q", q=512)
    m.op("dve", lambda e: e.tensor_tensor(Rd[:], Cg[:, :, 511:512].rearrange("h g o -> h o g").to_broadcast([NH, NH, 8]),
                                          identf[0:NH, 0:NH].unsqueeze(2).to_broadcast([NH, NH, 8]), ALU.mult),
         reads=[C, identf], writes=[Rd])
    onesk = sb([NH, 128], F32)
    m.op("pool", lambda e: e.memset(onesk[:], 1.0), writes=[onesk])
    m.op("pe", lambda e: e.matmul(pc[:, 256:256 + NH * 8], onesk[:], Rd[:].rearrange("h a g -> h (a g)"), start=True, stop=True),
         reads=[onesk, Rd, CkT], writes=[pc])
    Rbc = sb([128, NH, 8], F32)
    m.op("dve", lambda e: e.tensor_copy(Rbc[:].rearrange("p h g -> p (h g)"), pc[:, 256:256 + NH * 8]), reads=[pc], writes=[Rbc])
    bias = sb([128, NH, 8, 32], F32)
    for h in range(NH):
        for g in range(8):
            nj = 4 * g + 4
            m.op("dve", lambda e: e.tensor_scalar(bias[:, h, g, 0:nj], CkT[:, 0:nj, h], Rbc[:, h, g:g + 1], None, ALU.subtract),
                 reads=[CkT, Rbc], writes=[bias])
    D8 = sb([NH, 4096], F32); Dhi = sb([NH, 4096], BF16); Dlo = sb([NH, 4096], BF16)
    D8g = D8[:].rearrange("h (g q) -> h g q", q=512)
    m.op("dve", lambda e: e.tensor_tensor(D8g, Cg[:, :, 511:512].to_broadcast([NH, 8, 512]), Cg, ALU.subtract),
         reads=[C], writes=[D8])
    m.op("dve", lambda e: e.tensor_scalar_mul(D8[:], D8[:], 8.0), reads=[D8], writes=[D8])
    m.op("dve", lambda e: e.tensor_copy(Dhi[:], D8[:]), reads=[D8], writes=[Dhi])
    m.op("dve", lambda e: e.tensor_tensor(D8[:], D8[:], Dhi[:], ALU.subtract), reads=[D8, Dhi], writes=[D8])
    m.op("dve", lambda e: e.tensor_copy(Dlo[:], D8[:]), reads=[D8], writes=[Dlo])
    QT = [sb([66, 4096], BF16) for _ in range(2)]
    KT = [sb([66, 4096], BF16) for _ in range(2)]
    V = [sb([128, 32, 65], BF16) for _ in range(2)]
    for i in range(2):
        m.op("pool", lambda e: e.memset(KT[i][64:66, :], 1.0), writes=[KT[i]])
        m.op("pool", lambda e: e.memset(V[i][:, :, 64:65], 1.0), writes=[V[i]])
    NP = 4
    P_t = [sb([128, 512], BF16) for _ in range(NP)]
    ost = [sb([128, 32, 64], BF16) for _ in range(2)]
    rc = sb([128, 4], F32)
    pzs = list(PS["z"]) + [PS["misc"]]
    NZ = len(pzs)
    po = PS["o"]

    def load(h):
        m.dma(QT[h % 2][0:64, :], fxqT[h], writes=[QT[h % 2]])
        m.dma(QT[h % 2][64:65, :], Dhi[h:h + 1, :], reads=[Dhi], writes=[QT[h % 2]])
        m.dma(QT[h % 2][65:66, :], Dlo[h:h + 1, :], reads=[Dlo], writes=[QT[h % 2]])
        m.dma(KT[h % 2][0:64, :], fxkT[h], writes=[KT[h % 2]])
        m.dma(V[h % 2][:, :, 0:64], fxv[:, h * 64:(h + 1) * 64].rearrange("(c p) d -> p c d", p=128), writes=[V[h % 2]])
    load(0)
    items = [(h, g, j) for h in range(NH) for g in range(8) for j in range(4 * g + 4)]

    def geom(ix):
        h, g, j = items[ix]
        md = j - 4 * g
        q0 = 128 * max(0, md)
        return h, g, j, md, q0, 512 - q0

    def s0(ix):
        h, g, j, md, q0, w = geom(ix)
        q = QT[h % 2]; k = KT[h % 2]; z = pzs[ix % NZ]
        m.op("pe", lambda e: e.matmul(z[:, 0:w], k[:, j * 128:(j + 1) * 128], q[:, 512 * g + q0:512 * g + 512], start=True, stop=True),
             reads=[q, k], writes=[z])

    def s1(ix):
        h, g, j, md, q0, w = geom(ix)
        z = pzs[ix % NZ]; P = P_t[ix % NP]
        m.op("act", lambda e: e.activation(P[:, 0:w], z[:, 0:w], AF.Exp, bias=bias[:, h, g, j:j + 1], scale=0.125),
             reads=[z, bias], writes=[P])
        if md >= 0:
            m.op("pool", lambda e: e.affine_select(out=P[:, 0:128], in_=P[:, 0:128], pattern=[[1, 128]],
                                                   compare_op=ALU.is_ge, fill=0.0, base=0, channel_multiplier=-1),
                 reads=[P], writes=[P])

    def s2(ix):
        h, g, j, md, q0, w = geom(ix)
        v = V[h % 2]; os_ = ost[h % 2]; P = P_t[ix % NP]
        if g == 0 and j == 0 and h + 1 < NH:
            load(h + 1)
        for s_ in range(max(0, md), 4):
            lc = (s_ * 128) - q0
            m.op("pe", lambda e: e.matmul(po[s_][:, 0:65], P[:, lc:lc + 128], v[:, j, :], start=(j == 0), stop=(j == 4 * g + s_)),
                 reads=[P, v], writes=[po[s_]], pe_accum=True)
        if j == 4 * g + 3:
            for s_ in range(4):
                m.op("dve", lambda e: e.reciprocal(rc[:, s_:s_ + 1], po[s_][:, 64:65]), reads=[po[s_]], writes=[rc])
                m.op("dve", lambda e: e.tensor_scalar(os_[:, 4 * g + s_, :], po[s_][:, 0:64], rc[:, s_:s_ + 1], None, ALU.mult),
                     reads=[po[s_], rc], writes=[os_])
            if g == 7:
                m.dma(o_fx[:, h * 64:(h + 1) * 64].rearrange("(i p) d -> p i d", p=128), os_[:], reads=[os_], is_output=True)
    run_pipeline(len(items), [s0, s1, s2])


def build_B(do_conv=True, n_sb=4, n_fx=8):
    from contextlib import ExitStack
    nc = bass.Bass("TRN2", target_bir_lowering=False)
    dt = lambda n, s, d, k="ExternalInput": nc.dram_tensor(n, s, d, kind=k).ap()
    sbqT = dt("sbqT", [4, 64, 4096], BF16); sbkTr = dt("sbkTr", [4, 64, 4096], BF16); sbvr = dt("sbvr", [4096, 256], BF16)
    fxqT = dt("fxqT", [8, 64, 4096], BF16); fxkT = dt("fxkT", [8, 64, 4096], BF16); fxv = dt("fxv", [4096, 512], BF16)
    fT = dt("fT", [8, 4096], F32); negb = dt("bfg", [8, 1], F32)
    aT = dt("aT", [512, 2078], F32); gT = dt("gT", [512, 2078], F32)
    cwT = dt("cwT", [128, 4, 31], F32); cb = dt("cb", [128, 4], F32); lng = dt("lng", [512], F32); lnb = dt("lnb", [512], F32)
    o_sb = dt("o_sb", [4096, 256], BF16, "ExternalOutput"); o_fx = dt("o_fx", [4096, 512], BF16, "ExternalOutput")
    o_cv = dt("o_cv", [2048, 512], BF16, "ExternalOutput")
    m = MK(nc)
    identf = make_ident(m, F32)
    identb = m.sb([128, 128], BF16)
    m.op("dve", lambda e: e.tensor_copy(identb[:], identf[:]), reads=[identf], writes=[identb])
    PS = {"z": [m.ps([128, 512], F32) for _ in range(2)],
          "o": [m.ps([128, 512], F32) for _ in range(4)],
          "T": [m.ps([128, 8, 128], BF16)] * 2,
          "misc": m.ps([128, 512], F32)}
    if do_conv:
        with ExitStack() as es:
            emit_conv(m, nc, es, aT, gT, cwT, cb, lng, lnb, o_cv, identf, PS)
            barrier(m)
    if n_sb:
        with ExitStack() as es:
            emit_sb(m, nc, es, sbqT, sbkTr, sbvr, o_sb, identb, PS, n_sb)
            barrier(m)
    if n_fx:
        with ExitStack() as es:
            emit_fox(m, nc, es, fxqT, fxkT, fxv, fT, negb, o_fx, identf, PS, n_fx)
            barrier(m)
    m.finish()
    return nc
def ln_affine(m, r, st, mv, rstd, g_bc, b_bc, out):
    ln_stats(m, r, st, mv, rstd)
    m.op("dve", lambda e: e.tensor_scalar(r[:], r[:], mv[:, 0:1], rstd[:, 0:1], ALU.subtract, ALU.mult),
         reads=[r, mv, rstd], writes=[r])
    m.op("pool", lambda e: e.tensor_tensor(r[:], r[:], g_bc[:], ALU.mult), reads=[r, g_bc], writes=[r])
    m.op("dve", lambda e: e.tensor_tensor(out[:], r[:], b_bc[:], ALU.add), reads=[r, b_bc], writes=[out])


def emit_C1(m, nc, es, x, o, modv, w_out, ln1g, ln1b, rw, rb, x1_d, h2T_d, Wt, identb, identf, PS):
    def sb(shape, dt):
        m.nbuf += 1
        return Buf(es.enter_context(nc.sbuf_tensor("c1_%d" % m.nbuf, list(shape), dt)), "c1")
    wob = sb([128, 16, 2048], BF16)
    wst = [sb([128, 2048], F32) for _ in range(2)]
    bc = {}
    for nm, src in (("gt1", modv[0, :]), ("sh2", modv[1, :]), ("sc2", modv[2, :]), ("l1g", ln1g), ("l1b", ln1b)):
        bc[nm] = sb([128, 2048], F32)
        m.dma(bc[nm][:], src.partition_broadcast(128), writes=[bc[nm]])
    m.op("pool", lambda e: e.tensor_scalar_add(bc["sc2"][:], bc["sc2"][:], 1.0), reads=[bc["sc2"]], writes=[bc["sc2"]])
    rwt = sb([128, 16, 36], F32); rbt = sb([128, 36], F32)
    m.dma(rwt[:], rw.rearrange("(k p) n -> p k n", p=128), writes=[rwt])
    m.dma(rbt[:], rb.partition_broadcast(128), writes=[rbt])
    m.dma(wst[0][:], w_out[0:128, :], writes=[wst[0]])
    for k in range(16):
        if k + 1 < 16:
            m.dma(wst[(k + 1) % 2][:], w_out[(k + 1) * 128:(k + 2) * 128, :], writes=[wst[(k + 1) % 2]])
        if k % 2 == 0:
            m.op("act", lambda e: e.copy(wob[:, k, :], wst[k % 2][:]), reads=[wst[k % 2]], writes=[wob])
        else:
            m.op("pool", lambda e: e.tensor_copy(wob[:, k, :], wst[k % 2][:]), reads=[wst[k % 2]], writes=[wob])
    ot = [sb([128, 2048], BF16) for _ in range(2)]
    xt = [sb([128, 2048], F32) for _ in range(2)]
    oTs = [sb([128, 16, 128], BF16) for _ in range(2)]
    tmps = [sb([128, 2048], F32) for _ in range(2)]; x1 = sb([128, 2048], F32); h2f = sb([128, 2048], F32); h2b = sb([128, 2048], BF16)
    h2T = sb([128, 16, 128], BF16); h2fT = sb([128, 16, 128], F32)
    st = sb([128, 24], F32); mv = sb([128, 2], F32); rstd = sb([128, 1], F32)
    lg = sb([128, 36], F32)
    sm = {k: sb([128, n], F32) for k, n in (("m1", 1), ("nm1", 1), ("ohg", 4), ("e1", 4), ("s1", 1), ("pg", 1), ("t48", 32),
                                           ("sel", 8), ("m2a", 1), ("nm2a", 1), ("oh1", 8), ("sel2", 8), ("m2b", 1), ("oh2", 8),
                                           ("r", 1), ("den", 1), ("w1", 1), ("w2", 1), ("wsel", 8))}
    pT = PS["T"]; py = PS["o"]; pz = PS["z"]; pm = PS["misc"]

    def load(t):
        m.dma(ot[t % 2][:], o[t * 128:(t + 1) * 128, :], writes=[ot[t % 2]])
        m.dma(xt[t % 2][:], x[t * 128:(t + 1) * 128, :], writes=[xt[t % 2]])
    load(0)

    def stage_a(t):
        if t + 1 < 16:
            load(t + 1)
        ob = ot[t % 2]; xx = xt[t % 2]; oT = oTs[t % 2]; tmp = tmps[t % 2]
        for half in range(2):
            for j in range(8):
                kk = half * 8 + j
                m.op("pe", lambda e: e.transpose(pT[:, j, :], ob[:, kk * 128:(kk + 1) * 128], identb[:]),
                     reads=[ob, identb], writes=[pT], pe_accum=True)
            m.op("act", lambda e: e.copy(oT[:, half * 8:(half + 1) * 8, :], pT[:]), reads=[pT], writes=[oT])
        for n in range(4):
            p = py[n]
            for k in range(16):
                m.op("pe", lambda e: e.matmul(p[:], oT[:, k, :], wob[:, k, n * 512:(n + 1) * 512], start=(k == 0), stop=(k == 15)),
                     reads=[oT, wob], writes=[p], pe_accum=True)
            m.op("dve", lambda e: e.tensor_tensor(tmp[:, n * 512:(n + 1) * 512], p[:], bc["gt1"][:, n * 512:(n + 1) * 512], ALU.mult),
                 reads=[p, bc["gt1"]], writes=[tmp])
        m.op("dve", lambda e: e.scalar_tensor_tensor(tmp[:], xx[:], ALPHA, tmp[:], ALU.mult, ALU.add), reads=[xx, tmp], writes=[tmp])

    def stage_b(t):
        tmp = tmps[t % 2]
        ln_affine(m, tmp, st, mv, rstd, bc["l1g"], bc["l1b"], x1)
        m.dma(x1_d[t * 128:(t + 1) * 128, :], x1[:], reads=[x1], writes=[x1_d])
        ln_stats(m, x1, st, mv, rstd)
        m.op("dve", lambda e: e.tensor_scalar(h2f[:], x1[:], mv[:, 0:1], rstd[:, 0:1], ALU.subtract, ALU.mult),
             reads=[x1, mv, rstd], writes=[h2f])
        m.op("pool", lambda e: e.tensor_tensor(h2f[:], h2f[:], bc["sc2"][:], ALU.mult), reads=[h2f, bc["sc2"]], writes=[h2f])
        m.op("dve", lambda e: e.tensor_tensor(h2f[:], h2f[:], bc["sh2"][:], ALU.add), reads=[h2f, bc["sh2"]], writes=[h2f])
        m.op("act", lambda e: e.copy(h2b[:], h2f[:]), reads=[h2f], writes=[h2b])
        for half in range(2):
            for j in range(8):
                kk = half * 8 + j
                m.op("pe", lambda e: e.transpose(pT[:, j, :], h2b[:, kk * 128:(kk + 1) * 128], identb[:]),
                     reads=[h2b, identb], writes=[pT], pe_accum=True)
            m.op("act", lambda e: e.copy(h2T[:, half * 8:(half + 1) * 8, :], pT[:]), reads=[pT], writes=[h2T])
        m.dma(h2T_d[:, :, t * 128:(t + 1) * 128], h2T[:], reads=[h2T], writes=[h2T_d])
        for q4 in range(4):
            p = pz[q4 % 2]
            for j in range(4):
                kk = q4 * 4 + j
                m.op("pe", lambda e: e.transpose(p[:, j * 128:(j + 1) * 128], h2f[:, kk * 128:(kk + 1) * 128], identf[:]),
                     reads=[h2f, identf], writes=[p], pe_accum=True)
            m.op("act", lambda e: e.copy(h2fT[:, q4 * 4:(q4 + 1) * 4, :].rearrange("p a b -> p (a b)"), p[:]), reads=[p], writes=[h2fT])
        for k in range(16):
            m.op("pe", lambda e: e.matmul(pm[:, 0:36], h2fT[:, k, :], rwt[:, k, :], start=(k == 0), stop=(k == 15)),
                 reads=[h2fT, rwt], writes=[pm], pe_accum=True)
        m.op("dve", lambda e: e.tensor_tensor(lg[:], pm[:, 0:36], rbt[:], ALU.add), reads=[pm, rbt], writes=[lg])
        S = sm
        def dv(fn, r, w):
            m.op("dve", fn, reads=r, writes=w)
        dv(lambda e: e.reduce_max(S["m1"][:], lg[:, 0:4], AX.X), [lg], [S["m1"]])
        dv(lambda e: e.tensor_scalar(S["ohg"][:], lg[:, 0:4], S["m1"][:, 0:1], None, ALU.is_equal), [lg, S["m1"]], [S["ohg"]])
        dv(lambda e: e.tensor_scalar_mul(S["nm1"][:], S["m1"][:], -1.0), [S["m1"]], [S["nm1"]])
        m.op("act", lambda e: e.activation(S["e1"][:], lg[:, 0:4], AF.Exp, bias=S["nm1"][:, 0:1], accum_out=S["s1"][:, 0:1]),
             reads=[lg, S["nm1"]], writes=[S["e1"], S["s1"]])
        dv(lambda e: e.reciprocal(S["pg"][:], S["s1"][:]), [S["s1"]], [S["pg"]])
        l2 = lg[:, 4:36].rearrange("p (g e) -> p g e", g=4)
        t48 = S["t48"][:].rearrange("p (g e) -> p g e", g=4)
        dv(lambda e: e.tensor_tensor(t48, l2, S["ohg"][:].unsqueeze(2).to_broadcast([128, 4, 8]), ALU.mult), [lg, S["ohg"]], [S["t48"]])
        dv(lambda e: e.tensor_reduce(S["sel"][:], S["t48"][:].rearrange("p (g e) -> p e g", g=4), AX.X, ALU.add), [S["t48"]], [S["sel"]])
        dv(lambda e: e.reduce_max(S["m2a"][:], S["sel"][:], AX.X), [S["sel"]], [S["m2a"]])
        dv(lambda e: e.tensor_scalar(S["oh1"][:], S["sel"][:], S["m2a"][:, 0:1], None, ALU.is_equal), [S["sel"], S["m2a"]], [S["oh1"]])
        dv(lambda e: e.scalar_tensor_tensor(S["sel2"][:], S["oh1"][:], -1e30, S["sel"][:], ALU.mult, ALU.add), [S["oh1"], S["sel"]], [S["sel2"]])
        dv(lambda e: e.reduce_max(S["m2b"][:], S["sel2"][:], AX.X), [S["sel2"]], [S["m2b"]])
        dv(lambda e: e.tensor_scalar(S["oh2"][:], S["sel2"][:], S["m2b"][:, 0:1], None, ALU.is_equal), [S["sel2"], S["m2b"]], [S["oh2"]])
        dv(lambda e: e.tensor_scalar_mul(S["nm2a"][:], S["m2a"][:], -1.0), [S["m2a"]], [S["nm2a"]])
        m.op("act", lambda e: e.activation(S["r"][:], S["m2b"][:], AF.Exp, bias=S["nm2a"][:, 0:1]), reads=[S["m2b"], S["nm2a"]], writes=[S["r"]])
        dv(lambda e: e.tensor_scalar_add(S["den"][:], S["r"][:], 1.0), [S["r"]], [S["den"]])
        dv(lambda e: e.reciprocal(S["den"][:], S["den"][:]), [S["den"]], [S["den"]])
        dv(lambda e: e.tensor_tensor(S["w1"][:], S["pg"][:], S["den"][:], ALU.mult), [S["pg"], S["den"]], [S["w1"]])
        dv(lambda e: e.tensor_tensor(S["w2"][:], S["w1"][:], S["r"][:], ALU.mult), [S["w1"], S["r"]], [S["w2"]])
        dv(lambda e: e.tensor_scalar(S["wsel"][:], S["oh1"][:], S["w1"][:, 0:1], None, ALU.mult), [S["oh1"], S["w1"]], [S["wsel"]])
        dv(lambda e: e.scalar_tensor_tensor(S["wsel"][:], S["oh2"][:], S["w2"][:, 0:1], S["wsel"][:], ALU.mult, ALU.add),
           [S["oh2"], S["w2"], S["wsel"]], [S["wsel"]])
        dv(lambda e: e.tensor_tensor(Wt[:, t, :].rearrange("p (g e) -> p g e", g=4), S["ohg"][:].unsqueeze(2).to_broadcast([128, 4, 8]),
                                     S["wsel"][:].unsqueeze(1).to_broadcast([128, 4, 8]), ALU.mult), [S["ohg"], S["wsel"]], [Wt])

    stage_a(0)
    for t in range(16):
        if t + 1 < 16:
            stage_a(t + 1)
        stage_b(t)


def emit_C2(m, nc, es, modv, ln2g, ln2b, wg, wu, wd, x1_d, h2T_d, Wt, x_out, PS, n_exp=32):
    from contextlib import ExitStack

    def mk_sb(stack, tag):
        def sb(shape, dt):
            m.nbuf += 1
            return Buf(stack.enter_context(nc.sbuf_tensor("%s_%d" % (tag, m.nbuf), list(shape), dt)), tag)
        return sb
    sbo = mk_sb(es, "c2o")
    NTOK = 1024; NTT = NTOK // 128; NTG = NTOK // 512
    h2T = sbo([128, 16, NTOK], BF16)
    yacc = sbo([128, NTT, 2048], F32)
    gbanks = PS["z"]; ubanks = PS["o"][0:2]; pyb = PS["o"][2:4]
    for hp in range(2048 // NTOK):
        m.dma(h2T[:], h2T_d[:, :, hp * NTOK:(hp + 1) * NTOK], reads=[h2T_d], writes=[h2T])
        with ExitStack() as es2:
            sb = mk_sb(es2, "c2w")
            hidT = sb([128, 8, NTOK], BF16)
            gst = sb([128, 16, 128], F32); ust = sb([128, 16, 128], F32)
            wgb = [sb([128, 16, 128], BF16) for _ in range(2)]
            wub = [sb([128, 16, 128], BF16) for _ in range(2)]
            dst = sb([128, 2048], F32)
            wdb = sb([128, 8, 2048], BF16)
            sg = [sb([128, 512], F32) for _ in range(2)]
            items = [(e, fc) for e in range(n_exp) for fc in range(8)]

            def load_gu(ix):
                e, fc = items[ix]
                m.dma(gst[:], wg[e, :, fc * 128:(fc + 1) * 128].rearrange("(k p) n -> p k n", p=128), writes=[gst])
                m.dma(ust[:], wu[e, :, fc * 128:(fc + 1) * 128].rearrange("(k p) n -> p k n", p=128), writes=[ust])
                m.dma(dst[:], wd[e, fc * 128:(fc + 1) * 128, :], writes=[dst])

            def convert(ix):
                e, fc = items[ix]
                m.op("act", lambda en: en.copy(wgb[ix % 2][:], gst[:]), reads=[gst], writes=[wgb[ix % 2]])
                m.op("pool", lambda en: en.tensor_copy(wub[ix % 2][:], ust[:]), reads=[ust], writes=[wub[ix % 2]])
            load_gu(0)
            convert(0)
            si = 0
            for ix, (e, fc) in enumerate(items):
                gb = wgb[ix % 2]; ub = wub[ix % 2]
                if fc == 0:
                    pass
                m.op("pool", lambda en: en.tensor_copy(wdb[:, fc, :], dst[:]), reads=[dst], writes=[wdb])
                if ix + 1 < len(items):
                    load_gu(ix + 1)
                for tg in range(NTG):
                    a = gbanks[tg]; b = ubanks[tg]; s_ = sg[si % 2]; si += 1
                    for k in range(16):
                        m.op("pe", lambda en: en.matmul(a[:], gb[:, k, :], h2T[:, k, tg * 512:(tg + 1) * 512], start=(k == 0), stop=(k == 15)),
                             reads=[gb, h2T], writes=[a], pe_accum=True)
                    for k in range(16):
                        m.op("pe", lambda en: en.matmul(b[:], ub[:, k, :], h2T[:, k, tg * 512:(tg + 1) * 512], start=(k == 0), stop=(k == 15)),
                             reads=[ub, h2T], writes=[b], pe_accum=True)
                    m.op("act", lambda en: en.activation(s_[:], a[:], AF.Silu), reads=[a], writes=[s_])
                    m.op("dve", lambda en: en.tensor_tensor(hidT[:, fc, tg * 512:(tg + 1) * 512], s_[:], b[:], ALU.mult),
                         reads=[s_, b], writes=[hidT])
                if ix + 1 < len(items):
                    convert(ix + 1)
                if fc == 7:
                    for tt in range(NTT):
                        tile_i = hp * NTT + tt
                        for n in range(4):
                            p = pyb[(tt * 4 + n) % 2]
                            for f2 in range(8):
                                m.op("pe", lambda en: en.matmul(p[:], hidT[:, f2, tt * 128:(tt + 1) * 128], wdb[:, f2, n * 512:(n + 1) * 512],
                                                                start=(f2 == 0), stop=(f2 == 7)),
                                     reads=[hidT, wdb], writes=[p], pe_accum=True)
                            ys = yacc[:, tt, n * 512:(n + 1) * 512]
                            if e == 0:
                                m.op("dve", lambda en: en.tensor_scalar(ys, p[:], Wt[:, tile_i, e:e + 1], None, ALU.mult),
                                     reads=[p, Wt], writes=[yacc])
                            else:
                                m.op("dve", lambda en: en.scalar_tensor_tensor(ys, p[:], Wt[:, tile_i, e:e + 1], ys, ALU.mult, ALU.add),
                                     reads=[p, Wt, yacc], writes=[yacc])
            barrier(m)
        with ExitStack() as es3:
            sb = mk_sb(es3, "c2e")
            bc = {}
            for nm, src in (("gt2", modv[3, :]), ("l2g", ln2g), ("l2b", ln2b)):
                bc[nm] = sb([128, 2048], F32)
                m.dma(bc[nm][:], src.partition_broadcast(128), writes=[bc[nm]])
            xt = [sb([128, 2048], F32) for _ in range(2)]; outt = [sb([128, 2048], F32) for _ in range(2)]
            st = sb([128, 24], F32); mv = sb([128, 2], F32); rstd = sb([128, 1], F32)
            for tt in range(NTT):
                tile_i = hp * NTT + tt
                x_ = xt[tt % 2]; o_ = outt[tt % 2]
                m.dma(x_[:], x1_d[tile_i * 128:(tile_i + 1) * 128, :], reads=[x1_d], writes=[x_])
                m.op("pool", lambda en: en.tensor_tensor(yacc[:, tt, :], yacc[:, tt, :], bc["gt2"][:], ALU.mult), reads=[yacc, bc["gt2"]], writes=[yacc])
                m.op("dve", lambda en: en.scalar_tensor_tensor(x_[:], x_[:], ALPHA, yacc[:, tt, :], ALU.mult, ALU.add), reads=[x_, yacc], writes=[x_])
                ln_affine(m, x_, st, mv, rstd, bc["l2g"], bc["l2b"], o_)
                m.dma(x_out[tile_i * 128:(tile_i + 1) * 128, :], o_[:], reads=[o_], is_output=True)
            barrier(m)


def build_C(n_exp=32, n_alloc=32):
    from contextlib import ExitStack
    nc = bass.Bass("TRN2", target_bir_lowering=False)
    dt = lambda n, s, d, k="ExternalInput": nc.dram_tensor(n, s, d, kind=k).ap()
    x = dt("x", [2048, 2048], F32); o = dt("o", [2048, 2048], BF16); modv = dt("modv", [4, 2048], F32)
    w_out = dt("w_out", [2048, 2048], F32)
    ln1g = dt("ln1g", [2048], F32); ln1b = dt("ln1b", [2048], F32); ln2g = dt("ln2g", [2048], F32); ln2b = dt("ln2b", [2048], F32)
    rw = dt("rw", [2048, 36], F32); rb = dt("rb", [36], F32)
    wg = dt("wg", [n_alloc, 2048, 1024], F32); wu = dt("wu", [n_alloc, 2048, 1024], F32); wd = dt("wd", [n_alloc, 1024, 2048], F32)
    x_out = dt("x_out", [2048, 2048], F32, "ExternalOutput")
    m = MK(nc)
    x1_d = m.dram("x1_d", [2048, 2048], F32)
    h2T_d = m.dram("h2T_d", [128, 16, 2048], BF16)
    identf = make_ident(m, F32)
    identb = m.sb([128, 128], BF16)
    m.op("dve", lambda e: e.tensor_copy(identb[:], identf[:]), reads=[identf], writes=[identb])
    Wt = m.sb([128, 16, 32], F32)
    PS = {"z": [m.ps([128, 512], F32) for _ in range(2)],
          "o": [m.ps([128, 512], F32) for _ in range(4)],
          "T": m.ps([128, 8, 128], BF16),
          "misc": m.ps([128, 512], F32)}
    with ExitStack() as es:
        emit_C1(m, nc, es, x, o, modv, w_out, ln1g, ln1b, rw, rb, x1_d, h2T_d, Wt, identb, identf, PS)
        barrier(m)
    with ExitStack() as es:
        emit_C2(m, nc, es, modv, ln2g, ln2b, wg, wu, wd, x1_d, h2T_d, Wt, x_out, PS, n_exp)
        barrier(m)
    m.finish()
    return nc
def cc_allgather(m, nc, src_ap, dst_ap):
    barrier(m)
    if "cc" not in m.sem:
        m.sem["cc"] = nc.alloc_semaphore("ccsem"); m.cnt["cc"] = 0
    ins = nc.gpsimd.collective_compute("AllGather", ALU.bypass, replica_groups=[[0, 1], [2, 3], [4, 5], [6, 7]],
                                       ins=[src_ap.opt()], outs=[dst_ap.opt()])
    m.cnt["cc"] += 1
    ins.then_inc(m.sem["cc"], 1)
    for e in m.E:
        m.E[e].wait_ge(m.sem["cc"], m.cnt["cc"])


class GB:
    def __init__(self, nc, name, rows, cols, dt, rows_k):
        self.nk = (rows + rows_k - 1) // rows_k; self.rk = rows_k
        self.src = nc.dram_tensor(name, [self.nk * rows_k, cols], dt, kind="Internal").ap()
        self.dst = nc.dram_tensor(name + "g", [self.nk * 2 * rows_k, cols], dt, kind="Internal").ap()

    def gather(self, m, nc):
        rk = self.rk
        for k in range(self.nk):
            cc_allgather(m, nc, self.src[k * rk:(k + 1) * rk, :], self.dst[k * 2 * rk:(k + 1) * 2 * rk, :])

    def g(self, r, lo, hi):
        rk = self.rk
        k = lo // rk
        assert (hi - 1) // rk == k, (lo, hi, rk)
        base = k * 2 * rk + r * rk + (lo - k * rk)
        return self.dst[base:base + (hi - lo), :]


FM_BLOCKS_F = ([(c0, "qk", o0) for c0, o0 in zip(range(0, 512, 256), range(0, 512, 256))] +
               [(c0, "qk", o0) for c0, o0 in zip(range(1536, 3584, 256), range(512, 2560, 256))] +
               [(c0, "fag", o0) for c0, o0 in zip(range(4624, 5648, 256), range(0, 1024, 256))])
TM_BLOCKS_F = ([(c0, o0) for c0, o0 in zip(range(512, 1536, 256), range(0, 1024, 256))] +
               [(c0, o0) for c0, o0 in zip(range(3584, 4608, 256), range(1024, 2048, 256))])


def emit_A2(m, nc, es, x, sh_ap, sc_ap, w_in, qkT, fagT, vtm, ident, PS):
    def sb(shape, dt):
        m.nbuf += 1
        return Buf(es.enter_context(nc.sbuf_tensor("a_%d" % m.nbuf, list(shape), dt)), "a")
    NT = 16
    hT = sb([128, 16, 2048], BF16)
    scb = sb([128, 2048], F32); shb = sb([128, 2048], F32)
    xts = [sb([128, 2048], F32) for _ in range(2)]
    hb = [sb([128, 2048], BF16) for _ in range(2)]
    st = sb([128, 24], F32); mv = sb([128, 2], F32); rstd = sb([128, 1], F32)
    pT = PS["T"]
    m.dma(shb[:], sh_ap.partition_broadcast(128), writes=[shb])
    m.dma(scb[:], sc_ap.partition_broadcast(128), writes=[scb])
    m.op("pool", lambda e: e.tensor_scalar_add(scb[:], scb[:], 1.0), reads=[scb], writes=[scb])
    m.dma(xts[0][:], x[0:128, :], writes=[xts[0]])
    for t in range(NT):
        xt = xts[t % 2]; h = hb[t % 2]
        if t + 1 < NT:
            m.dma(xts[(t + 1) % 2][:], x[(t + 1) * 128:(t + 2) * 128, :], writes=[xts[(t + 1) % 2]])
        ln_stats(m, xt, st, mv, rstd)
        m.op("dve", lambda e: e.tensor_scalar(xt[:], xt[:], mv[:, 0:1], rstd[:, 0:1], ALU.subtract, ALU.mult),
             reads=[xt, mv, rstd], writes=[xt])
        m.op("pool", lambda e: e.tensor_tensor(xt[:], xt[:], scb[:], ALU.mult), reads=[xt, scb], writes=[xt])
        m.op("dve", lambda e: e.tensor_tensor(h[:], xt[:], shb[:], ALU.add), reads=[xt, shb], writes=[h])
        for half in range(2):
            for j in range(8):
                k = half * 8 + j
                m.op("pe", lambda e: e.transpose(pT[:, j, :], h[:, k * 128:(k + 1) * 128], ident[:]),
                     reads=[h, ident], writes=[pT], pe_accum=True)
            m.op("act", lambda e: e.copy(hT[:, half * 8:(half + 1) * 8, t * 128:(t + 1) * 128], pT[:]),
                 reads=[pT], writes=[hT])
    wst = [sb([128, 16, 256], F32) for _ in range(2)]
    wbf = [sb([128, 16, 256], BF16) for _ in range(2)]
    pm = PS["o"]
    oq = [sb([128, 2048], BF16) for _ in range(2)]
    of = [sb([128, 2048], F32) for _ in range(2)]
    ov = [sb([128, 256], BF16) for _ in range(2)]
    blocks = [("fm", c0, kind, o0) for (c0, kind, o0) in FM_BLOCKS_F] + [("tm", c0, None, o0) for (c0, o0) in TM_BLOCKS_F]
    blocks.append(("fm16", 4608, "fag", 1024))
    pi = 0; oi = 0

    def load_w(bi):
        typ, c0, kind, o0 = blocks[bi]
        cw = 16 if typ == "fm16" else 256
        m.dma(wst[bi % 2][:, :, 0:cw], w_in[:, c0:c0 + cw].rearrange("(k p) n -> p k n", p=128), writes=[wst[bi % 2]])
    load_w(0)
    for bi, (typ, c0, kind, o0) in enumerate(blocks):
        cw = 16 if typ == "fm16" else 256
        ws = wst[bi % 2]; wb = wbf[bi % 2]
        if bi + 1 < len(blocks):
            load_w(bi + 1)
        m.op("act", lambda e: e.copy(wb[:, 0:8, 0:cw], ws[:, 0:8, 0:cw]), reads=[ws], writes=[wb])
        m.op("pool", lambda e: e.tensor_copy(wb[:, 8:16, 0:cw], ws[:, 8:16, 0:cw]), reads=[ws], writes=[wb])
        if typ in ("fm", "fm16"):
            for cc in range(0, cw, 128):
                cn = min(128, cw - cc)
                o = (oq if kind == "qk" else of)[oi % 2]; oi += 1
                for tg in range(4):
                    p = pm[pi % 4]; pi += 1
                    for k in range(16):
                        m.op("pe", lambda e: e.matmul(p[0:cn, :], wb[:, k, cc:cc + cn], hT[:, k, tg * 512:(tg + 1) * 512],
                                                      start=(k == 0), stop=(k == 15)),
                             reads=[wb, hT], writes=[p], pe_accum=True)
                    if tg % 2 == 0:
                        m.op("act", lambda e: e.copy(o[0:cn, tg * 512:(tg + 1) * 512], p[0:cn, :]), reads=[p], writes=[o])
                    else:
                        m.op("dve", lambda e: e.tensor_copy(o[0:cn, tg * 512:(tg + 1) * 512], p[0:cn, :]), reads=[p], writes=[o])
                dst = qkT if kind == "qk" else fagT
                m.dma(dst[o0 + cc:o0 + cc + cn, :], o[0:cn, :], reads=[o])
        else:
            for tt in range(16):
                p = pm[pi % 4]; pi += 1
                o = ov[oi % 2]; oi += 1
                for k in range(16):
                    m.op("pe", lambda e: e.matmul(p[:, 0:256], hT[:, k, tt * 128:(tt + 1) * 128], wb[:, k, :],
                                                  start=(k == 0), stop=(k == 15)),
                         reads=[wb, hT], writes=[p], pe_accum=True)
                if tt % 2 == 0:
                    m.op("act", lambda e: e.copy(o[:], p[:, 0:256]), reads=[p], writes=[o])
                else:
                    m.op("dve", lambda e: e.tensor_copy(o[:], p[:, 0:256]), reads=[p], writes=[o])
                m.dma(vtm[tt * 128:(tt + 1) * 128, o0:o0 + 256], o[:], reads=[o])


def blend_tiles(m, sb_pool, sel, dst_fn, srcA_fn, srcB_fn, ntiles, shape, dt, post=None):
    ta, tb = (sb_pool["a"], sb_pool["b"]) if dt == BF16 else (sb_pool["a32"], sb_pool["b32"])
    np_ = shape[0]
    NBUF = len(ta); PF = NBUF - 1

    def views(i):
        a = ta[i % NBUF]; b = tb[i % NBUF]
        return a, b, a[0:np_, 0:shape[1]], b[0:np_, 0:shape[1]]

    def load(i):
        a, b, av, bv = views(i)
        m.dma(av, srcA_fn(i), writes=[a])
        m.dma(bv, srcB_fn(i), writes=[b])
    for i in range(min(PF, ntiles)):
        load(i)
    for i in range(ntiles):
        if i + PF < ntiles:
            load(i + PF)
        a, b, av, bv = views(i)
        m.op("dve", lambda e: e.tensor_scalar(av, av, sel[0:np_, 0:1], None, ALU.mult), reads=[a, sel], writes=[a])
        m.op("dve", lambda e: e.scalar_tensor_tensor(av, bv, sel[0:np_, 1:2], av, ALU.mult, ALU.add), reads=[a, b, sel], writes=[a])
        if post is None:
            m.dma(dst_fn(i), av, reads=[a])
        else:
            post(i, a, av)


def emit_select1(m, nc, es, sel, G1, G2, G3, S, identb, jmat, PS):
    def sb(shape, dt):
        m.nbuf += 1
        return Buf(es.enter_context(nc.sbuf_tensor("s_%d" % m.nbuf, list(shape), dt)), "s")
    pool = {"a": [sb([128, 2048], BF16) for _ in range(4)], "b": [sb([128, 2048], BF16) for _ in range(4)],
            "a32": [sb([128, 2048], F32) for _ in range(3)], "b32": [sb([128, 2048], F32) for _ in range(3)]}
    for r in range(2):
        tk = slice(r * 2048, (r + 1) * 2048)
        for (dst, a0, b0, nrows) in ((S["sbqT"], 0, 256, 256), (S["fxqT"], 512, 1024, 512), (S["fxkT"], 1536, 2048, 512)):
            blend_tiles(m, pool, sel, lambda i: dst[i * 128:(i + 1) * 128, tk], lambda i: G1.g(r, a0 + i * 128, a0 + (i + 1) * 128),
                        lambda i: G1.g(r, b0 + i * 128, b0 + (i + 1) * 128), nrows // 128, [128, 2048], BF16)
        blend_tiles(m, pool, sel, lambda i: S["fxv"][r * 2048 + i * 128:r * 2048 + (i + 1) * 128, :],
                    lambda i: G2.g(r, i * 128, (i + 1) * 128)[:, 1024:1536], lambda i: G2.g(r, i * 128, (i + 1) * 128)[:, 1536:2048], 16, [128, 512], BF16)
        blend_tiles(m, pool, sel, lambda i: S["fT"][:, tk], lambda i: G3.g(r, 1024, 1032), lambda i: G3.g(r, 1032, 1040), 1, [8, 2048], F32)
        blend_tiles(m, pool, sel, lambda i: S["aT"][i * 128:(i + 1) * 128, 30 + r * 2048:30 + (r + 1) * 2048],
                    lambda i: G3.g(r, i * 128, (i + 1) * 128), lambda i: G3.g(r, 256 + i * 128, 256 + (i + 1) * 128), 2, [128, 2048], F32)
        blend_tiles(m, pool, sel, lambda i: S["gT"][i * 128:(i + 1) * 128, 30 + r * 2048:30 + (r + 1) * 2048],
                    lambda i: G3.g(r, 512 + i * 128, 512 + (i + 1) * 128), lambda i: G3.g(r, 768 + i * 128, 768 + (i + 1) * 128), 2, [128, 2048], F32)
        pk = PS["z"]; pv = PS["misc"]
        ko = [sb([64, 4, 128], BF16) for _ in range(2)]
        vo = [sb([128, 256], BF16) for _ in range(2)]

        def post_k(i, a, av):
            gt = r * 16 + i; rb = 31 - gt
            p = pk[i % 2]; o = ko[i % 2]
            for h in range(4):
                m.op("pe", lambda e: e.matmul(p[0:64, h * 128:(h + 1) * 128], a[:, h * 64:(h + 1) * 64], jmat[:], start=True, stop=True),
                     reads=[a, jmat], writes=[p], pe_accum=True)
            m.op("act", lambda e: e.copy(o[:].rearrange("p h t -> p (h t)"), p[0:64, 0:512]), reads=[p], writes=[o])
            m.dma(S["sbkTr"][:, :, rb * 128:(rb + 1) * 128].rearrange("h d t -> d h t"), o[:], reads=[o])

        def post_v(i, a, av):
            gt = r * 16 + i; rb = 31 - gt
            o = vo[i % 2]
            m.op("pe", lambda e: e.matmul(pv[:, 0:256], jmat[:], a[:, 0:256], start=True, stop=True), reads=[a, jmat], writes=[pv])
            m.op("act", lambda e: e.copy(o[:], pv[:, 0:256]), reads=[pv], writes=[o])
            m.dma(S["sbvr"][rb * 128:(rb + 1) * 128, :], o[:], reads=[o])
        blend_tiles(m, pool, sel, None, lambda i: G2.g(r, i * 128, (i + 1) * 128)[:, 0:256], lambda i: G2.g(r, i * 128, (i + 1) * 128)[:, 256:512],
                    16, [128, 256], BF16, post=post_k)
        blend_tiles(m, pool, sel, None, lambda i: G2.g(r, i * 128, (i + 1) * 128)[:, 512:768], lambda i: G2.g(r, i * 128, (i + 1) * 128)[:, 768:1024],
                    16, [128, 256], BF16, post=post_v)
    z = sb([128, 30], F32)
    m.op("pool", lambda e: e.memset(z[:], 0.0), writes=[z])
    for i in range(2):
        m.dma(S["aT"][i * 128:(i + 1) * 128, 0:30], z[:], reads=[z])
        m.dma(S["gT"][i * 128:(i + 1) * 128, 0:30], z[:], reads=[z])


def emit_select2(m, nc, es, sel, G5, ocv, So):
    def sb(shape, dt):
        m.nbuf += 1
        return Buf(es.enter_context(nc.sbuf_tensor("s2_%d" % m.nbuf, list(shape), dt)), "s2")
    pool = {"a": [sb([128, 512], BF16) for _ in range(4)], "b": [sb([128, 512], BF16) for _ in range(4)]}
    rows = lambda hf, i: (hf * 2048 + i * 128, hf * 2048 + (i + 1) * 128)
    for (src_fn, c0, cw) in ((lambda hf, i: G5.g(0, *rows(hf, i))[:, 0:256], 0, 256), (lambda hf, i: G5.g(1, *rows(hf, i))[:, 0:256], 256, 256),
                             (lambda hf, i: G5.g(0, *rows(hf, i))[:, 256:768], 512, 512), (lambda hf, i: G5.g(1, *rows(hf, i))[:, 256:768], 1024, 512),
                             (lambda hf, i: ocv[rows(hf, i)[0]:rows(hf, i)[1], :], 1536, 512)):
        blend_tiles(m, pool, sel, lambda i: So[i * 128:(i + 1) * 128, c0:c0 + cw], lambda i: src_fn(0, i), lambda i: src_fn(1, i),
                    16, [128, cw], BF16)


def emit_conv2(m, nc, es, aT, gT, cwT, cb, G4):
    def sb(shape, dt):
        m.nbuf += 1
        return Buf(es.enter_context(nc.sbuf_tensor("cv%d" % m.nbuf, list(shape), dt)), "cv")
    a = sb([128, 4126], F32); g = sb([128, 4126], F32); acc = sb([128, 4096], F32)
    cw = sb([128, 2, 31], F32); cbt = sb([128, 2], F32)
    m.dma(cw[:], cwT, writes=[cw])
    m.dma(cbt[:], cb, writes=[cbt])
    for cc in range(2):
        m.dma(a[:], aT[cc * 128:(cc + 1) * 128, :], writes=[a])
        m.dma(g[:], gT[cc * 128:(cc + 1) * 128, :], writes=[g])
        m.op("act", lambda e: e.activation(g[:], g[:], AF.Sigmoid), reads=[g], writes=[g])
        m.op("dve", lambda e: e.tensor_tensor(a[:], a[:], g[:], ALU.mult), reads=[a, g], writes=[a])
        m.op("dve", lambda e: e.tensor_scalar(acc[:], a[:, 0:4096], cw[:, cc, 0:1], cbt[:, cc:cc + 1], ALU.mult, ALU.add),
             reads=[a, cw, cbt], writes=[acc])
        for w in range(1, 31):
            m.op("dve", lambda e: e.scalar_tensor_tensor(acc[:], a[:, w:w + 4096], cw[:, cc, w:w + 1], acc[:], ALU.mult, ALU.add),
                 reads=[a, cw, acc], writes=[acc])
        m.dma(G4[cc * 128:(cc + 1) * 128, :], acc[:], reads=[acc])


def emit_convln(m, nc, es, G4, lng, lnb, o_cv, identf, PS):
    def sb(shape, dt):
        m.nbuf += 1
        return Buf(es.enter_context(nc.sbuf_tensor("cl%d" % m.nbuf, list(shape), dt)), "cl")
    cvT = sb([128, 4, 4096], F32)
    lg = sb([128, 512], F32); lb = sb([128, 512], F32)
    m.dma(lg[:], lng.partition_broadcast(128), writes=[lg])
    m.dma(lb[:], lnb.partition_broadcast(128), writes=[lb])
    for cc in range(4):
        m.dma(cvT[:, cc, :], G4.g(cc // 2, (cc % 2) * 128, (cc % 2 + 1) * 128), writes=[cvT])
    pt = PS["z"]
    ut = [sb([128, 512], F32) for _ in range(2)]
    ob = [sb([128, 512], BF16) for _ in range(2)]
    st = sb([128, 6], F32); mv = sb([128, 2], F32); rstd = sb([128, 1], F32)
    for t in range(32):
        p = pt[t % 2]; u = ut[t % 2]; o = ob[t % 2]
        for cc in range(4):
            m.op("pe", lambda e: e.transpose(p[:, cc * 128:(cc + 1) * 128], cvT[:, cc, t * 128:(t + 1) * 128], identf[:]),
                 reads=[cvT, identf], writes=[p], pe_accum=True)
        m.op("act", lambda e: e.copy(u[:], p[:]), reads=[p], writes=[u])
        m.op("dve", lambda e: e.bn_stats(st[:], u[:]), reads=[u], writes=[st])
        m.op("dve", lambda e: e.bn_aggr(mv[:], st[:]), reads=[st], writes=[mv])
        m.op("dve", lambda e: e.tensor_scalar_add(rstd[:], mv[:, 1:2], LN_EPS), reads=[mv], writes=[rstd])
        m.op("act", lambda e: e.activation(rstd[:], rstd[:], AF.Sqrt), reads=[rstd], writes=[rstd])
        m.op("dve", lambda e: e.reciprocal(rstd[:], rstd[:]), reads=[rstd], writes=[rstd])
        m.op("dve", lambda e: e.tensor_scalar(u[:], u[:], mv[:, 0:1], rstd[:, 0:1], ALU.subtract, ALU.mult),
             reads=[u, mv, rstd], writes=[u])
        m.op("pool", lambda e: e.tensor_tensor(u[:], u[:], lg[:], ALU.mult), reads=[u, lg], writes=[u])
        m.op("dve", lambda e: e.tensor_tensor(u[:], u[:], lb[:], ALU.add), reads=[u, lb], writes=[u])
        m.op("act", lambda e: e.activation(o[:], u[:], AF.Silu), reads=[u], writes=[o])
        m.dma(o_cv[t * 128:(t + 1) * 128, :], o[:], reads=[o])


def emit_mod(m, nc, es, cT, aw, ab, Gm, PS):
    def sb(shape, dt):
        m.nbuf += 1
        return Buf(es.enter_context(nc.sbuf_tensor("mo%d" % m.nbuf, list(shape), dt)), "mo")
    ct = sb([128, 16], F32); sc = sb([128, 16], F32)
    wst = [sb([128, 16, 512], F32) for _ in range(2)]
    bt = [sb([1, 512], F32) for _ in range(2)]
    ot = [sb([1, 512], F32) for _ in range(2)]
    pp = PS["z"]
    m.dma(ct[:], cT, writes=[ct])
    m.op("act", lambda e: e.activation(sc[:], ct[:], AF.Silu), reads=[ct], writes=[sc])
    items = [(l, n) for l in range(2) for n in range(12)]

    def load(i):
        l, n = items[i]
        m.dma(wst[i % 2][:], aw[l, :, n * 512:(n + 1) * 512].rearrange("(k p) n -> p k n", p=128), writes=[wst[i % 2]])
        m.dma(bt[i % 2][:], ab[l:l + 1, n * 512:(n + 1) * 512], writes=[bt[i % 2]])
    load(0)
    for i, (l, n) in enumerate(items):
        if i + 1 < len(items):
            load(i + 1)
        w = wst[i % 2]; b = bt[i % 2]; o = ot[i % 2]; p = pp[i % 2]
        for k in range(16):
            m.op("pe", lambda e: e.matmul(p[0:1, :], sc[:, k:k + 1], w[:, k, :], start=(k == 0), stop=(k == 15)),
                 reads=[sc, w], writes=[p], pe_accum=True)
        m.op("dve", lambda e: e.tensor_tensor(o[:], p[0:1, :], b[:], ALU.add), reads=[p, b], writes=[o])
        m.dma(Gm[l:l + 1, n * 512:(n + 1) * 512], o[:], reads=[o])


def build_F(n_exp=32, depth=DEPTH, n_alloc=32):
    from contextlib import ExitStack
    nc = bass.Bass("TRN2", target_bir_lowering=False)
    dt = lambda n, s, d, k="ExternalInput": nc.dram_tensor(n, s, d, kind=k).ap()
    x_in = dt("x", [2048, 2048], F32); cT = dt("cT", [128, 16], F32); sel_d = dt("sel", [128, 2], F32)
    aw = dt("aw", [2, 2048, 6144], F32); ab = dt("ab", [2, 6144], F32)
    w_in = dt("w_in", [2, 2048, MIX_COLS], F32); w_out = dt("w_out", [2, 2048, 2048], F32)
    bfg = dt("bfg", [2, 8, 1], F32); cwT = dt("cwT", [2, 128, 2, 31], F32); cb = dt("cb", [2, 128, 2], F32)
    lng = dt("lng", [2, 512], F32); lnb = dt("lnb", [2, 512], F32)
    ln1g = dt("ln1g", [2, 2048], F32); ln1b = dt("ln1b", [2, 2048], F32); ln2g = dt("ln2g", [2, 2048], F32); ln2b = dt("ln2b", [2, 2048], F32)
    rw = dt("rw", [2, 2048, 36], F32); rb = dt("rb", [2, 36], F32)
    wg = dt("wg", [2, n_alloc, 2048, 1024], F32); wu = dt("wu", [2, n_alloc, 2048, 1024], F32); wd = dt("wd", [2, n_alloc, 1024, 2048], F32)
    x_out = dt("x_out", [2048, 2048], F32, "ExternalOutput")
    m = MK(nc)
    D = lambda n, s, d: nc.dram_tensor(n, list(s), d, kind="Internal").ap()
    Gm = GB(nc, "Gm", 2, 6144, F32, 2)
    G1 = GB(nc, "G1", 2560, 2048, BF16, 512); G2 = GB(nc, "G2", 2048, 2048, BF16, 512)
    G3 = GB(nc, "G3", 1040, 2048, F32, 256); G4 = GB(nc, "G4", 256, 4096, F32, 128); G5 = GB(nc, "G5", 4096, 768, BF16, 1024)
    S = {"sbqT": D("S_sbqT", [256, 4096], BF16), "fxqT": D("S_fxqT", [512, 4096], BF16), "fxkT": D("S_fxkT", [512, 4096], BF16),
         "sbkTr": D("S_sbkTr", [4, 64, 4096], BF16), "sbvr": D("S_sbvr", [4096, 256], BF16), "fxv": D("S_fxv", [4096, 512], BF16),
         "fT": D("S_fT", [8, 4096], F32), "aT": D("S_aT", [256, 4126], F32), "gT": D("S_gT", [256, 4126], F32)}
    ocv = D("ocv", [4096, 512], BF16); So = D("So", [2048, 2048], BF16)
    x_mid = D("x_mid", [2048, 2048], F32)
    x1_d = m.dram("x1_d", [2048, 2048], F32)
    h2T_d = m.dram("h2T_d", [128, 16, 2048], BF16)
    identf = make_ident(m, F32)
    identb = m.sb([128, 128], BF16)
    m.op("dve", lambda e: e.tensor_copy(identb[:], identf[:]), reads=[identf], writes=[identb])
    jf = m.sb([128, 128], F32); jmat = m.sb([128, 128], BF16)
    m.op("pool", lambda e: e.memset(jf[:], 1.0), writes=[jf])
    m.op("pool", lambda e: e.affine_select(out=jf[:], in_=jf[:], pattern=[[1, 128]], compare_op=ALU.is_equal, fill=0.0,
                                           base=-127, channel_multiplier=1), reads=[jf], writes=[jf])
    m.op("dve", lambda e: e.tensor_copy(jmat[:], jf[:]), reads=[jf], writes=[jmat])
    sel = m.sb([128, 2], F32)
    m.dma(sel[:], sel_d, writes=[sel])
    Wt = m.sb([128, 16, 32], F32)
    PS = {"z": [m.ps([128, 512], F32) for _ in range(2)],
          "o": [m.ps([128, 512], F32) for _ in range(4)],
          "T": m.ps([128, 8, 128], BF16),
          "misc": m.ps([128, 512], F32)}
    with ExitStack() as es:
        emit_mod(m, nc, es, cT, aw, ab, Gm.src, PS)
    Gm.gather(m, nc)
    modsec = lambda l, s: Gm.dst[2 * (s // 3) + l, (s % 3) * 2048:(s % 3 + 1) * 2048]
    for l in range(depth):
        xin = x_in if l == 0 else x_mid
        xo = x_out if l == depth - 1 else x_mid
        with ExitStack() as es:
            emit_A2(m, nc, es, xin, modsec(l, 0), modsec(l, 1), w_in[l], G1.src, G3.src, G2.src, identb, PS)
        G1.gather(m, nc); G2.gather(m, nc); G3.gather(m, nc)
        with ExitStack() as es:
            emit_select1(m, nc, es, sel, G1, G2, G3, S, identb, jmat, PS)
            barrier(m)
        with ExitStack() as es:
            emit_conv2(m, nc, es, S["aT"], S["gT"], cwT[l], cb[l], G4.src)
        G4.gather(m, nc)
        with ExitStack() as es:
            emit_convln(m, nc, es, G4, lng[l], lnb[l], ocv, identf, PS)
            barrier(m)
        with ExitStack() as es:
            emit_sb(m, nc, es, S["sbqT"].rearrange("(h d) t -> h d t", d=64), S["sbkTr"], S["sbvr"], G5.src[:, 0:256], identb, PSB_fix(PS), 4)
            barrier(m)
        with ExitStack() as es:
            emit_fox(m, nc, es, S["fxqT"].rearrange("(h d) t -> h d t", d=64), S["fxkT"].rearrange("(h d) t -> h d t", d=64),
                     S["fxv"], S["fT"], bfg[l], G5.src[:, 256:768], identf, PS, 8)
        G5.gather(m, nc)
        with ExitStack() as es:
            emit_select2(m, nc, es, sel, G5, ocv, So)
            barrier(m)
        modv4 = [modsec(l, 2), modsec(l, 3), modsec(l, 4), modsec(l, 5)]
        with ExitStack() as es:
            emit_C1(m, nc, es, xin, So, ModV(modv4), w_out[l], ln1g[l], ln1b[l], rw[l], rb[l], x1_d, h2T_d, Wt, identb, identf, PS)
            barrier(m)
        with ExitStack() as es:
            emit_C2(m, nc, es, ModV(modv4), ln2g[l], ln2b[l], wg[l], wu[l], wd[l], x1_d, h2T_d, Wt, xo, PS, n_exp)
            barrier(m)
    m.finish()
    return nc


class ModV:
    def __init__(self, aps):
        self.aps = aps

    def __getitem__(self, idx):
        return self.aps[idx[0]]


def PSB_fix(PS):
    d = dict(PS)
    d["T"] = [PS["T"], PS["T"]]
    return d
import ml_dtypes
from concourse.bass_utils import run_bass_kernel_spmd

_PROGS = {}


def _fused_in_maps(x, c, ada_w, ada_b, w_in, b_forget, conv_w, conv_b, conv_ln_g, conv_ln_b,
                   w_out, ln1_g, ln1_b, r1_w, r1_b, r2_w, r2_b, w_gate, w_up, w_down, ln2_g, ln2_b, n_alloc=32):
    f = lambda a: np.ascontiguousarray(np.asarray(a, dtype=np.float32))
    A = lambda a: np.asarray(a)
    rw = f(np.concatenate([A(r1_w), A(r2_w).transpose(0, 2, 1, 3).reshape(2, 2048, 32)], axis=2))
    rb = f(np.concatenate([A(r1_b), A(r2_b).reshape(2, 32)], axis=1))
    shared = {"w_in": f(w_in), "w_out": f(w_out), "lng": f(conv_ln_g), "lnb": f(conv_ln_b), "ln1g": f(ln1_g), "ln1b": f(ln1_b),
              "ln2g": f(ln2_g), "ln2b": f(ln2_b), "rw": rw, "rb": rb,
              "wg": f(A(w_gate)[:, :n_alloc]), "wu": f(A(w_up)[:, :n_alloc]), "wd": f(A(w_down)[:, :n_alloc])}
    maps = []
    for i in range(8):
        b, j = divmod(i, 2)
        sel = np.zeros((128, 2), np.float32); sel[:, j] = 1.0
        d = dict(shared)
        d["x"] = f(A(x)[b, j * 2048:(j + 1) * 2048])
        d["cT"] = f(A(c)[b].reshape(16, 128).T)
        d["sel"] = sel
        d["aw"] = f(A(ada_w)[:, :, j * 6144:(j + 1) * 6144]); d["ab"] = f(A(ada_b)[:, j * 6144:(j + 1) * 6144])
        d["bfg"] = f(A(b_forget)[:, 8 * j:8 * j + 8].reshape(2, 8, 1))
        d["cwT"] = f(A(conv_w)[:, :, j * 256:(j + 1) * 256].transpose(0, 2, 1).reshape(2, 2, 128, 31).transpose(0, 2, 1, 3))
        d["cb"] = f(A(conv_b)[:, j * 256:(j + 1) * 256].reshape(2, 2, 128).transpose(0, 2, 1))
        maps.append(d)
    return maps


def kernel(**inputs):
    if "F" not in _PROGS:
        _PROGS["F"] = build_F()
    maps = _fused_in_maps(**inputs)
    res = run_bass_kernel_spmd(_PROGS["F"], maps, core_ids=list(range(8)))
    xs = [np.asarray(r["x_out"]) for r in res.results]
    out = np.stack([np.concatenate([xs[2 * b], xs[2 * b + 1]], axis=0) for b in range(4)], axis=0)
    return out.astype(np.float32)
```

```python
D_MODEL = 2048; BATCH = 4; SEQ = 4096; DEPTH = 2
MIX_COLS = 5648
LN_EPS = 1e-5
ALPHA = (2 * DEPTH) ** 0.25
import numpy as np
import concourse.bass as bass
import concourse.mybir as mybir

F32 = mybir.dt.float32
BF16 = mybir.dt.bfloat16
I32 = mybir.dt.int32
ALU = mybir.AluOpType
AF = mybir.ActivationFunctionType
AX = mybir.AxisListType


class Buf:
    __slots__ = ("t", "name", "w", "r")

    def __init__(self, t, name):
        self.t = t
        self.name = name
        self.w = None
        self.r = []

    def __getitem__(self, idx):
        return self.t[idx]


class MK:
    def __init__(self, nc, n_dma_sems=24):
        self.nc = nc
        self.E = {"pe": nc.tensor, "act": nc.scalar, "dve": nc.vector,
                  "pool": nc.gpsimd, "sp": nc.sync}
        self.sem = {k: nc.alloc_semaphore("c_" + k) for k in self.E}
        self.cnt = {k: 0 for k in self.E}
        self.dsem = [nc.alloc_semaphore("d%d" % i) for i in range(n_dma_sems)]
        self.dcnt = [0] * n_dma_sems
        self.dnext = 0
        self.seen = {k: {} for k in self.E}
        self.nbuf = 0
        self.out_tokens = []

    def sb(self, shape, dtype, name=None):
        self.nbuf += 1
        name = name or "b%d" % self.nbuf
        return Buf(self.nc.alloc_sbuf_tensor(name, list(shape), dtype), name)

    def ps(self, shape, dtype, name=None):
        self.nbuf += 1
        name = name or "p%d" % self.nbuf
        return Buf(self.nc.alloc_psum_tensor(name, list(shape), dtype), name)

    def dram(self, name, shape, dtype, kind="Internal"):
        t = self.nc.dram_tensor(name, list(shape), dtype, kind=kind)
        return Buf(t.ap(), name)

    def _semobj(self, key):
        return self.sem[key] if isinstance(key, str) else self.dsem[key]

    def _wait(self, eng, tok):
        if tok is None:
            return
        key, val, _ = tok
        if self.seen[eng].get(key, 0) >= val:
            return
        self.E[eng].wait_ge(self._semobj(key), val)
        self.seen[eng][key] = val

    def _deps(self, eng, reads, writes, pe_accum=False):
        for b in reads:
            self._wait(eng, b.w)
        for b in writes:
            if not (pe_accum and b.w is not None and b.w[2] == "pe" and eng == "pe"):
                self._wait(eng, b.w)
            for tok in b.r:
                self._wait(eng, tok)

    def _commit(self, tok, reads, writes):
        for b in reads:
            b.r.append(tok)
            if len(b.r) > 6:
                d = {}
                for t in b.r:
                    if t[0] not in d or d[t[0]][1] < t[1]:
                        d[t[0]] = t
                b.r = list(d.values())
        for b in writes:
            b.w = tok
            b.r = []

    def op(self, eng, fn, reads=(), writes=(), pe_accum=False):
        self._deps(eng, reads, writes, pe_accum)
        ins = fn(self.E[eng])
        self.cnt[eng] += 1
        ins.then_inc(self.sem[eng], 1)
        tok = (eng, self.cnt[eng], eng)
        self._commit(tok, reads, writes)
        return tok

    def dma(self, out, in_, reads=(), writes=(), eng="sp", is_output=False, **kw):
        i = self.dnext
        self.dnext = (self.dnext + 1) % len(self.dsem)
        if self.dcnt[i] > 0:
            self._wait(eng, (i, self.dcnt[i], "dma"))
        self._deps(eng, reads, writes)
        ins = self.E[eng].dma_start(out=out, in_=in_, **kw)
        self.dcnt[i] += 16
        ins.then_inc(self.dsem[i], 16)
        tok = (i, self.dcnt[i], "dma")
        self._commit(tok, reads, writes)
        if is_output:
            self.out_tokens.append(tok)
        return tok

    def finish(self, eng="sp"):
        for tok in self.out_tokens:
            key, val, _ = tok
            self.E[eng].wait_ge(self._semobj(key), val)
        for i, c in enumerate(self.dcnt):
            if c > 0:
                self.E[eng].wait_ge(self.dsem[i], c)
def build_M():
    nc = bass.Bass("TRN2", target_bir_lowering=False)
    cT = nc.dram_tensor("cT", [128, 16, 4], F32, kind="ExternalInput").ap()
    aw = nc.dram_tensor("aw", [2, 2048, 1536], F32, kind="ExternalInput").ap()
    ab = nc.dram_tensor("ab", [2, 1536], F32, kind="ExternalInput").ap()
    mo = nc.dram_tensor("mo", [2, 4, 1536], F32, kind="ExternalOutput").ap()
    m = MK(nc)
    ct = m.sb([128, 16, 4], F32)
    sc = m.sb([128, 16, 4], F32)
    wst = [m.sb([128, 16, 512], F32) for _ in range(2)]
    bt = [m.sb([4, 512], F32) for _ in range(2)]
    ot = [m.sb([4, 512], F32) for _ in range(2)]
    pp = [m.ps([4, 512], F32) for _ in range(2)]
    m.dma(ct[:], cT, writes=[ct])
    m.op("act", lambda e: e.activation(sc[:], ct[:], AF.Silu), reads=[ct], writes=[sc])
    i = 0
    for l in range(2):
        for n in range(3):
            w = wst[i % 2]; b = bt[i % 2]; o = ot[i % 2]; p = pp[i % 2]
            m.dma(w[:], aw[l, :, n * 512:(n + 1) * 512].rearrange("(k p) n -> p k n", p=128), writes=[w])
            m.dma(b[:], ab[l, n * 512:(n + 1) * 512].partition_broadcast(4), writes=[b])
            for k in range(16):
                m.op("pe", lambda e: e.matmul(p[:], sc[:, k, :], w[:, k, :], start=(k == 0), stop=(k == 15)),
                     reads=[sc, w], writes=[p], pe_accum=True)
            m.op("dve", lambda e: e.tensor_tensor(o[:], p[:], b[:], ALU.add), reads=[p, b], writes=[o])
            m.dma(mo[l, :, n * 512:(n + 1) * 512], o[:], reads=[o], is_output=True)
            i += 1
    m.finish()
    return nc


def make_ident(m, dtype):
    idf = m.sb([128, 128], F32)
    m.op("pool", lambda e: e.memset(idf[:], 1.0), writes=[idf])
    m.op("pool", lambda e: e.affine_select(out=idf[:], in_=idf[:], pattern=[[1, 128]], compare_op=ALU.is_equal,
                                           fill=0.0, base=0, channel_multiplier=-1), reads=[idf], writes=[idf])
    if dtype == F32:
        return idf
    idb = m.sb([128, 128], dtype)
    m.op("dve", lambda e: e.tensor_copy(idb[:], idf[:]), reads=[idf], writes=[idb])
    return idb


def ln_stats(m, xt, st, mv, rstd):
    for q in range(4):
        m.op("dve", lambda e: e.bn_stats(st[:, q * 6:(q + 1) * 6], xt[:, q * 512:(q + 1) * 512]), reads=[xt], writes=[st])
    m.op("dve", lambda e: e.bn_aggr(mv[:], st[:]), reads=[st], writes=[mv])
    m.op("dve", lambda e: e.tensor_scalar_add(rstd[:], mv[:, 1:2], LN_EPS), reads=[mv], writes=[rstd])
    m.op("act", lambda e: e.activation(rstd[:], rstd[:], AF.Sqrt), reads=[rstd], writes=[rstd])
    m.op("dve", lambda e: e.reciprocal(rstd[:], rstd[:]), reads=[rstd], writes=[rstd])


FM_BLOCKS = [(0, "qk", 0), (256, "qk", 256), (512, "qk", 512), (768, "qk", 768),
             (1536, "qk", 1024), (1792, "qk", 1280), (2048, "qk", 1536), (2304, "qk", 1792),
             (2560, "qk", 2048), (2816, "qk", 2304), (3072, "qk", 2560), (3328, "qk", 2816),
             (4608, "fag", 0), (4864, "fag", 256), (5120, "fag", 512), (5376, "fag", 768)]
TM_BLOCKS = [(1024, 0), (1280, 256), (3584, 512), (3840, 768), (4096, 1024), (4352, 1280)]


def emit_A(m, nc, x, modv, w_in, qkT, fagT, vtm, ident):
    NT = 16
    hT = m.sb([128, 16, 2048], BF16, "hT")
    scb = m.sb([128, 2048], F32, "scb")
    shb = m.sb([128, 2048], F32, "shb")
    xts = [m.sb([128, 2048], F32) for _ in range(2)]
    hb = [m.sb([128, 2048], BF16) for _ in range(2)]
    st = m.sb([128, 24], F32); mv = m.sb([128, 2], F32); rstd = m.sb([128, 1], F32)
    pT = [m.ps([128, 8, 128], BF16) for _ in range(2)]
    m.dma(shb[:], modv[0, :].partition_broadcast(128), writes=[shb])
    m.dma(scb[:], modv[1, :].partition_broadcast(128), writes=[scb])
    m.op("pool", lambda e: e.tensor_scalar_add(scb[:], scb[:], 1.0), reads=[scb], writes=[scb])
    m.dma(xts[0][:], x[0:128, :], writes=[xts[0]])
    for t in range(NT):
        xt = xts[t % 2]; h = hb[t % 2]
        if t + 1 < NT:
            m.dma(xts[(t + 1) % 2][:], x[(t + 1) * 128:(t + 2) * 128, :], writes=[xts[(t + 1) % 2]])
        ln_stats(m, xt, st, mv, rstd)
        m.op("dve", lambda e: e.tensor_scalar(xt[:], xt[:], mv[:, 0:1], rstd[:, 0:1], ALU.subtract, ALU.mult),
             reads=[xt, mv, rstd], writes=[xt])
        m.op("pool", lambda e: e.tensor_tensor(xt[:], xt[:], scb[:], ALU.mult), reads=[xt, scb], writes=[xt])
        m.op("dve", lambda e: e.tensor_tensor(h[:], xt[:], shb[:], ALU.add), reads=[xt, shb], writes=[h])
        for half in range(2):
            p = pT[half]
            for j in range(8):
                k = half * 8 + j
                m.op("pe", lambda e: e.transpose(p[:, j, :], h[:, k * 128:(k + 1) * 128], ident[:]),
                     reads=[h, ident], writes=[p], pe_accum=True)
            m.op("act", lambda e: e.copy(hT[:, half * 8:(half + 1) * 8, t * 128:(t + 1) * 128], p[:]),
                 reads=[p], writes=[hT])
    wst = [m.sb([128, 16, 256], F32) for _ in range(2)]
    wbf = [m.sb([128, 16, 256], BF16) for _ in range(2)]
    pm = [m.ps([128, 512], F32) for _ in range(4)]
    oq = [m.sb([128, 2048], BF16) for _ in range(2)]
    of = [m.sb([128, 2048], F32) for _ in range(2)]
    ov = [m.sb([128, 256], BF16) for _ in range(2)]
    blocks = [("fm", c0, kind, o0) for (c0, kind, o0) in FM_BLOCKS] + [("tm", c0, None, o0) for (c0, o0) in TM_BLOCKS]
    blocks.append(("fm16", 5632, "fag", 1024))
    pi = 0; oi = 0
    def load_w(bi):
        typ, c0, kind, o0 = blocks[bi]
        cw = 16 if typ == "fm16" else 256
        m.dma(wst[bi % 2][:, :, 0:cw], w_in[:, c0:c0 + cw].rearrange("(k p) n -> p k n", p=128), writes=[wst[bi % 2]])
    load_w(0)
    for bi, (typ, c0, kind, o0) in enumerate(blocks):
        cw = 16 if typ == "fm16" else 256
        ws = wst[bi % 2]; wb = wbf[bi % 2]
        if bi + 1 < len(blocks):
            load_w(bi + 1)
        m.op("act", lambda e: e.copy(wb[:, 0:8, 0:cw], ws[:, 0:8, 0:cw]), reads=[ws], writes=[wb])
        m.op("pool", lambda e: e.tensor_copy(wb[:, 8:16, 0:cw], ws[:, 8:16, 0:cw]), reads=[ws], writes=[wb])
        if typ in ("fm", "fm16"):
            for cc in range(0, cw, 128):
                cn = min(128, cw - cc)
                o = (oq if kind == "qk" else of)[oi % 2]; oi += 1
                for tg in range(4):
                    p = pm[pi % 4]; pi += 1
                    for k in range(16):
                        m.op("pe", lambda e: e.matmul(p[0:cn, :], wb[:, k, cc:cc + cn], hT[:, k, tg * 512:(tg + 1) * 512],
                                                      start=(k == 0), stop=(k == 15)),
                             reads=[wb, hT], writes=[p], pe_accum=True)
                    if tg % 2 == 0:
                        m.op("act", lambda e: e.copy(o[0:cn, tg * 512:(tg + 1) * 512], p[0:cn, :]), reads=[p], writes=[o])
                    else:
                        m.op("dve", lambda e: e.tensor_copy(o[0:cn, tg * 512:(tg + 1) * 512], p[0:cn, :]), reads=[p], writes=[o])
                dst = qkT if kind == "qk" else fagT
                m.dma(dst[o0 + cc:o0 + cc + cn, :], o[0:cn, :], reads=[o], is_output=True)
        else:
            for tt in range(16):
                p = pm[pi % 4]; pi += 1
                o = ov[oi % 2]; oi += 1
                for k in range(16):
                    m.op("pe", lambda e: e.matmul(p[:, 0:256], hT[:, k, tt * 128:(tt + 1) * 128], wb[:, k, :],
                                                  start=(k == 0), stop=(k == 15)),
                         reads=[wb, hT], writes=[p], pe_accum=True)
                if tt % 2 == 0:
                    m.op("act", lambda e: e.copy(o[:], p[:, 0:256]), reads=[p], writes=[o])
                else:
                    m.op("dve", lambda e: e.tensor_copy(o[:], p[:, 0:256]), reads=[p], writes=[o])
                m.dma(vtm[tt * 128:(tt + 1) * 128, o0:o0 + 256], o[:], reads=[o], is_output=True)


def build_A():
    nc = bass.Bass("TRN2", target_bir_lowering=False)
    x = nc.dram_tensor("x", [2048, 2048], F32, kind="ExternalInput").ap()
    modv = nc.dram_tensor("modv", [2, 2048], F32, kind="ExternalInput").ap()
    w_in = nc.dram_tensor("w_in", [2048, MIX_COLS], F32, kind="ExternalInput").ap()
    qkT = nc.dram_tensor("qkT", [3072, 2048], BF16, kind="ExternalOutput").ap()
    fagT = nc.dram_tensor("fagT", [1040, 2048], F32, kind="ExternalOutput").ap()
    vtm = nc.dram_tensor("vtm", [2048, 1536], BF16, kind="ExternalOutput").ap()
    m = MK(nc)
    ident = make_ident(m, BF16)
    emit_A(m, nc, x, modv, w_in, qkT, fagT, vtm, ident)
    m.finish()
    return nc
def barrier(m):
    for e in m.E:
        for f in m.E:
            if m.cnt[f] > 0:
                m._wait(e, (f, m.cnt[f], f))
        for i, c in enumerate(m.dcnt):
            if c > 0:
                m._wait(e, (i, c, "dma"))


def emit_conv(m, nc, es, aT, gT, cwT, cb, lng, lnb, o_cv, identf, PS):
    def sb(shape, dt):
        m.nbuf += 1
        return Buf(es.enter_context(nc.sbuf_tensor("cv%d" % m.nbuf, list(shape), dt)), "cv")
    cvT = sb([128, 4, 2048], F32)
    at = [sb([128, 2078], F32) for _ in range(2)]
    gt = [sb([128, 2078], F32) for _ in range(2)]
    cw = sb([128, 4, 31], F32); cbt = sb([128, 4], F32)
    lg = sb([128, 512], F32); lb = sb([128, 512], F32)
    m.dma(cw[:], cwT, writes=[cw])
    m.dma(cbt[:], cb, writes=[cbt])
    m.dma(lg[:], lng.partition_broadcast(128), writes=[lg])
    m.dma(lb[:], lnb.partition_broadcast(128), writes=[lb])
    for cc in range(4):
        a = at[cc % 2]; g = gt[cc % 2]
        m.dma(a[:], aT[cc * 128:(cc + 1) * 128, :], writes=[a])
        m.dma(g[:], gT[cc * 128:(cc + 1) * 128, :], writes=[g])
        m.op("act", lambda e: e.activation(g[:], g[:], AF.Sigmoid), reads=[g], writes=[g])
        m.op("dve", lambda e: e.tensor_tensor(a[:], a[:], g[:], ALU.mult), reads=[a, g], writes=[a])
        m.op("dve", lambda e: e.tensor_scalar(cvT[:, cc, :], a[:, 0:2048], cw[:, cc, 0:1], cbt[:, cc:cc + 1], ALU.mult, ALU.add),
             reads=[a, cw, cbt], writes=[cvT])
        for w in range(1, 31):
            m.op("dve", lambda e: e.scalar_tensor_tensor(cvT[:, cc, :], a[:, w:w + 2048], cw[:, cc, w:w + 1], cvT[:, cc, :], ALU.mult, ALU.add),
                 reads=[a, cw, cvT], writes=[cvT])
    pt = PS["z"]
    ut = [sb([128, 512], F32) for _ in range(2)]
    ob = [sb([128, 512], BF16) for _ in range(2)]
    st = sb([128, 6], F32); mv = sb([128, 2], F32); rstd = sb([128, 1], F32)
    for t in range(16):
        p = pt[t % 2]; u = ut[t % 2]; o = ob[t % 2]
        for cc in range(4):
            m.op("pe", lambda e: e.transpose(p[:, cc * 128:(cc + 1) * 128], cvT[:, cc, t * 128:(t + 1) * 128], identf[:]),
                 reads=[cvT, identf], writes=[p], pe_accum=True)
        m.op("act", lambda e: e.copy(u[:], p[:]), reads=[p], writes=[u])
        m.op("dve", lambda e: e.bn_stats(st[:], u[:]), reads=[u], writes=[st])
        m.op("dve", lambda e: e.bn_aggr(mv[:], st[:]), reads=[st], writes=[mv])
        m.op("dve", lambda e: e.tensor_scalar_add(rstd[:], mv[:, 1:2], LN_EPS), reads=[mv], writes=[rstd])
        m.op("act", lambda e: e.activation(rstd[:], rstd[:], AF.Sqrt), reads=[rstd], writes=[rstd])
        m.op("dve", lambda e: e.reciprocal(rstd[:], rstd[:]), reads=[rstd], writes=[rstd])
        m.op("dve", lambda e: e.tensor_scalar(u[:], u[:], mv[:, 0:1], rstd[:, 0:1], ALU.subtract, ALU.mult),
             reads=[u, mv, rstd], writes=[u])
        m.op("pool", lambda e: e.tensor_tensor(u[:], u[:], lg[:], ALU.mult), reads=[u, lg], writes=[u])
        m.op("dve", lambda e: e.tensor_tensor(u[:], u[:], lb[:], ALU.add), reads=[u, lb], writes=[u])
        m.op("act", lambda e: e.activation(o[:], u[:], AF.Silu), reads=[u], writes=[o])
        m.dma(o_cv[t * 128:(t + 1) * 128, :], o[:], reads=[o], is_output=True)


def run_pipeline(n, stages):
    S = len(stages)
    for step in range(n + S - 1):
        for st in range(S - 1, -1, -1):
            i = step - st
            if 0 <= i < n:
                stages[st](i)


def emit_sb(m, nc, es, sbqT, sbkTr, sbvr, o_sb, identb, PS, nheads=4):
    def sb(shape, dt):
        m.nbuf += 1
        return Buf(es.enter_context(nc.sbuf_tensor("sb%d" % m.nbuf, list(shape), dt)), "sb")
    QT = [sb([64, 4096], BF16) for _ in range(2)]
    KT = [sb([64, 4096], BF16) for _ in range(2)]
    VR = [sb([128, 32, 64], BF16) for _ in range(2)]
    ones = sb([128, 512], F32)
    m.op("pool", lambda e: e.memset(ones[:], 1.0), writes=[ones])
    NB = 4
    e_t = [sb([128, 512], F32) for _ in range(NB)]
    sp_t = [sb([128, 512], F32) for _ in range(NB)]
    r_t = [sb([128, 512], F32) for _ in range(NB)]
    la_t = [sb([128, 512], F32) for _ in range(NB)]
    a_t = [sb([128, 512], BF16) for _ in range(NB)]
    aT_t = [sb([128, 4, 128], BF16) for _ in range(2)]
    ost = [sb([128, 32, 64], BF16) for _ in range(2)]
    pzs = list(PS["z"]) + [PS["misc"]]
    NZ = len(pzs)
    pT2 = [Buf(PS["T"][0].t[:, 0:4, :], "pTa"), Buf(PS["T"][0].t[:, 4:8, :], "pTb")]
    po = PS["o"][0]

    def load(h):
        m.dma(QT[h % 2][:], sbqT[h], writes=[QT[h % 2]])
        m.dma(KT[h % 2][:], sbkTr[h], writes=[KT[h % 2]])
        m.dma(VR[h % 2][:], sbvr[:, h * 64:(h + 1) * 64].rearrange("(c p) d -> p c d", p=128), writes=[VR[h % 2]])
    items = []
    for h in range(nheads):
        for i in range(32):
            nch = (i + 1 + 3) // 4
            for c in range(nch):
                items.append((h, i, c, nch))
    load(0)

    def geom(ix):
        h, i, c, nch = items[ix]
        c0 = 128 * (31 - i) + 512 * c
        w = min(512, 4096 - c0)
        return h, i, c, nch, c0, w

    def s0(ix):
        h, i, c, nch, c0, w = geom(ix)
        q = QT[h % 2]; k = KT[h % 2]
        z = pzs[ix % NZ]; et = e_t[ix % NB]; spt = sp_t[ix % NB]
        m.op("pe", lambda e: e.matmul(z[:, 0:w], q[:, i * 128:(i + 1) * 128], k[:, c0:c0 + w], start=True, stop=True),
             reads=[q, k], writes=[z])
        m.op("act", lambda e: e.activation(et[:, 0:w], z[:, 0:w], AF.Exp, scale=0.125), reads=[z], writes=[et])
        m.op("act", lambda e: e.activation(spt[:, 0:w], et[:, 0:w], AF.Ln, bias=1.0), reads=[et], writes=[spt])
        if c == 0:
            m.op("pool", lambda e: e.affine_select(out=spt[:, 0:128], in_=spt[:, 0:128], pattern=[[1, 128]],
                                                   compare_op=ALU.is_gt, fill=0.0, base=-127, channel_multiplier=1),
                 reads=[spt], writes=[spt])

    def s1(ix):
        h, i, c, nch, c0, w = geom(ix)
        z = pzs[ix % NZ]; spt = sp_t[ix % NB]; rt = r_t[ix % NB]; lat = la_t[ix % NB]
        if c == 0:
            init = 0.0; rd = [ones, spt]
        else:
            prt = r_t[(ix - 1) % NB]
            init = prt[:, 511:512]; rd = [ones, spt, prt]
        m.op("dve", lambda e: e.tensor_tensor_scan(rt[:, 0:w], ones[:, 0:w], spt[:, 0:w], init, ALU.mult, ALU.add),
             reads=rd, writes=[rt])
        m.op("dve", lambda e: e.scalar_tensor_tensor(lat[:, 0:w], z[:, 0:w], 0.125, rt[:, 0:w], ALU.mult, ALU.subtract),
             reads=[z, rt], writes=[lat])

    def s2(ix):
        h, i, c, nch, c0, w = geom(ix)
        lat = la_t[ix % NB]; at_ = a_t[ix % NB]
        m.op("act", lambda e: e.activation(at_[:, 0:w], lat[:, 0:w], AF.Exp), reads=[lat], writes=[at_])
        if c == 0:
            m.op("pool", lambda e: e.affine_select(out=at_[:, 0:128], in_=at_[:, 0:128], pattern=[[1, 128]],
                                                   compare_op=ALU.is_gt, fill=0.0, base=-127, channel_multiplier=1),
                 reads=[at_], writes=[at_])

    def s3(ix):
        h, i, c, nch, c0, w = geom(ix)
        nb = w // 128
        v = VR[h % 2]; os_ = ost[h % 2]
        at_ = a_t[ix % NB]; aTt = aT_t[ix % 2]
        pTb = pT2[ix % 2]
        if i == 0 and c == 0 and h + 1 < nheads:
            load(h + 1)
        for bb in range(nb):
            m.op("pe", lambda e: e.transpose(pTb[:, bb, :], at_[:, bb * 128:(bb + 1) * 128], identb[:]),
                 reads=[at_, identb], writes=[pTb], pe_accum=True)
        m.op("dve", lambda e: e.tensor_copy(aTt[:, 0:nb, :], pTb[:, 0:nb, :]), reads=[pTb], writes=[aTt])
        for bb in range(nb):
            kb = (c0 // 128) + bb
            first = (c == 0 and bb == 0); last = (c == nch - 1 and bb == nb - 1)
            m.op("pe", lambda e: e.matmul(po[:, 0:64], aTt[:, bb, :], v[:, kb, :], start=first, stop=last),
                 reads=[aTt, v], writes=[po], pe_accum=True)
        if c == nch - 1:
            m.op("act", lambda e: e.copy(os_[:, i, :], po[:, 0:64]), reads=[po], writes=[os_])
            if i == 31:
                m.dma(o_sb[:, h * 64:(h + 1) * 64].rearrange("(i p) d -> p i d", p=128), os_[:], reads=[os_], is_output=True)
    run_pipeline(len(items), [s0, s1, s2, s3])


def emit_fox(m, nc, es, fxqT, fxkT, fxv, fT, negb, o_fx, identf, PS, nheads=8):
    def sb(shape, dt):
        m.nbuf += 1
        return Buf(es.enter_context(nc.sbuf_tensor("fx%d" % m.nbuf, list(shape), dt)), "fx")
    NH = nheads
    C = sb([NH, 4096], F32); W1 = sb([NH, 4096], F32); nb_t = sb([NH, 1], F32)
    onesr = sb([NH, 4096], F32)
    m.dma(W1[:], fT[0:NH, :], writes=[W1])
    m.dma(nb_t[:], negb[0:NH, :], writes=[nb_t])
    m.op("pool", lambda e: e.memset(onesr[:], 1.0), writes=[onesr])
    m.op("dve", lambda e: e.tensor_scalar_mul(nb_t[:], nb_t[:], -1.0), reads=[nb_t], writes=[nb_t])
    m.op("act", lambda e: e.activation(W1[:], W1[:], AF.Exp, bias=nb_t[:, 0:1], scale=-1.0), reads=[W1, nb_t], writes=[W1])
    m.op("act", lambda e: e.activation(W1[:], W1[:], AF.Ln, bias=1.0), reads=[W1], writes=[W1])
    m.op("dve", lambda e: e.tensor_tensor_scan(C[:], onesr[:], W1[:], 0.0, ALU.mult, ALU.add), reads=[onesr, W1], writes=[C])
    CkT = sb([128, 32, NH], F32)
    pc = PS["misc"]
    for j in range(32):
        m.op("pe", lambda e: e.transpose(pc[:, j * NH:(j + 1) * NH], C[:, j * 128:(j + 1) * 128], identf[0:NH, 0:NH]),
             reads=[C, identf], writes=[pc], pe_accum=True)
    m.op("dve", lambda e: e.tensor_copy(CkT[:].rearrange("p j h -> p (j h)"), pc[:, 0:32 * NH]), reads=[pc], writes=[CkT])
    Rd = sb([NH, NH, 8], F32)
    Cg = C[:].rearrange("h (g q) -> h g q", q=512)
    m.op("dve", lambda e: e.tensor_tensor(Rd[:], Cg[:, :, 511:512].rearrange("h g o -> h o g").to_broadcast([NH, NH, 8]),
                                          identf[0:NH, 0:NH].unsqueeze(2).to_broadcast([NH, NH, 8]), ALU.mult),
         reads=[C, identf], writes=[Rd])
    onesk = sb([NH, 128], F32)
    m.op("pool", lambda e: e.memset(onesk[:], 1.0), writes=[onesk])
    m.op("pe", lambda e: e.matmul(pc[:, 256:256 + NH * 8], onesk[:], Rd[:].rearrange("h a g -> h (a g)"), start=True, stop=True),
         reads=[onesk, Rd, CkT], writes=[pc])
    Rbc = sb([128, NH, 8], F32)
    m.op("dve", lambda e: e.tensor_copy(Rbc[:].rearrange("p h g -> p (h g)"), pc[:, 256:256 + NH * 8]), reads=[pc], writes=[Rbc])
    bias = sb([128, NH, 8, 32], F32)
    for h in range(NH):
        for g in range(8):
            nj = 4 * g + 4
            m.op("dve", lambda e: e.tensor_scalar(bias[:, h, g, 0:nj], CkT[:, 0:nj, h], Rbc[:, h, g:g + 1], None, ALU.subtract),
                 reads=[CkT, Rbc], writes=[bias])
    D8 = sb([NH, 4096], F32); Dhi = sb([NH, 4096], BF16); Dlo = sb([NH, 4096], BF16)
    D8g = D8[:].rearrange("h (g q) -> h g q", q=512)
    m.op("dve", lambda e: e.tensor_tensor(D8g, Cg[:, :, 511:512].to_broadcast([NH, 8, 512]), Cg, ALU.subtract),
         reads=[C], writes=[D8])
    m.op("dve", lambda e: e.tensor_scalar_mul(D8[:], D8[:], 8.0), reads=[D8], writes=[D8])
    m.op("dve", lambda e: e.tensor_copy(Dhi[:], D8[:]), reads=[D8], writes=[Dhi])
    m.op("dve", lambda e: e.tensor_tensor(D8[:], D8[:], Dhi[:], ALU.subtract), reads=[D8, Dhi], writes=[D8])
    m.op("dve", lambda e: e.tensor_copy(Dlo[:], D8[:]), reads=[D8], writes=[Dlo])
    QT = [sb([66, 4096], BF16) for _ in range(2)]
    KT = [sb([66, 4096], BF16) for _ in range(2)]
    V = [sb([128, 32, 65], BF16) for _ in range(2)]
    for i in range(2):
        m.op("pool", lambda e: e.memset(KT[i][64:66, :], 1.0), writes=[KT[i]])
        m.op("pool", lambda e: e.memset(V[i][:, :, 64:65], 1.0), writes=[V[i]])
    NP = 4
    P_t = [sb([128, 512], BF16) for _ in range(NP)]
    ost = [sb([128, 32, 64], BF16) for _ in range(2)]
    rc = sb([128, 4], F32)
    pzs = list(PS["z"]) + [PS["misc"]]
    NZ = len(pzs)
    po = PS["o"]

    def load(h):
        m.dma(QT[h % 2][0:64, :], fxqT[h], writes=[QT[h % 2]])
        m.dma(QT[h % 2][64:65, :], Dhi[h:h + 1, :], reads=[Dhi], writes=[QT[h % 2]])
        m.dma(QT[h % 2][65:66, :], Dlo[h:h + 1, :], reads=[Dlo], writes=[QT[h % 2]])
        m.dma(KT[h % 2][0:64, :], fxkT[h], writes=[KT[h % 2]])
        m.dma(V[h % 2][:, :, 0:64], fxv[:, h * 64:(h + 1) * 64].rearrange("(c p) d -> p c d", p=128), writes=[V[h % 2]])
    load(0)
    items = [(h, g, j) for h in range(NH) for g in range(8) for j in range(4 * g + 4)]

    def geom(ix):
        h, g, j = items[ix]
        md = j - 4 * g
        q0 = 128 * max(0, md)
        return h, g, j, md, q0, 512 - q0

    def s0(ix):
        h, g, j, md, q0, w = geom(ix)
        q = QT[h % 2]; k = KT[h % 2]; z = pzs[ix % NZ]
        m.op("pe", lambda e: e.matmul(z[:, 0:w], k[:, j * 128:(j + 1) * 128], q[:, 512 * g + q0:512 * g + 512], start=True, stop=True),
             reads=[q, k], writes=[z])

    def s1(ix):
        h, g, j, md, q0, w = geom(ix)
        z = pzs[ix % NZ]; P = P_t[ix % NP]
        m.op("act", lambda e: e.activation(P[:, 0:w], z[:, 0:w], AF.Exp, bias=bias[:, h, g, j:j + 1], scale=0.125),
             reads=[z, bias], writes=[P])
        if md >= 0:
            m.op("pool", lambda e: e.affine_select(out=P[:, 0:128], in_=P[:, 0:128], pattern=[[1, 128]],
                                                   compare_op=ALU.is_ge, fill=0.0, base=0, channel_multiplier=-1),
                 reads=[P], writes=[P])

    def s2(ix):
        h, g, j, md, q0, w = geom(ix)
        v = V[h % 2]; os_ = ost[h % 2]; P = P_t[ix % NP]
        if g == 0 and j == 0 and h + 1 < NH:
            load(h + 1)
        for s_ in range(max(0, md), 4):
            lc = (s_ * 128) - q0
            m.op("pe", lambda e: e.matmul(po[s_][:, 0:65], P[:, lc:lc + 128], v[:, j, :], start=(j == 0), stop=(j == 4 * g + s_)),
                 reads=[P, v], writes=[po[s_]], pe_accum=True)
        if j == 4 * g + 3:
            for s_ in range(4):
                m.op("dve", lambda e: e.reciprocal(rc[:, s_:s_ + 1], po[s_][:, 64:65]), reads=[po[s_]], writes=[rc])
                m.op("dve", lambda e: e.tensor_scalar(os_[:, 4 * g + s_, :], po[s_][:, 0:64], rc[:, s_:s_ + 1], None, ALU.mult),
                     reads=[po[s_], rc], writes=[os_])
            if g == 7:
                m.dma(o_fx[:, h * 64:(h + 1) * 64].rearrange("(i p) d -> p i d", p=128), os_[:], reads=[os_], is_output=True)
    run_pipeline(len(items), [s0, s1, s2])


def build_B(do_conv=True, n_sb=4, n_fx=8):
    from contextlib import ExitStack
    nc = bass.Bass("TRN2", target_bir_lowering=False)
    dt = lambda n, s, d, k="ExternalInput": nc.dram_tensor(n, s, d, kind=k).ap()
    sbqT = dt("sbqT", [4, 64, 4096], BF16); sbkTr = dt("sbkTr", [4, 64, 4096], BF16); sbvr = dt("sbvr", [4096, 256], BF16)
    fxqT = dt("fxqT", [8, 64, 4096], BF16); fxkT = dt("fxkT", [8, 64, 4096], BF16); fxv = dt("fxv", [4096, 512], BF16)
    fT = dt("fT", [8, 4096], F32); negb = dt("bfg", [8, 1], F32)
    aT = dt("aT", [512, 2078], F32); gT = dt("gT", [512, 2078], F32)
    cwT = dt("cwT", [128, 4, 31], F32); cb = dt("cb", [128, 4], F32); lng = dt("lng", [512], F32); lnb = dt("lnb", [512], F32)
    o_sb = dt("o_sb", [4096, 256], BF16, "ExternalOutput"); o_fx = dt("o_fx", [4096, 512], BF16, "ExternalOutput")
    o_cv = dt("o_cv", [2048, 512], BF16, "ExternalOutput")
    m = MK(nc)
    identf = make_ident(m, F32)
    identb = m.sb([128, 128], BF16)
    m.op("dve", lambda e: e.tensor_copy(identb[:], identf[:]), reads=[identf], writes=[identb])
    PS = {"z": [m.ps([128, 512], F32) for _ in range(2)],
          "o": [m.ps([128, 512], F32) for _ in range(4)],
          "T": [m.ps([128, 8, 128], BF16)] * 2,
          "misc": m.ps([128, 512], F32)}
    if do_conv:
        with ExitStack() as es:
            emit_conv(m, nc, es, aT, gT, cwT, cb, lng, lnb, o_cv, identf, PS)
            barrier(m)
    if n_sb:
        with ExitStack() as es:
            emit_sb(m, nc, es, sbqT, sbkTr, sbvr, o_sb, identb, PS, n_sb)
            barrier(m)
    if n_fx:
        with ExitStack() as es:
            emit_fox(m, nc, es, fxqT, fxkT, fxv, fT, negb, o_fx, identf, PS, n_fx)
            barrier(m)
    m.finish()
    return nc
def ln_affine(m, r, st, mv, rstd, g_bc, b_bc, out):
    ln_stats(m, r, st, mv, rstd)
    m.op("dve", lambda e: e.tensor_scalar(r[:], r[:], mv[:, 0:1], rstd[:, 0:1], ALU.subtract, ALU.mult),
         reads=[r, mv, rstd], writes=[r])
    m.op("pool", lambda e: e.tensor_tensor(r[:], r[:], g_bc[:], ALU.mult), reads=[r, g_bc], writes=[r])
    m.op("dve", lambda e: e.tensor_tensor(out[:], r[:], b_bc[:], ALU.add), reads=[r, b_bc], writes=[out])


def emit_C1(m, nc, es, x, o, modv, w_out, ln1g, ln1b, rw, rb, x1_d, h2T_d, Wt, identb, identf, PS):
    def sb(shape, dt):
        m.nbuf += 1
        return Buf(es.enter_context(nc.sbuf_tensor("c1_%d" % m.nbuf, list(shape), dt)), "c1")
    wob = sb([128, 16, 2048], BF16)
    wst = [sb([128, 2048], F32) for _ in range(2)]
    bc = {}
    for nm, src in (("gt1", modv[0, :]), ("sh2", modv[1, :]), ("sc2", modv[2, :]), ("l1g", ln1g), ("l1b", ln1b)):
        bc[nm] = sb([128, 2048], F32)
        m.dma(bc[nm][:], src.partition_broadcast(128), writes=[bc[nm]])
    m.op("pool", lambda e: e.tensor_scalar_add(bc["sc2"][:], bc["sc2"][:], 1.0), reads=[bc["sc2"]], writes=[bc["sc2"]])
    rwt = sb([128, 16, 36], F32); rbt = sb([128, 36], F32)
    m.dma(rwt[:], rw.rearrange("(k p) n -> p k n", p=128), writes=[rwt])
    m.dma(rbt[:], rb.partition_broadcast(128), writes=[rbt])
    m.dma(wst[0][:], w_out[0:128, :], writes=[wst[0]])
    for k in range(16):
        if k + 1 < 16:
            m.dma(wst[(k + 1) % 2][:], w_out[(k + 1) * 128:(k + 2) * 128, :], writes=[wst[(k + 1) % 2]])
        if k % 2 == 0:
            m.op("act", lambda e: e.copy(wob[:, k, :], wst[k % 2][:]), reads=[wst[k % 2]], writes=[wob])
        else:
            m.op("pool", lambda e: e.tensor_copy(wob[:, k, :], wst[k % 2][:]), reads=[wst[k % 2]], writes=[wob])
    ot = [sb([128, 2048], BF16) for _ in range(2)]
    xt = [sb([128, 2048], F32) for _ in range(2)]
    oTs = [sb([128, 16, 128], BF16) for _ in range(2)]
    tmps = [sb([128, 2048], F32) for _ in range(2)]; x1 = sb([128, 2048], F32); h2f = sb([128, 2048], F32); h2b = sb([128, 2048], BF16)
    h2T = sb([128, 16, 128], BF16); h2fT = sb([128, 16, 128], F32)
    st = sb([128, 24], F32); mv = sb([128, 2], F32); rstd = sb([128, 1], F32)
    lg = sb([128, 36], F32)
    sm = {k: sb([128, n], F32) for k, n in (("m1", 1), ("nm1", 1), ("ohg", 4), ("e1", 4), ("s1", 1), ("pg", 1), ("t48", 32),
                                           ("sel", 8), ("m2a", 1), ("nm2a", 1), ("oh1", 8), ("sel2", 8), ("m2b", 1), ("oh2", 8),
                                           ("r", 1), ("den", 1), ("w1", 1), ("w2", 1), ("wsel", 8))}
    pT = PS["T"]; py = PS["o"]; pz = PS["z"]; pm = PS["misc"]

    def load(t):
        m.dma(ot[t % 2][:], o[t * 128:(t + 1) * 128, :], writes=[ot[t % 2]])
        m.dma(xt[t % 2][:], x[t * 128:(t + 1) * 128, :], writes=[xt[t % 2]])
    load(0)

    def stage_a(t):
        if t + 1 < 16:
            load(t + 1)
        ob = ot[t % 2]; xx = xt[t % 2]; oT = oTs[t % 2]; tmp = tmps[t % 2]
        for half in range(2):
            for j in range(8):
                kk = half * 8 + j
                m.op("pe", lambda e: e.transpose(pT[:, j, :], ob[:, kk * 128:(kk + 1) * 128], identb[:]),
                     reads=[ob, identb], writes=[pT], pe_accum=True)
            m.op("act", lambda e: e.copy(oT[:, half * 8:(half + 1) * 8, :], pT[:]), reads=[pT], writes=[oT])
        for n in range(4):
            p = py[n]
            for k in range(16):
                m.op("pe", lambda e: e.matmul(p[:], oT[:, k, :], wob[:, k, n * 512:(n + 1) * 512], start=(k == 0), stop=(k == 15)),
                     reads=[oT, wob], writes=[p], pe_accum=True)
            m.op("dve", lambda e: e.tensor_tensor(tmp[:, n * 512:(n + 1) * 512], p[:], bc["gt1"][:, n * 512:(n + 1) * 512], ALU.mult),
                 reads=[p, bc["gt1"]], writes=[tmp])
        m.op("dve", lambda e: e.scalar_tensor_tensor(tmp[:], xx[:], ALPHA, tmp[:], ALU.mult, ALU.add), reads=[xx, tmp], writes=[tmp])

    def stage_b(t):
        tmp = tmps[t % 2]
        ln_affine(m, tmp, st, mv, rstd, bc["l1g"], bc["l1b"], x1)
        m.dma(x1_d[t * 128:(t + 1) * 128, :], x1[:], reads=[x1], writes=[x1_d])
        ln_stats(m, x1, st, mv, rstd)
        m.op("dve", lambda e: e.tensor_scalar(h2f[:], x1[:], mv[:, 0:1], rstd[:, 0:1], ALU.subtract, ALU.mult),
             reads=[x1, mv, rstd], writes=[h2f])
        m.op("pool", lambda e: e.tensor_tensor(h2f[:], h2f[:], bc["sc2"][:], ALU.mult), reads=[h2f, bc["sc2"]], writes=[h2f])
        m.op("dve", lambda e: e.tensor_tensor(h2f[:], h2f[:], bc["sh2"][:], ALU.add), reads=[h2f, bc["sh2"]], writes=[h2f])
        m.op("act", lambda e: e.copy(h2b[:], h2f[:]), reads=[h2f], writes=[h2b])
        for half in range(2):
            for j in range(8):
                kk = half * 8 + j
                m.op("pe", lambda e: e.transpose(pT[:, j, :], h2b[:, kk * 128:(kk + 1) * 128], identb[:]),
                     reads=[h2b, identb], writes=[pT], pe_accum=True)
            m.op("act", lambda e: e.copy(h2T[:, half * 8:(half + 1) * 8, :], pT[:]), reads=[pT], writes=[h2T])
        m.dma(h2T_d[:, :, t * 128:(t + 1) * 128], h2T[:], reads=[h2T], writes=[h2T_d])
        for q4 in range(4):
            p = pz[q4 % 2]
            for j in range(4):
                kk = q4 * 4 + j
                m.op("pe", lambda e: e.transpose(p[:, j * 128:(j + 1) * 128], h2f[:, kk * 128:(kk + 1) * 128], identf[:]),
                     reads=[h2f, identf], writes=[p], pe_accum=True)
            m.op("act", lambda e: e.copy(h2fT[:, q4 * 4:(q4 + 1) * 4, :].rearrange("p a b -> p (a b)"), p[:]), reads=[p], writes=[h2fT])
        for k in range(16):
            m.op("pe", lambda e: e.matmul(pm[:, 0:36], h2fT[:, k, :], rwt[:, k, :], start=(k == 0), stop=(k == 15)),
                 reads=[h2fT, rwt], writes=[pm], pe_accum=True)
        m.op("dve", lambda e: e.tensor_tensor(lg[:], pm[:, 0:36], rbt[:], ALU.add), reads=[pm, rbt], writes=[lg])
        S = sm
        def dv(fn, r, w):
            m.op("dve", fn, reads=r, writes=w)
        dv(lambda e: e.reduce_max(S["m1"][:], lg[:, 0:4], AX.X), [lg], [S["m1"]])
        dv(lambda e: e.tensor_scalar(S["ohg"][:], lg[:, 0:4], S["m1"][:, 0:1], None, ALU.is_equal), [lg, S["m1"]], [S["ohg"]])
        dv(lambda e: e.tensor_scalar_mul(S["nm1"][:], S["m1"][:], -1.0), [S["m1"]], [S["nm1"]])
        m.op("act", lambda e: e.activation(S["e1"][:], lg[:, 0:4], AF.Exp, bias=S["nm1"][:, 0:1], accum_out=S["s1"][:, 0:1]),
             reads=[lg, S["nm1"]], writes=[S["e1"], S["s1"]])
        dv(lambda e: e.reciprocal(S["pg"][:], S["s1"][:]), [S["s1"]], [S["pg"]])
        l2 = lg[:, 4:36].rearrange("p (g e) -> p g e", g=4)
        t48 = S["t48"][:].rearrange("p (g e) -> p g e", g=4)
        dv(lambda e: e.tensor_tensor(t48, l2, S["ohg"][:].unsqueeze(2).to_broadcast([128, 4, 8]), ALU.mult), [lg, S["ohg"]], [S["t48"]])
        dv(lambda e: e.tensor_reduce(S["sel"][:], S["t48"][:].rearrange("p (g e) -> p e g", g=4), AX.X, ALU.add), [S["t48"]], [S["sel"]])
        dv(lambda e: e.reduce_max(S["m2a"][:], S["sel"][:], AX.X), [S["sel"]], [S["m2a"]])
        dv(lambda e: e.tensor_scalar(S["oh1"][:], S["sel"][:], S["m2a"][:, 0:1], None, ALU.is_equal), [S["sel"], S["m2a"]], [S["oh1"]])
        dv(lambda e: e.scalar_tensor_tensor(S["sel2"][:], S["oh1"][:], -1e30, S["sel"][:], ALU.mult, ALU.add), [S["oh1"], S["sel"]], [S["sel2"]])
        dv(lambda e: e.reduce_max(S["m2b"][:], S["sel2"][:], AX.X), [S["sel2"]], [S["m2b"]])
        dv(lambda e: e.tensor_scalar(S["oh2"][:], S["sel2"][:], S["m2b"][:, 0:1], None, ALU.is_equal), [S["sel2"], S["m2b"]], [S["oh2"]])
        dv(lambda e: e.tensor_scalar_mul(S["nm2a"][:], S["m2a"][:], -1.0), [S["m2a"]], [S["nm2a"]])
        m.op("act", lambda e: e.activation(S["r"][:], S["m2b"][:], AF.Exp, bias=S["nm2a"][:, 0:1]), reads=[S["m2b"], S["nm2a"]], writes=[S["r"]])
        dv(lambda e: e.tensor_scalar_add(S["den"][:], S["r"][:], 1.0), [S["r"]], [S["den"]])
        dv(lambda e: e.reciprocal(S["den"][:], S["den"][:]), [S["den"]], [S["den"]])
        dv(lambda e: e.tensor_tensor(S["w1"][:], S["pg"][:], S["den"][:], ALU.mult), [S["pg"], S["den"]], [S["w1"]])
        dv(lambda e: e.tensor_tensor(S["w2"][:], S["w1"][:], S["r"][:], ALU.mult), [S["w1"], S["r"]], [S["w2"]])
        dv(lambda e: e.tensor_scalar(S["wsel"][:], S["oh1"][:], S["w1"][:, 0:1], None, ALU.mult), [S["oh1"], S["w1"]], [S["wsel"]])
        dv(lambda e: e.scalar_tensor_tensor(S["wsel"][:], S["oh2"][:], S["w2"][:, 0:1], S["wsel"][:], ALU.mult, ALU.add),
           [S["oh2"], S["w2"], S["wsel"]], [S["wsel"]])
        dv(lambda e: e.tensor_tensor(Wt[:, t, :].rearrange("p (g e) -> p g e", g=4), S["ohg"][:].unsqueeze(2).to_broadcast([128, 4, 8]),
                                     S["wsel"][:].unsqueeze(1).to_broadcast([128, 4, 8]), ALU.mult), [S["ohg"], S["wsel"]], [Wt])

    stage_a(0)
    for t in range(16):
        if t + 1 < 16:
            stage_a(t + 1)
        stage_b(t)


def emit_C2(m, nc, es, modv, ln2g, ln2b, wg, wu, wd, x1_d, h2T_d, Wt, x_out, PS, n_exp=32):
    from contextlib import ExitStack

    def mk_sb(stack, tag):
        def sb(shape, dt):
            m.nbuf += 1
            return Buf(stack.enter_context(nc.sbuf_tensor("%s_%d" % (tag, m.nbuf), list(shape), dt)), tag)
        return sb
    sbo = mk_sb(es, "c2o")
    NTOK = 1024; NTT = NTOK // 128; NTG = NTOK // 512
    h2T = sbo([128, 16, NTOK], BF16)
    yacc = sbo([128, NTT, 2048], F32)
    gbanks = PS["z"]; ubanks = PS["o"][0:2]; pyb = PS["o"][2:4]
    for hp in range(2048 // NTOK):
        m.dma(h2T[:], h2T_d[:, :, hp * NTOK:(hp + 1) * NTOK], reads=[h2T_d], writes=[h2T])
        with ExitStack() as es2:
            sb = mk_sb(es2, "c2w")
            hidT = sb([128, 8, NTOK], BF16)
            gst = sb([128, 16, 128], F32); ust = sb([128, 16, 128], F32)
            wgb = [sb([128, 16, 128], BF16) for _ in range(2)]
            wub = [sb([128, 16, 128], BF16) for _ in range(2)]
            dsts = [sb([128, 2048], F32) for _ in range(2)]
            wdb = sb([128, 8, 2048], BF16)
            sg = [sb([128, 512], F32) for _ in range(2)]
            items = [(e, fc) for e in range(n_exp) for fc in range(8)]

            def load_gu(ix):
                e, fc = items[ix]
                m.dma(gst[:], wg[e, :, fc * 128:(fc + 1) * 128].rearrange("(k p) n -> p k n", p=128), writes=[gst])
                m.dma(ust[:], wu[e, :, fc * 128:(fc + 1) * 128].rearrange("(k p) n -> p k n", p=128), writes=[ust])
                m.dma(dsts[ix % 2][:], wd[e, fc * 128:(fc + 1) * 128, :], writes=[dsts[ix % 2]])

            def convert(ix):
                e, fc = items[ix]
                m.op("act", lambda en: en.copy(wgb[ix % 2][:], gst[:]), reads=[gst], writes=[wgb[ix % 2]])
                m.op("pool", lambda en: en.tensor_copy(wub[ix % 2][:], ust[:]), reads=[ust], writes=[wub[ix % 2]])
            load_gu(0)
            convert(0)
            si = 0
            for ix, (e, fc) in enumerate(items):
                gb = wgb[ix % 2]; ub = wub[ix % 2]
                if fc == 0:
                    pass
                m.op("pool", lambda en: en.tensor_copy(wdb[:, fc, :], dsts[ix % 2][:]), reads=[dsts[ix % 2]], writes=[wdb])
                if ix + 1 < len(items):
                    load_gu(ix + 1)
                for tg in range(NTG):
                    a = gbanks[tg]; b = ubanks[tg]; s_ = sg[si % 2]; si += 1
                    for k in range(16):
                        m.op("pe", lambda en: en.matmul(a[:], gb[:, k, :], h2T[:, k, tg * 512:(tg + 1) * 512], start=(k == 0), stop=(k == 15)),
                             reads=[gb, h2T], writes=[a], pe_accum=True)
                    for k in range(16):
                        m.op("pe", lambda en: en.matmul(b[:], ub[:, k, :], h2T[:, k, tg * 512:(tg + 1) * 512], start=(k == 0), stop=(k == 15)),
                             reads=[ub, h2T], writes=[b], pe_accum=True)
                    m.op("act", lambda en: en.activation(s_[:], a[:], AF.Silu), reads=[a], writes=[s_])
                    m.op("dve", lambda en: en.tensor_tensor(hidT[:, fc, tg * 512:(tg + 1) * 512], s_[:], b[:], ALU.mult),
                         reads=[s_, b], writes=[hidT])
                if ix + 1 < len(items):
                    convert(ix + 1)
                if fc == 7:
                    for tt in range(NTT):
                        tile_i = hp * NTT + tt
                        for n in range(4):
                            p = pyb[(tt * 4 + n) % 2]
                            for f2 in range(8):
                                m.op("pe", lambda en: en.matmul(p[:], hidT[:, f2, tt * 128:(tt + 1) * 128], wdb[:, f2, n * 512:(n + 1) * 512],
                                                                start=(f2 == 0), stop=(f2 == 7)),
                                     reads=[hidT, wdb], writes=[p], pe_accum=True)
                            ys = yacc[:, tt, n * 512:(n + 1) * 512]
                            if e == 0:
                                m.op("dve", lambda en: en.tensor_scalar(ys, p[:], Wt[:, tile_i, e:e + 1], None, ALU.mult),
                                     reads=[p, Wt], writes=[yacc])
                            else:
                                m.op("dve", lambda en: en.scalar_tensor_tensor(ys, p[:], Wt[:, tile_i, e:e + 1], ys, ALU.mult, ALU.add),
                                     reads=[p, Wt, yacc], writes=[yacc])
            barrier(m)
        with ExitStack() as es3:
            sb = mk_sb(es3, "c2e")
            bc = {}
            for nm, src in (("gt2", modv[3, :]), ("l2g", ln2g), ("l2b", ln2b)):
                bc[nm] = sb([128, 2048], F32)
                m.dma(bc[nm][:], src.partition_broadcast(128), writes=[bc[nm]])
            xt = [sb([128, 2048], F32) for _ in range(2)]; outt = [sb([128, 2048], F32) for _ in range(2)]
            st = sb([128, 24], F32); mv = sb([128, 2], F32); rstd = sb([128, 1], F32)
            for tt in range(NTT):
                tile_i = hp * NTT + tt
                x_ = xt[tt % 2]; o_ = outt[tt % 2]
                m.dma(x_[:], x1_d[tile_i * 128:(tile_i + 1) * 128, :], reads=[x1_d], writes=[x_])
                m.op("pool", lambda en: en.tensor_tensor(yacc[:, tt, :], yacc[:, tt, :], bc["gt2"][:], ALU.mult), reads=[yacc, bc["gt2"]], writes=[yacc])
                m.op("dve", lambda en: en.scalar_tensor_tensor(x_[:], x_[:], ALPHA, yacc[:, tt, :], ALU.mult, ALU.add), reads=[x_, yacc], writes=[x_])
                ln_affine(m, x_, st, mv, rstd, bc["l2g"], bc["l2b"], o_)
                m.dma(x_out[tile_i * 128:(tile_i + 1) * 128, :], o_[:], reads=[o_], is_output=True)
            barrier(m)


def build_C(n_exp=32, n_alloc=32):
    from contextlib import ExitStack
    nc = bass.Bass("TRN2", target_bir_lowering=False)
    dt = lambda n, s, d, k="ExternalInput": nc.dram_tensor(n, s, d, kind=k).ap()
    x = dt("x", [2048, 2048], F32); o = dt("o", [2048, 2048], BF16); modv = dt("modv", [4, 2048], F32)
    w_out = dt("w_out", [2048, 2048], F32)
    ln1g = dt("ln1g", [2048], F32); ln1b = dt("ln1b", [2048], F32); ln2g = dt("ln2g", [2048], F32); ln2b = dt("ln2b", [2048], F32)
    rw = dt("rw", [2048, 36], F32); rb = dt("rb", [36], F32)
    wg = dt("wg", [n_alloc, 2048, 1024], F32); wu = dt("wu", [n_alloc, 2048, 1024], F32); wd = dt("wd", [n_alloc, 1024, 2048], F32)
    x_out = dt("x_out", [2048, 2048], F32, "ExternalOutput")
    m = MK(nc)
    x1_d = m.dram("x1_d", [2048, 2048], F32)
    h2T_d = m.dram("h2T_d", [128, 16, 2048], BF16)
    identf = make_ident(m, F32)
    identb = m.sb([128, 128], BF16)
    m.op("dve", lambda e: e.tensor_copy(identb[:], identf[:]), reads=[identf], writes=[identb])
    Wt = m.sb([128, 16, 32], F32)
    PS = {"z": [m.ps([128, 512], F32) for _ in range(2)],
          "o": [m.ps([128, 512], F32) for _ in range(4)],
          "T": m.ps([128, 8, 128], BF16),
          "misc": m.ps([128, 512], F32)}
    with ExitStack() as es:
        emit_C1(m, nc, es, x, o, modv, w_out, ln1g, ln1b, rw, rb, x1_d, h2T_d, Wt, identb, identf, PS)
        barrier(m)
    with ExitStack() as es:
        emit_C2(m, nc, es, modv, ln2g, ln2b, wg, wu, wd, x1_d, h2T_d, Wt, x_out, PS, n_exp)
        barrier(m)
    m.finish()
    return nc
def cc_allgather(m, nc, src_ap, dst_ap):
    barrier(m)
    if "cc" not in m.sem:
        m.sem["cc"] = nc.alloc_semaphore("ccsem"); m.cnt["cc"] = 0
    ins = nc.gpsimd.collective_compute("AllGather", ALU.bypass, replica_groups=[[0, 1], [2, 3], [4, 5], [6, 7]],
                                       ins=[src_ap.opt()], outs=[dst_ap.opt()])
    m.cnt["cc"] += 1
    ins.then_inc(m.sem["cc"], 1)
    for e in m.E:
        m.E[e].wait_ge(m.sem["cc"], m.cnt["cc"])


class GB:
    def __init__(self, nc, name, rows, cols, dt, rows_k):
        self.nk = (rows + rows_k - 1) // rows_k; self.rk = rows_k
        self.src = nc.dram_tensor(name, [self.nk * rows_k, cols], dt, kind="Internal").ap()
        self.dst = nc.dram_tensor(name + "g", [self.nk * 2 * rows_k, cols], dt, kind="Internal").ap()

    def gather(self, m, nc):
        rk = self.rk
        for k in range(self.nk):
            cc_allgather(m, nc, self.src[k * rk:(k + 1) * rk, :], self.dst[k * 2 * rk:(k + 1) * 2 * rk, :])

    def g(self, r, lo, hi):
        rk = self.rk
        k = lo // rk
        assert (hi - 1) // rk == k, (lo, hi, rk)
        base = k * 2 * rk + r * rk + (lo - k * rk)
        return self.dst[base:base + (hi - lo), :]


FM_BLOCKS_F = ([(c0, "qk", o0) for c0, o0 in zip(range(0, 512, 256), range(0, 512, 256))] +
               [(c0, "qk", o0) for c0, o0 in zip(range(1536, 3584, 256), range(512, 2560, 256))] +
               [(c0, "fag", o0) for c0, o0 in zip(range(4624, 5648, 256), range(0, 1024, 256))])
TM_BLOCKS_F = ([(c0, o0) for c0, o0 in zip(range(512, 1536, 256), range(0, 1024, 256))] +
               [(c0, o0) for c0, o0 in zip(range(3584, 4608, 256), range(1024, 2048, 256))])


def emit_A2(m, nc, es, x, sh_ap, sc_ap, w_in, qkT, fagT, vtm, ident, PS):
    def sb(shape, dt):
        m.nbuf += 1
        return Buf(es.enter_context(nc.sbuf_tensor("a_%d" % m.nbuf, list(shape), dt)), "a")
    NT = 16
    hT = sb([128, 16, 2048], BF16)
    scb = sb([128, 2048], F32); shb = sb([128, 2048], F32)
    xts = [sb([128, 2048], F32) for _ in range(2)]
    hb = [sb([128, 2048], BF16) for _ in range(2)]
    st = sb([128, 24], F32); mv = sb([128, 2], F32); rstd = sb([128, 1], F32)
    pT = PS["T"]
    m.dma(shb[:], sh_ap.partition_broadcast(128), writes=[shb])
    m.dma(scb[:], sc_ap.partition_broadcast(128), writes=[scb])
    m.op("pool", lambda e: e.tensor_scalar_add(scb[:], scb[:], 1.0), reads=[scb], writes=[scb])
    m.dma(xts[0][:], x[0:128, :], writes=[xts[0]])
    for t in range(NT):
        xt = xts[t % 2]; h = hb[t % 2]
        if t + 1 < NT:
            m.dma(xts[(t + 1) % 2][:], x[(t + 1) * 128:(t + 2) * 128, :], writes=[xts[(t + 1) % 2]])
        ln_stats(m, xt, st, mv, rstd)
        m.op("dve", lambda e: e.tensor_scalar(xt[:], xt[:], mv[:, 0:1], rstd[:, 0:1], ALU.subtract, ALU.mult),
             reads=[xt, mv, rstd], writes=[xt])
        m.op("pool", lambda e: e.tensor_tensor(xt[:], xt[:], scb[:], ALU.mult), reads=[xt, scb], writes=[xt])
        m.op("dve", lambda e: e.tensor_tensor(h[:], xt[:], shb[:], ALU.add), reads=[xt, shb], writes=[h])
        for half in range(2):
            for j in range(8):
                k = half * 8 + j
                m.op("pe", lambda e: e.transpose(pT[:, j, :], h[:, k * 128:(k + 1) * 128], ident[:]),
                     reads=[h, ident], writes=[pT], pe_accum=True)
            m.op("act", lambda e: e.copy(hT[:, half * 8:(half + 1) * 8, t * 128:(t + 1) * 128], pT[:]),
                 reads=[pT], writes=[hT])
    wst = [sb([128, 16, 256], F32) for _ in range(2)]
    wbf = [sb([128, 16, 256], BF16) for _ in range(2)]
    pm = PS["o"]
    oq = [sb([128, 2048], BF16) for _ in range(2)]
    of = [sb([128, 2048], F32) for _ in range(2)]
    ov = [sb([128, 256], BF16) for _ in range(2)]
    blocks = [("fm", c0, kind, o0) for (c0, kind, o0) in FM_BLOCKS_F] + [("tm", c0, None, o0) for (c0, o0) in TM_BLOCKS_F]
    blocks.append(("fm16", 4608, "fag", 1024))
    pi = 0; oi = 0

    def load_w(bi):
        typ, c0, kind, o0 = blocks[bi]
        cw = 16 if typ == "fm16" else 256
        m.dma(wst[bi % 2][:, :, 0:cw], w_in[:, c0:c0 + cw].rearrange("(k p) n -> p k n", p=128), writes=[wst[bi % 2]])
    load_w(0)
    for bi, (typ, c0, kind, o0) in enumerate(blocks):
        cw = 16 if typ == "fm16" else 256
        ws = wst[bi % 2]; wb = wbf[bi % 2]
        if bi + 1 < len(blocks):
            load_w(bi + 1)
        m.op("act", lambda e: e.copy(wb[:, 0:8, 0:cw], ws[:, 0:8, 0:cw]), reads=[ws], writes=[wb])
        m.op("pool", lambda e: e.tensor_copy(wb[:, 8:16, 0:cw], ws[:, 8:16, 0:cw]), reads=[ws], writes=[wb])
        if typ in ("fm", "fm16"):
            for cc in range(0, cw, 128):
                cn = min(128, cw - cc)
                o = (oq if kind == "qk" else of)[oi % 2]; oi += 1
                for tg in range(4):
                    p = pm[pi % 4]; pi += 1
                    for k in range(16):
                        m.op("pe", lambda e: e.matmul(p[0:cn, :], wb[:, k, cc:cc + cn], hT[:, k, tg * 512:(tg + 1) * 512],
                                                      start=(k == 0), stop=(k == 15)),
                             reads=[wb, hT], writes=[p], pe_accum=True)
                    if tg % 2 == 0:
                        m.op("act", lambda e: e.copy(o[0:cn, tg * 512:(tg + 1) * 512], p[0:cn, :]), reads=[p], writes=[o])
                    else:
                        m.op("dve", lambda e: e.tensor_copy(o[0:cn, tg * 512:(tg + 1) * 512], p[0:cn, :]), reads=[p], writes=[o])
                dst = qkT if kind == "qk" else fagT
                m.dma(dst[o0 + cc:o0 + cc + cn, :], o[0:cn, :], reads=[o])
        else:
            for tt in range(16):
                p = pm[pi % 4]; pi += 1
                o = ov[oi % 2]; oi += 1
                for k in range(16):
                    m.op("pe", lambda e: e.matmul(p[:, 0:256], hT[:, k, tt * 128:(tt + 1) * 128], wb[:, k, :],
                                                  start=(k == 0), stop=(k == 15)),
                         reads=[wb, hT], writes=[p], pe_accum=True)
                if tt % 2 == 0:
                    m.op("act", lambda e: e.copy(o[:], p[:, 0:256]), reads=[p], writes=[o])
                else:
                    m.op("dve", lambda e: e.tensor_copy(o[:], p[:, 0:256]), reads=[p], writes=[o])
                m.dma(vtm[tt * 128:(tt + 1) * 128, o0:o0 + 256], o[:], reads=[o])


def blend_tiles(m, sb_pool, sel, dst_fn, srcA_fn, srcB_fn, ntiles, shape, dt, post=None):
    ta, tb = (sb_pool["a"], sb_pool["b"]) if dt == BF16 else (sb_pool["a32"], sb_pool["b32"])
    np_ = shape[0]
    NBUF = len(ta); PF = NBUF - 1

    def views(i):
        a = ta[i % NBUF]; b = tb[i % NBUF]
        return a, b, a[0:np_, 0:shape[1]], b[0:np_, 0:shape[1]]

    def load(i):
        a, b, av, bv = views(i)
        m.dma(av, srcA_fn(i), writes=[a])
        m.dma(bv, srcB_fn(i), writes=[b])
    for i in range(min(PF, ntiles)):
        load(i)
    for i in range(ntiles):
        if i + PF < ntiles:
            load(i + PF)
        a, b, av, bv = views(i)
        m.op("dve", lambda e: e.tensor_scalar(av, av, sel[0:np_, 0:1], None, ALU.mult), reads=[a, sel], writes=[a])
        m.op("dve", lambda e: e.scalar_tensor_tensor(av, bv, sel[0:np_, 1:2], av, ALU.mult, ALU.add), reads=[a, b, sel], writes=[a])
        if post is None:
            m.dma(dst_fn(i), av, reads=[a])
        else:
            post(i, a, av)


def emit_select1(m, nc, es, sel, G1, G2, G3, S, identb, jmat, PS):
    def sb(shape, dt):
        m.nbuf += 1
        return Buf(es.enter_context(nc.sbuf_tensor("s_%d" % m.nbuf, list(shape), dt)), "s")
    pool = {"a": [sb([128, 2048], BF16) for _ in range(4)], "b": [sb([128, 2048], BF16) for _ in range(4)],
            "a32": [sb([128, 2048], F32) for _ in range(3)], "b32": [sb([128, 2048], F32) for _ in range(3)]}
    for r in range(2):
        tk = slice(r * 2048, (r + 1) * 2048)
        for (dst, a0, b0, nrows) in ((S["sbqT"], 0, 256, 256), (S["fxqT"], 512, 1024, 512), (S["fxkT"], 1536, 2048, 512)):
            blend_tiles(m, pool, sel, lambda i: dst[i * 128:(i + 1) * 128, tk], lambda i: G1.g(r, a0 + i * 128, a0 + (i + 1) * 128),
                        lambda i: G1.g(r, b0 + i * 128, b0 + (i + 1) * 128), nrows // 128, [128, 2048], BF16)
        blend_tiles(m, pool, sel, lambda i: S["fxv"][r * 2048 + i * 128:r * 2048 + (i + 1) * 128, :],
                    lambda i: G2.g(r, i * 128, (i + 1) * 128)[:, 1024:1536], lambda i: G2.g(r, i * 128, (i + 1) * 128)[:, 1536:2048], 16, [128, 512], BF16)
        blend_tiles(m, pool, sel, lambda i: S["fT"][:, tk], lambda i: G3.g(r, 1024, 1032), lambda i: G3.g(r, 1032, 1040), 1, [8, 2048], F32)
        blend_tiles(m, pool, sel, lambda i: S["aT"][i * 128:(i + 1) * 128, 30 + r * 2048:30 + (r + 1) * 2048],
                    lambda i: G3.g(r, i * 128, (i + 1) * 128), lambda i: G3.g(r, 256 + i * 128, 256 + (i + 1) * 128), 2, [128, 2048], F32)
        blend_tiles(m, pool, sel, lambda i: S["gT"][i * 128:(i + 1) * 128, 30 + r * 2048:30 + (r + 1) * 2048],
                    lambda i: G3.g(r, 512 + i * 128, 512 + (i + 1) * 128), lambda i: G3.g(r, 768 + i * 128, 768 + (i + 1) * 128), 2, [128, 2048], F32)
        pk = PS["z"]; pv = PS["misc"]
        ko = [sb([64, 4, 128], BF16) for _ in range(2)]
        vo = [sb([128, 256], BF16) for _ in range(2)]

        def post_k(i, a, av):
            gt = r * 16 + i; rb = 31 - gt
            p = pk[i % 2]; o = ko[i % 2]
            for h in range(4):
                m.op("pe", lambda e: e.matmul(p[0:64, h * 128:(h + 1) * 128], a[:, h * 64:(h + 1) * 64], jmat[:], start=True, stop=True),
                     reads=[a, jmat], writes=[p], pe_accum=True)
            m.op("act", lambda e: e.copy(o[:].rearrange("p h t -> p (h t)"), p[0:64, 0:512]), reads=[p], writes=[o])
            m.dma(S["sbkTr"][:, :, rb * 128:(rb + 1) * 128].rearrange("h d t -> d h t"), o[:], reads=[o])

        def post_v(i, a, av):
            gt = r * 16 + i; rb = 31 - gt
            o = vo[i % 2]
            m.op("pe", lambda e: e.matmul(pv[:, 0:256], jmat[:], a[:, 0:256], start=True, stop=True), reads=[a, jmat], writes=[pv])
            m.op("act", lambda e: e.copy(o[:], pv[:, 0:256]), reads=[pv], writes=[o])
            m.dma(S["sbvr"][rb * 128:(rb + 1) * 128, :], o[:], reads=[o])
        blend_tiles(m, pool, sel, None, lambda i: G2.g(r, i * 128, (i + 1) * 128)[:, 0:256], lambda i: G2.g(r, i * 128, (i + 1) * 128)[:, 256:512],
                    16, [128, 256], BF16, post=post_k)
        blend_tiles(m, pool, sel, None, lambda i: G2.g(r, i * 128, (i + 1) * 128)[:, 512:768], lambda i: G2.g(r, i * 128, (i + 1) * 128)[:, 768:1024],
                    16, [128, 256], BF16, post=post_v)
    z = sb([128, 30], F32)
    m.op("pool", lambda e: e.memset(z[:], 0.0), writes=[z])
    for i in range(2):
        m.dma(S["aT"][i * 128:(i + 1) * 128, 0:30], z[:], reads=[z])
        m.dma(S["gT"][i * 128:(i + 1) * 128, 0:30], z[:], reads=[z])


def emit_select2(m, nc, es, sel, G5, ocv, So):
    def sb(shape, dt):
        m.nbuf += 1
        return Buf(es.enter_context(nc.sbuf_tensor("s2_%d" % m.nbuf, list(shape), dt)), "s2")
    pool = {"a": [sb([128, 512], BF16) for _ in range(4)], "b": [sb([128, 512], BF16) for _ in range(4)]}
    rows = lambda hf, i: (hf * 2048 + i * 128, hf * 2048 + (i + 1) * 128)
    for (src_fn, c0, cw) in ((lambda hf, i: G5.g(0, *rows(hf, i))[:, 0:256], 0, 256), (lambda hf, i: G5.g(1, *rows(hf, i))[:, 0:256], 256, 256),
                             (lambda hf, i: G5.g(0, *rows(hf, i))[:, 256:768], 512, 512), (lambda hf, i: G5.g(1, *rows(hf, i))[:, 256:768], 1024, 512),
                             (lambda hf, i: ocv[rows(hf, i)[0]:rows(hf, i)[1], :], 1536, 512)):
        blend_tiles(m, pool, sel, lambda i: So[i * 128:(i + 1) * 128, c0:c0 + cw], lambda i: src_fn(0, i), lambda i: src_fn(1, i),
                    16, [128, cw], BF16)


def emit_conv2(m, nc, es, aT, gT, cwT, cb, G4):
    def sb(shape, dt):
        m.nbuf += 1
        return Buf(es.enter_context(nc.sbuf_tensor("cv%d" % m.nbuf, list(shape), dt)), "cv")
    a = sb([128, 4126], F32); g = sb([128, 4126], F32); acc = sb([128, 4096], F32)
    cw = sb([128, 2, 31], F32); cbt = sb([128, 2], F32)
    m.dma(cw[:], cwT, writes=[cw])
    m.dma(cbt[:], cb, writes=[cbt])
    for cc in range(2):
        m.dma(a[:], aT[cc * 128:(cc + 1) * 128, :], writes=[a])
        m.dma(g[:], gT[cc * 128:(cc + 1) * 128, :], writes=[g])
        m.op("act", lambda e: e.activation(g[:], g[:], AF.Sigmoid), reads=[g], writes=[g])
        m.op("dve", lambda e: e.tensor_tensor(a[:], a[:], g[:], ALU.mult), reads=[a, g], writes=[a])
        m.op("dve", lambda e: e.tensor_scalar(acc[:], a[:, 0:4096], cw[:, cc, 0:1], cbt[:, cc:cc + 1], ALU.mult, ALU.add),
             reads=[a, cw, cbt], writes=[acc])
        for w in range(1, 31):
            m.op("dve", lambda e: e.scalar_tensor_tensor(acc[:], a[:, w:w + 4096], cw[:, cc, w:w + 1], acc[:], ALU.mult, ALU.add),
                 reads=[a, cw, acc], writes=[acc])
        m.dma(G4[cc * 128:(cc + 1) * 128, :], acc[:], reads=[acc])


def emit_convln(m, nc, es, G4, lng, lnb, o_cv, identf, PS):
    def sb(shape, dt):
        m.nbuf += 1
        return Buf(es.enter_context(nc.sbuf_tensor("cl%d" % m.nbuf, list(shape), dt)), "cl")
    cvT = sb([128, 4, 4096], F32)
    lg = sb([128, 512], F32); lb = sb([128, 512], F32)
    m.dma(lg[:], lng.partition_broadcast(128), writes=[lg])
    m.dma(lb[:], lnb.partition_broadcast(128), writes=[lb])
    for cc in range(4):
        m.dma(cvT[:, cc, :], G4.g(cc // 2, (cc % 2) * 128, (cc % 2 + 1) * 128), writes=[cvT])
    pt = PS["z"]
    ut = [sb([128, 512], F32) for _ in range(2)]
    ob = [sb([128, 512], BF16) for _ in range(2)]
    st = sb([128, 6], F32); mv = sb([128, 2], F32); rstd = sb([128, 1], F32)
    for t in range(32):
        p = pt[t % 2]; u = ut[t % 2]; o = ob[t % 2]
        for cc in range(4):
            m.op("pe", lambda e: e.transpose(p[:, cc * 128:(cc + 1) * 128], cvT[:, cc, t * 128:(t + 1) * 128], identf[:]),
                 reads=[cvT, identf], writes=[p], pe_accum=True)
        m.op("act", lambda e: e.copy(u[:], p[:]), reads=[p], writes=[u])
        m.op("dve", lambda e: e.bn_stats(st[:], u[:]), reads=[u], writes=[st])
        m.op("dve", lambda e: e.bn_aggr(mv[:], st[:]), reads=[st], writes=[mv])
        m.op("dve", lambda e: e.tensor_scalar_add(rstd[:], mv[:, 1:2], LN_EPS), reads=[mv], writes=[rstd])
        m.op("act", lambda e: e.activation(rstd[:], rstd[:], AF.Sqrt), reads=[rstd], writes=[rstd])
        m.op("dve", lambda e: e.reciprocal(rstd[:], rstd[:]), reads=[rstd], writes=[rstd])
        m.op("dve", lambda e: e.tensor_scalar(u[:], u[:], mv[:, 0:1], rstd[:, 0:1], ALU.subtract, ALU.mult),
             reads=[u, mv, rstd], writes=[u])
        m.op("pool", lambda e: e.tensor_tensor(u[:], u[:], lg[:], ALU.mult), reads=[u, lg], writes=[u])
        m.op("dve", lambda e: e.tensor_tensor(u[:], u[:], lb[:], ALU.add), reads=[u, lb], writes=[u])
        m.op("act", lambda e: e.activation(o[:], u[:], AF.Silu), reads=[u], writes=[o])
        m.dma(o_cv[t * 128:(t + 1) * 128, :], o[:], reads=[o])


def emit_mod(m, nc, es, cT, aw, ab, Gm, PS):
    def sb(shape, dt):
        m.nbuf += 1
        return Buf(es.enter_context(nc.sbuf_tensor("mo%d" % m.nbuf, list(shape), dt)), "mo")
    ct = sb([128, 16], F32); sc = sb([128, 16], F32)
    wst = [sb([128, 16, 512], F32) for _ in range(2)]
    bt = [sb([1, 512], F32) for _ in range(2)]
    ot = [sb([1, 512], F32) for _ in range(2)]
    pp = PS["z"]
    m.dma(ct[:], cT, writes=[ct])
    m.op("act", lambda e: e.activation(sc[:], ct[:], AF.Silu), reads=[ct], writes=[sc])
    items = [(l, n) for l in range(2) for n in range(12)]

    def load(i):
        l, n = items[i]
        m.dma(wst[i % 2][:], aw[l, :, n * 512:(n + 1) * 512].rearrange("(k p) n -> p k n", p=128), writes=[wst[i % 2]])
        m.dma(bt[i % 2][:], ab[l:l + 1, n * 512:(n + 1) * 512], writes=[bt[i % 2]])
    load(0)
    for i, (l, n) in enumerate(items):
        if i + 1 < len(items):
            load(i + 1)
        w = wst[i % 2]; b = bt[i % 2]; o = ot[i % 2]; p = pp[i % 2]
        for k in range(16):
            m.op("pe", lambda e: e.matmul(p[0:1, :], sc[:, k:k + 1], w[:, k, :], start=(k == 0), stop=(k == 15)),
                 reads=[sc, w], writes=[p], pe_accum=True)
        m.op("dve", lambda e: e.tensor_tensor(o[:], p[0:1, :], b[:], ALU.add), reads=[p, b], writes=[o])
        m.dma(Gm[l:l + 1, n * 512:(n + 1) * 512], o[:], reads=[o])


def build_F(n_exp=32, depth=DEPTH, n_alloc=32):
    from contextlib import ExitStack
    nc = bass.Bass("TRN2", target_bir_lowering=False)
    dt = lambda n, s, d, k="ExternalInput": nc.dram_tensor(n, s, d, kind=k).ap()
    x_in = dt("x", [2048, 2048], F32); cT = dt("cT", [128, 16], F32); sel_d = dt("sel", [128, 2], F32)
    aw = dt("aw", [2, 2048, 6144], F32); ab = dt("ab", [2, 6144], F32)
    w_in = dt("w_in", [2, 2048, MIX_COLS], F32); w_out = dt("w_out", [2, 2048, 2048], F32)
    bfg = dt("bfg", [2, 8, 1], F32); cwT = dt("cwT", [2, 128, 2, 31], F32); cb = dt("cb", [2, 128, 2], F32)
    lng = dt("lng", [2, 512], F32); lnb = dt("lnb", [2, 512], F32)
    ln1g = dt("ln1g", [2, 2048], F32); ln1b = dt("ln1b", [2, 2048], F32); ln2g = dt("ln2g", [2, 2048], F32); ln2b = dt("ln2b", [2, 2048], F32)
    rw = dt("rw", [2, 2048, 36], F32); rb = dt("rb", [2, 36], F32)
    wg = dt("wg", [2, n_alloc, 2048, 1024], F32); wu = dt("wu", [2, n_alloc, 2048, 1024], F32); wd = dt("wd", [2, n_alloc, 1024, 2048], F32)
    x_out = dt("x_out", [2048, 2048], F32, "ExternalOutput")
    m = MK(nc)
    D = lambda n, s, d: nc.dram_tensor(n, list(s), d, kind="Internal").ap()
    Gm = GB(nc, "Gm", 2, 6144, F32, 2)
    G1 = GB(nc, "G1", 2560, 2048, BF16, 512); G2 = GB(nc, "G2", 2048, 2048, BF16, 512)
    G3 = GB(nc, "G3", 1040, 2048, F32, 256); G4 = GB(nc, "G4", 256, 4096, F32, 128); G5 = GB(nc, "G5", 4096, 768, BF16, 1024)
    S = {"sbqT": D("S_sbqT", [256, 4096], BF16), "fxqT": D("S_fxqT", [512, 4096], BF16), "fxkT": D("S_fxkT", [512, 4096], BF16),
         "sbkTr": D("S_sbkTr", [4, 64, 4096], BF16), "sbvr": D("S_sbvr", [4096, 256], BF16), "fxv": D("S_fxv", [4096, 512], BF16),
         "fT": D("S_fT", [8, 4096], F32), "aT": D("S_aT", [256, 4126], F32), "gT": D("S_gT", [256, 4126], F32)}
    ocv = D("ocv", [4096, 512], BF16); So = D("So", [2048, 2048], BF16)
    x_mid = D("x_mid", [2048, 2048], F32)
    x1_d = m.dram("x1_d", [2048, 2048], F32)
    h2T_d = m.dram("h2T_d", [128, 16, 2048], BF16)
    identf = make_ident(m, F32)
    identb = m.sb([128, 128], BF16)
    m.op("dve", lambda e: e.tensor_copy(identb[:], identf[:]), reads=[identf], writes=[identb])
    jf = m.sb([128, 128], F32); jmat = m.sb([128, 128], BF16)
    m.op("pool", lambda e: e.memset(jf[:], 1.0), writes=[jf])
    m.op("pool", lambda e: e.affine_select(out=jf[:], in_=jf[:], pattern=[[1, 128]], compare_op=ALU.is_equal, fill=0.0,
                                           base=-127, channel_multiplier=1), reads=[jf], writes=[jf])
    m.op("dve", lambda e: e.tensor_copy(jmat[:], jf[:]), reads=[jf], writes=[jmat])
    sel = m.sb([128, 2], F32)
    m.dma(sel[:], sel_d, writes=[sel])
    Wt = m.sb([128, 16, 32], F32)
    PS = {"z": [m.ps([128, 512], F32) for _ in range(2)],
          "o": [m.ps([128, 512], F32) for _ in range(4)],
          "T": m.ps([128, 8, 128], BF16),
          "misc": m.ps([128, 512], F32)}
    with ExitStack() as es:
        emit_mod(m, nc, es, cT, aw, ab, Gm.src, PS)
    Gm.gather(m, nc)
    modsec = lambda l, s: Gm.dst[2 * (s // 3) + l, (s % 3) * 2048:(s % 3 + 1) * 2048]
    for l in range(depth):
        xin = x_in if l == 0 else x_mid
        xo = x_out if l == depth - 1 else x_mid
        with ExitStack() as es:
            emit_A2(m, nc, es, xin, modsec(l, 0), modsec(l, 1), w_in[l], G1.src, G3.src, G2.src, identb, PS)
        G1.gather(m, nc); G2.gather(m, nc); G3.gather(m, nc)
        with ExitStack() as es:
            emit_select1(m, nc, es, sel, G1, G2, G3, S, identb, jmat, PS)
            barrier(m)
        with ExitStack() as es:
            emit_conv2(m, nc, es, S["aT"], S["gT"], cwT[l], cb[l], G4.src)
        G4.gather(m, nc)
        with ExitStack() as es:
            emit_convln(m, nc, es, G4, lng[l], lnb[l], ocv, identf, PS)
            barrier(m)
        with ExitStack() as es:
            emit_sb(m, nc, es, S["sbqT"].rearrange("(h d) t -> h d t", d=64), S["sbkTr"], S["sbvr"], G5.src[:, 0:256], identb, PSB_fix(PS), 4)
            barrier(m)
        with ExitStack() as es:
            emit_fox(m, nc, es, S["fxqT"].rearrange("(h d) t -> h d t", d=64), S["fxkT"].rearrange("(h d) t -> h d t", d=64),
                     S["fxv"], S["fT"], bfg[l], G5.src[:, 256:768], identf, PS, 8)
        G5.gather(m, nc)
        with ExitStack() as es:
            emit_select2(m, nc, es, sel, G5, ocv, So)
            barrier(m)
        modv4 = [modsec(l, 2), modsec(l, 3), modsec(l, 4), modsec(l, 5)]
        with ExitStack() as es:
            emit_C1(m, nc, es, xin, So, ModV(modv4), w_out[l], ln1g[l], ln1b[l], rw[l], rb[l], x1_d, h2T_d, Wt, identb, identf, PS)
            barrier(m)
        with ExitStack() as es:
            emit_C2(m, nc, es, ModV(modv4), ln2g[l], ln2b[l], wg[l], wu[l], wd[l], x1_d, h2T_d, Wt, xo, PS, n_exp)
            barrier(m)
    m.finish()
    return nc


class ModV:
    def __init__(self, aps):
        self.aps = aps

    def __getitem__(self, idx):
        return self.aps[idx[0]]


def PSB_fix(PS):
    d = dict(PS)
    d["T"] = [PS["T"], PS["T"]]
    return d
import ml_dtypes
from concourse.bass_utils import run_bass_kernel_spmd

_PROGS = {}


def _fused_in_maps(x, c, ada_w, ada_b, w_in, b_forget, conv_w, conv_b, conv_ln_g, conv_ln_b,
                   w_out, ln1_g, ln1_b, r1_w, r1_b, r2_w, r2_b, w_gate, w_up, w_down, ln2_g, ln2_b, n_alloc=32):
    f = lambda a: np.ascontiguousarray(np.asarray(a, dtype=np.float32))
    A = lambda a: np.asarray(a)
    rw = f(np.concatenate([A(r1_w), A(r2_w).transpose(0, 2, 1, 3).reshape(2, 2048, 32)], axis=2))
    rb = f(np.concatenate([A(r1_b), A(r2_b).reshape(2, 32)], axis=1))
    shared = {"w_in": f(w_in), "w_out": f(w_out), "lng": f(conv_ln_g), "lnb": f(conv_ln_b), "ln1g": f(ln1_g), "ln1b": f(ln1_b),
              "ln2g": f(ln2_g), "ln2b": f(ln2_b), "rw": rw, "rb": rb,
              "wg": f(A(w_gate)[:, :n_alloc]), "wu": f(A(w_up)[:, :n_alloc]), "wd": f(A(w_down)[:, :n_alloc])}
    maps = []
    for i in range(8):
        b, j = divmod(i, 2)
        sel = np.zeros((128, 2), np.float32); sel[:, j] = 1.0
        d = dict(shared)
        d["x"] = f(A(x)[b, j * 2048:(j + 1) * 2048])
        d["cT"] = f(A(c)[b].reshape(16, 128).T)
        d["sel"] = sel
        d["aw"] = f(A(ada_w)[:, :, j * 6144:(j + 1) * 6144]); d["ab"] = f(A(ada_b)[:, j * 6144:(j + 1) * 6144])
        d["bfg"] = f(A(b_forget)[:, 8 * j:8 * j + 8].reshape(2, 8, 1))
        d["cwT"] = f(A(conv_w)[:, :, j * 256:(j + 1) * 256].transpose(0, 2, 1).reshape(2, 2, 128, 31).transpose(0, 2, 1, 3))
        d["cb"] = f(A(conv_b)[:, j * 256:(j + 1) * 256].reshape(2, 2, 128).transpose(0, 2, 1))
        maps.append(d)
    return maps


def kernel(**inputs):
    if "F" not in _PROGS:
        _PROGS["F"] = build_F()
    maps = _fused_in_maps(**inputs)
    res = run_bass_kernel_spmd(_PROGS["F"], maps, core_ids=list(range(8)))
    xs = [np.asarray(r["x_out"]) for r in res.results]
    out = np.stack([np.concatenate([xs[2 * b], xs[2 * b + 1]], axis=0) for b in range(4)], axis=0)
    return out.astype(np.float32)
```
